# Optimizing a Trainium2 kernel written in Bass

```python
import math
import jax, jax.numpy as jnp
from jax import lax
import numpy as np

D_MODEL = 1024
BATCH = 16
SEQ = 2048
DEPTH = 4

N_MIXERS = 2
RMS_EPS = 1e-6
LN_EPS = 1e-5
S5_WIDTH = D_MODEL
S5_GROUP = 16
S5_GROUPS = S5_WIDTH // S5_GROUP
S5_STATE = 64
S5_DT_MIN = 1e-3
S5_DT_MAX = 1e-1
ML_WIDTH = 2 * D_MODEL
ML_HEADS = 4
ML_HEAD_DIM = ML_WIDTH // ML_HEADS
ML_QKV_BLOCK = 4
ML_CONV = 5
ML_CHUNK = 128
N_S5_LAYERS = (DEPTH + 1) // 2
N_ML_LAYERS = DEPTH // 2

kernel_name = "bidir_s5_mlstm_hybrid_trunk"


def rmsnorm(x, g):
    xf = x.astype(jnp.float32)
    y = xf * lax.rsqrt(jnp.mean(xf * xf, axis=-1, keepdims=True) + RMS_EPS)
    return (y * g.astype(jnp.float32)).astype(x.dtype)


def ada_modulation(c, w, b):
    mod = jax.nn.silu(c) @ w + b
    shift, scale, gate = jnp.split(mod, 3, axis=-1)
    return shift[:, None, :], scale[:, None, :], gate[:, None, :]


def s5_direction(u, lam_re, lam_im, log_dt, b_re, b_im, c_re, c_im, reverse):
    L = u.shape[1]
    dt = jnp.exp(log_dt)[:, None]
    mag = jnp.exp(lam_re * dt)
    a_re = mag * jnp.cos(lam_im * dt)
    a_im = mag * jnp.sin(lam_im * dt)
    den = lam_re * lam_re + lam_im * lam_im
    z_re = ((a_re - 1.0) * lam_re + a_im * lam_im) / den
    z_im = (a_im * lam_re - (a_re - 1.0) * lam_im) / den
    bb_re = z_re[..., None] * b_re - z_im[..., None] * b_im
    bb_im = z_re[..., None] * b_im + z_im[..., None] * b_re
    bu_re = jnp.einsum('blgp,gnp->blgn', u, bb_re)
    bu_im = jnp.einsum('blgp,gnp->blgn', u, bb_im)
    ar = jnp.broadcast_to(a_re[None, None], (1, L) + a_re.shape)
    ai = jnp.broadcast_to(a_im[None, None], (1, L) + a_im.shape)

    def combine(e1, e2):
        a1r, a1i, b1r, b1i = e1
        a2r, a2i, b2r, b2i = e2
        return (a2r * a1r - a2i * a1i,
                a2r * a1i + a2i * a1r,
                a2r * b1r - a2i * b1i + b2r,
                a2r * b1i + a2i * b1r + b2i)

    _, _, s_re, s_im = lax.associative_scan(combine, (ar, ai, bu_re, bu_im), axis=1, reverse=reverse)
    return jnp.einsum('blgn,gpn->blgp', s_re, c_re) - jnp.einsum('blgn,gpn->blgp', s_im, c_im)


def s5_mixer(h, w_in, lam_re, lam_im, log_dt, b_re, b_im, c_re, c_im, d_skip, w_glu, b_glu, w_out):
    Bsz, L, _ = h.shape
    u, z = jnp.split(h @ w_in, 2, axis=-1)
    ug = u.reshape(Bsz, L, S5_GROUPS, S5_GROUP)
    y_f = s5_direction(ug, lam_re[0], lam_im[0], log_dt[0], b_re[0], b_im[0], c_re[0], c_im[0], False)
    y_b = s5_direction(ug, lam_re[1], lam_im[1], log_dt[1], b_re[1], b_im[1], c_re[1], c_im[1], True)
    y = (y_f + y_b).reshape(Bsz, L, S5_WIDTH) + d_skip * u
    y = jax.nn.gelu(y)
    y = y * jax.nn.sigmoid(y @ w_glu + b_glu)
    return (y * jax.nn.silu(z)) @ w_out


def depthwise_conv_centred(x, w, b):
    k = w.shape[0]
    y = lax.conv_general_dilated(x, w[:, None, :], window_strides=(1,),
                                 padding=[(k // 2, k // 2)],
                                 dimension_numbers=('NWC', 'WIO', 'NWC'),
                                 feature_group_count=x.shape[-1])
    return y + b


def headwise_linear(x, w):
    Bsz, L, E = x.shape
    xb = x.reshape(Bsz, L, E // ML_QKV_BLOCK, ML_QKV_BLOCK)
    return jnp.einsum('blnj,njk->blnk', xb, w).reshape(Bsz, L, E)


def mlstm_chunkwise(q, k, v, i_pre, f_pre):
    Bsz, H, L, d = q.shape
    T = ML_CHUNK
    nc = L // T
    q = q.astype(jnp.float32)
    k = k.astype(jnp.float32) * (d ** -0.5)
    v = v.astype(jnp.float32)
    li = i_pre.astype(jnp.float32)
    lf = jax.nn.log_sigmoid(f_pre.astype(jnp.float32))

    def chunks(t):
        return jnp.moveaxis(t.reshape((Bsz, H, nc, T) + t.shape[3:]), 2, 0)

    xs = (chunks(q), chunks(k), chunks(v), chunks(li), chunks(lf))
    lower = jnp.tril(jnp.ones((T, T), dtype=bool))

    def step(carry, inp):
        C, n, m = carry
        qt, kt, vt, it, ft = inp
        b = jnp.cumsum(ft, axis=-1)
        dmat = b[..., :, None] - b[..., None, :] + it[..., None, :]
        dmat = jnp.where(lower, dmat, -jnp.inf)
        inter = b + m[..., None]
        m_t = jnp.maximum(inter, jnp.max(dmat, axis=-1))
        w_intra = jnp.exp(dmat - m_t[..., None])
        w_inter = jnp.exp(inter - m_t)
        s = jnp.einsum('bhtd,bhsd->bhts', qt, kt) * w_intra
        num = jnp.einsum('bhts,bhsd->bhtd', s, vt) + w_inter[..., None] * jnp.einsum('bhtd,bhde->bhte', qt, C)
        nq = jnp.sum(s, axis=-1) + w_inter * jnp.einsum('bhtd,bhd->bht', qt, n)
        h = num / jnp.maximum(jnp.abs(nq), jnp.exp(-m_t))[..., None]
        bT = b[..., -1]
        g = bT[..., None] - b + it
        m_new = jnp.maximum(bT + m, jnp.max(g, axis=-1))
        wk = jnp.exp(g - m_new[..., None])
        decay = jnp.exp(bT + m - m_new)
        kw = kt * wk[..., None]
        C_new = decay[..., None, None] * C + jnp.einsum('bhsd,bhse->bhde', kw, vt)
        n_new = decay[..., None] * n + jnp.sum(kw, axis=2)
        return (C_new, n_new, m_new), h

    init = (jnp.zeros((Bsz, H, d, d), jnp.float32),
            jnp.zeros((Bsz, H, d), jnp.float32),
            jnp.zeros((Bsz, H), jnp.float32))
    _, hs = lax.scan(step, init, xs)
    return jnp.moveaxis(hs, 0, 2).reshape(Bsz, H, L, d)


def mlstm_mixer(h, w_in, conv_w, conv_b, w_q, w_k, w_v, w_gates, b_gates, gn_w, skip, w_out):
    Bsz, L, _ = h.shape
    xm, z = jnp.split(h @ w_in, 2, axis=-1)
    xc = jax.nn.silu(depthwise_conv_centred(xm, conv_w, conv_b))
    q = headwise_linear(xc, w_q)
    k = headwise_linear(xc, w_k)
    v = headwise_linear(xm, w_v)
    gates = (jnp.einsum('ble,eg->blg', q, w_gates[0]) + jnp.einsum('ble,eg->blg', k, w_gates[1])
             + jnp.einsum('ble,eg->blg', v, w_gates[2]) + b_gates).astype(jnp.float32)
    gates = gates.reshape(Bsz, L, 2, 2, ML_HEADS).transpose(2, 3, 0, 4, 1)

    def heads(t):
        return t.reshape(Bsz, L, ML_HEADS, ML_HEAD_DIM).transpose(0, 2, 1, 3)

    def flip(t):
        return jnp.flip(t, axis=2)

    qh, kh, vh = heads(q), heads(k), heads(v)
    h_f = mlstm_chunkwise(qh, kh, vh, gates[0, 0], gates[0, 1])
    h_b = flip(mlstm_chunkwise(flip(qh), flip(kh), flip(vh), flip(gates[1, 0]), flip(gates[1, 1])))
    hs = h_f + h_b
    mu = jnp.mean(hs, axis=-1, keepdims=True)
    var = jnp.mean(jnp.square(hs - mu), axis=-1, keepdims=True)
    hn = (hs - mu) * lax.rsqrt(var + LN_EPS)
    hn = hn.transpose(0, 2, 1, 3).reshape(Bsz, L, ML_WIDTH).astype(h.dtype) * gn_w
    y = (hn + skip * xc) * jax.nn.silu(z)
    return y @ w_out


def setup_inputs(seed: int = 0) -> dict:
    key = jax.random.key(seed)
    ks = iter(jax.random.split(key, 40))
    f32 = jnp.float32

    def nrm(shape, scale):
        return jax.random.normal(next(ks), shape, f32) * scale

    D, EA, EM, G, N, P, H = D_MODEL, S5_WIDTH, ML_WIDTH, S5_GROUPS, S5_STATE, S5_GROUP, ML_HEADS
    nA, nB = N_S5_LAYERS, N_ML_LAYERS
    inp = {}
    inp['x'] = nrm((BATCH, SEQ, D), 1.0)
    inp['c'] = nrm((BATCH, D), 1.0)
    inp['ada_w'] = nrm((DEPTH, D, 3 * D), 0.5 * D ** -0.5)
    inp['ada_b'] = nrm((DEPTH, 3 * D), 0.01)
    inp['norm_g'] = 1.0 + nrm((DEPTH, D), 0.02)
    inp['s5_w_in'] = nrm((nA, D, 2 * EA), D ** -0.5)
    n_idx = jnp.arange(N, dtype=f32)
    inp['s5_lam_re'] = -0.5 + nrm((nA, 2, G, N), 0.01)
    inp['s5_lam_im'] = math.pi * n_idx + nrm((nA, 2, G, N), 0.01)
    inp['s5_log_dt'] = jax.random.uniform(next(ks), (nA, 2, G), f32,
                                          math.log(S5_DT_MIN), math.log(S5_DT_MAX))
    inp['s5_b_re'] = nrm((nA, 2, G, N, P), (2.0 * P) ** -0.5)
    inp['s5_b_im'] = nrm((nA, 2, G, N, P), (2.0 * P) ** -0.5)
    inp['s5_c_re'] = nrm((nA, 2, G, P, N), (2.0 * N) ** -0.5)
    inp['s5_c_im'] = nrm((nA, 2, G, P, N), (2.0 * N) ** -0.5)
    inp['s5_d'] = nrm((nA, EA), 0.5)
    inp['s5_w_glu'] = nrm((nA, EA, EA), EA ** -0.5)
    inp['s5_b_glu'] = nrm((nA, EA), 0.01)
    inp['s5_w_out'] = nrm((nA, EA, D), EA ** -0.5)
    inp['ml_w_in'] = nrm((nB, D, 2 * EM), D ** -0.5)
    inp['ml_conv_w'] = nrm((nB, ML_CONV, EM), ML_CONV ** -0.5)
    inp['ml_conv_b'] = nrm((nB, EM), 0.01)
    nblk = EM // ML_QKV_BLOCK
    inp['ml_w_q'] = nrm((nB, nblk, ML_QKV_BLOCK, ML_QKV_BLOCK), ML_QKV_BLOCK ** -0.5)
    inp['ml_w_k'] = nrm((nB, nblk, ML_QKV_BLOCK, ML_QKV_BLOCK), ML_QKV_BLOCK ** -0.5)
    inp['ml_w_v'] = nrm((nB, nblk, ML_QKV_BLOCK, ML_QKV_BLOCK), ML_QKV_BLOCK ** -0.5)
    inp['ml_w_gates'] = nrm((nB, 3, EM, 4 * H), 0.1 * (3.0 * EM) ** -0.5)
    f_bias = jnp.linspace(3.0, 6.0, H, dtype=f32)
    i_bias = nrm((nB, 2, 1, H), 0.1)
    fb = f_bias + nrm((nB, 2, 1, H), 0.1)
    inp['ml_b_gates'] = jnp.concatenate([i_bias, fb], axis=2).reshape(nB, 4 * H)
    inp['ml_gn_w'] = 1.0 + nrm((nB, EM), 0.02)
    inp['ml_skip'] = 1.0 + nrm((nB, EM), 0.02)
    inp['ml_w_out'] = nrm((nB, EM, D), EM ** -0.5)
    inp['final_g'] = 1.0 + nrm((D,), 0.02)
    return inp


def reference(x, c, ada_w, ada_b, norm_g,
              s5_w_in, s5_lam_re, s5_lam_im, s5_log_dt, s5_b_re, s5_b_im, s5_c_re, s5_c_im,
              s5_d, s5_w_glu, s5_b_glu, s5_w_out,
              ml_w_in, ml_conv_w, ml_conv_b, ml_w_q, ml_w_k, ml_w_v, ml_w_gates, ml_b_gates,
              ml_gn_w, ml_skip, ml_w_out, final_g):
    for i in range(DEPTH):
        shift, scale, gate = ada_modulation(c, ada_w[i], ada_b[i])
        h = rmsnorm(x, norm_g[i]) * (1.0 + scale) + shift
        j = i // N_MIXERS
        if i % N_MIXERS == 0:
            y = s5_mixer(h, s5_w_in[j], s5_lam_re[j], s5_lam_im[j], s5_log_dt[j],
                         s5_b_re[j], s5_b_im[j], s5_c_re[j], s5_c_im[j],
                         s5_d[j], s5_w_glu[j], s5_b_glu[j], s5_w_out[j])
        else:
            y = mlstm_mixer(h, ml_w_in[j], ml_conv_w[j], ml_conv_b[j], ml_w_q[j], ml_w_k[j], ml_w_v[j],
                            ml_w_gates[j], ml_b_gates[j], ml_gn_w[j], ml_skip[j], ml_w_out[j])
        x = x + gate * y
    return rmsnorm(x, final_g)
```

```python
import numpy as np
from contextlib import ExitStack
import concourse.bass as bass
import concourse.mybir as mybir
from concourse.ap import AP
from concourse.bass_utils import run_bass_kernel_spmd

F32 = mybir.dt.float32
BF16 = mybir.dt.bfloat16
I32 = mybir.dt.int32
ALU = mybir.AluOpType
AF = mybir.ActivationFunctionType
AX = mybir.AxisListType

NCORES = 8
D = 1024
L = 2048
NB = 2
NT = NB * L
TB = 512
NTB = NT // TB
SEM_LIMIT = 30000
N_DMA_SEMS = 12
TWO_PI = float(2 * np.pi)
SERIAL_DMA = True


class FW:
    ENGS = ("pe", "act", "dve", "pool", "sp")
    same_engine_sync = True

    def __init__(self, nc):
        self.nc = nc
        self.prog = {e: [] for e in self.ENGS}
        self.cur_sem = {}
        self.cnt = {}
        self.nsem = 0
        for e in self.ENGS:
            self._new_sem(e)
        self.waited = {}
        self.res = {}
        self.dma_sems = {}
        self.dma_rr = {}
        for e in ("sp", "act", "pool"):
            self.dma_sems[e] = [[self._alloc_sem(f"dma_{e}_{i}"), 0] for i in range(N_DMA_SEMS)]
            self.dma_rr[e] = 0
        self.n_inst = 0
        self.last_dma = {}

    def _alloc_sem(self, name):
        self.nsem += 1
        return self.nc.alloc_semaphore(name=f"{name}_{self.nsem}")

    def _new_sem(self, e):
        self.cur_sem[e] = self._alloc_sem(f"cnt_{e}")
        self.cnt[e] = 0

    def _need(self, eng, tok, waits):
        if tok is None:
            return
        sem, val, owner = tok
        if self.waited.get((eng, id(sem)), 0) >= val:
            return
        if owner == eng and (eng == "pe" or not self.same_engine_sync):
            return
        old = waits.get(id(sem))
        if old is None or old[1] < val:
            waits[id(sem)] = (sem, val)

    def _deps(self, eng, reads, writes):
        waits = {}
        for r in reads:
            ent = self.res.get(r)
            if ent is not None:
                self._need(eng, ent[0], waits)
        for w in writes:
            ent = self.res.get(w)
            if ent is not None:
                self._need(eng, ent[0], waits)
                for t in ent[1]:
                    self._need(eng, t, waits)
        return waits

    def _emit_waits(self, eng, waits):
        for sid, (sem, val) in waits.items():
            self.waited[(eng, sid)] = val
            self.prog[eng].append(lambda E, sem=sem, val=val: E.wait_ge(sem, val))
            self.n_inst += 1

    def _update(self, tok, reads, writes):
        for r in reads:
            ent = self.res.setdefault(r, [None, []])
            ent[1].append(tok)
            if len(ent[1]) > 48:
                best = {}
                for t in ent[1]:
                    k = id(t[0])
                    if k not in best or best[k][1] < t[1]:
                        best[k] = t
                ent[1] = list(best.values())
        for w in writes:
            self.res[w] = [tok, []]

    def op(self, eng, fn, reads=(), writes=()):
        reads = [r for r in reads if r is not None]
        writes = [w for w in writes if w is not None]
        waits = self._deps(eng, reads, writes)
        self._emit_waits(eng, waits)
        if self.cnt[eng] >= SEM_LIMIT:
            self._new_sem(eng)
        sem = self.cur_sem[eng]
        self.cnt[eng] += 1
        val = self.cnt[eng]
        self.prog[eng].append(lambda E, fn=fn, sem=sem: fn(E).then_inc(sem, 1))
        self.n_inst += 1
        tok = (sem, val, eng)
        self._update(tok, reads, writes)
        return tok

    def dma(self, q, out, in_, reads=(), writes=()):
        reads = [r for r in reads if r is not None]
        writes = [w for w in writes if w is not None]
        waits = self._deps(q, reads, writes)
        slot = self.dma_sems[q][self.dma_rr[q] % N_DMA_SEMS]
        self.dma_rr[q] += 1
        sem, used = slot
        if used > 0 and self.waited.get((q, id(sem)), 0) < used:
            old = waits.get(id(sem))
            if old is None or old[1] < used:
                waits[id(sem)] = (sem, used)
        if SERIAL_DMA and self.last_dma.get(q) is not None:
            ps_, pv_ = self.last_dma[q]
            if self.waited.get((q, id(ps_)), 0) < pv_:
                old = waits.get(id(ps_))
                if old is None or old[1] < pv_:
                    waits[id(ps_)] = (ps_, pv_)
        self._emit_waits(q, waits)
        slot[1] = used + 16
        self.last_dma[q] = (sem, slot[1])
        val = slot[1]
        self.prog[q].append(lambda E, out=out, in_=in_, sem=sem: E.dma_start(out=out, in_=in_).then_inc(sem, 16))
        self.n_inst += 1
        tok = (sem, val, "dma")
        self._update(tok, reads, writes)
        return tok

    def barrier(self):
        for eng in self.ENGS:
            waits = {}
            for other in self.ENGS:
                if other == eng or self.cnt[other] == 0:
                    continue
                sem = self.cur_sem[other]
                if self.waited.get((eng, id(sem)), 0) < self.cnt[other]:
                    waits[id(sem)] = (sem, self.cnt[other])
            for q in self.dma_sems:
                for sem, used in self.dma_sems[q]:
                    if used > 0 and self.waited.get((eng, id(sem)), 0) < used:
                        waits[id(sem)] = (sem, used)
            self._emit_waits(eng, waits)

    def finish(self, names, eng="sp"):
        waits = {}
        for n in names:
            ent = self.res.get(n)
            if ent is not None:
                self._need(eng, ent[0], waits)
        self._emit_waits(eng, waits)

    def run_block(self):
        nc = self.nc
        with nc.Block() as block:
            @block.tensor
            def _(E):
                for f in self.prog["pe"]:
                    f(E)

            @block.scalar
            def _(E):
                for f in self.prog["act"]:
                    f(E)

            @block.vector
            def _(E):
                for f in self.prog["dve"]:
                    f(E)

            @block.gpsimd
            def _(E):
                for f in self.prog["pool"]:
                    f(E)

            @block.sync
            def _(E):
                for f in self.prog["sp"]:
                    f(E)


def rev_ap(ap2d, start, n):
    a = list(ap2d.ap)
    return AP(ap2d.tensor, ap2d.offset + start * a[-1][0], [list(a[0]), [-a[-1][0], n]])


INPUT_SPECS = [
    ("x", [NT, D]), ("cT", [128, 8, NB]), ("ada_w", [4, D, 3 * D]), ("ada_bT", [128, 4, 24]),
    ("norm_gT", [128, 4, 8]), ("final_gT", [128, 8]), ("ident", [128, 128]), ("ones", [128, 128]),
    ("j1", [128, TB]), ("maskF", [128, 128]), ("maskB", [128, 128]), ("triF", [128, 128]), ("triB", [128, 128]),
    ("s5_w_in", [2, D, 2 * D]), ("s5_w_glu", [2, D, D]), ("s5_w_out", [2, D, D]),
    ("s5_b_gluT", [128, 2, 8]), ("s5_dT", [128, 2, 8]),
    ("s5_lre", [128, 2, 2, 32]), ("s5_lim", [128, 2, 2, 32]), ("s5_ldt", [128, 2, 2, 32]),
    ("s5_bre", [2, 2, 128, 32 * 128]), ("s5_bim", [2, 2, 128, 32 * 128]),
    ("s5_cre", [2, 2, 128, 32 * 128]), ("s5_cim", [2, 2, 128, 32 * 128]),
    ("ml_w_in", [2, D, 4 * D]), ("ml_w_out", [2, 2 * D, D]),
    ("ml_convT", [128, 2, 16, 5]), ("ml_convbT", [128, 2, 16]),
    ("ml_wq_bd", [2, 128, 16 * 128]), ("ml_wk_bd", [2, 128, 16 * 128]), ("ml_wv_bd", [2, 128, 16 * 128]),
    ("ml_wg", [2, 128, 3 * 16 * 16]), ("ml_bg_rep", [128, 2, 16]),
    ("ml_gnT", [128, 2, 16]), ("ml_skipT", [128, 2, 16]),
]


def build_program(dbg=(), dbg_stop=()):
    nc = bass.Bass("TRN2", target_bir_lowering=False)
    I = {}
    for name, shape in INPUT_SPECS:
        I[name] = nc.dram_tensor(name, shape, F32, kind="ExternalInput").ap()
    OUT = nc.dram_tensor("out", [NT, D], F32, kind="ExternalOutput").ap()

    def scratch(name, shape, dt):
        kind = "ExternalOutput" if name in dbg else "Internal"
        return nc.dram_tensor(name, shape, dt, kind=kind).ap()

    XT = scratch("XT", [8, 128, NT], F32)
    UT = scratch("UT", [8, 128, NT], BF16)
    SZT = scratch("SZT", [16, 128, NT], BF16)
    GT = scratch("GT", [8, 128, NT], BF16)
    XMT = scratch("XMT", [16, 128, NT], BF16)
    XCT = scratch("XCT", [16, 128, NT], BF16)
    QT = scratch("QT", [16, 128, NT], BF16)
    KT = scratch("KT", [16, 128, NT], BF16)
    KTOK = scratch("KTOK", [NT, 2 * D], BF16)
    VTOK = scratch("VTOK", [NT, 2 * D], BF16)
    HNT = scratch("HNT", [16, 128, NT], BF16)

    fw = FW(nc)
    rr = {"cast": 0, "ev": 0}

    with ExitStack() as top:
        def SB(es, name, shape, dt=F32):
            rr["sb"] = rr.get("sb", 0) + 1
            return es.enter_context(nc.sbuf_tensor(f"sb{rr['sb']}_{name}", shape, dt))

        PS = [top.enter_context(nc.psum_tensor(f"ps{i}", [128, 512], F32)) for i in range(8)]
        PSN = [f"ps{i}" for i in range(8)]

        ident = SB(top, "ident", [128, 128]); ones = SB(top, "ones", [128, 128])
        onesb = SB(top, "onesb", [128, 128], BF16)
        maskF = SB(top, "maskF", [128, 128]); maskB = SB(top, "maskB", [128, 128])
        triF = SB(top, "triF", [128, 128]); triB = SB(top, "triB", [128, 128])
        j1 = SB(top, "j1", [128, TB])
        MOD = SB(top, "MOD", [128, 4, 24, NB])
        S1 = SB(top, "S1", [128, 4, 8, NB])
        ngT = SB(top, "ngT", [128, 4, 8]); fgT = SB(top, "fgT", [128, 8])
        for t, n, rn in ((ident, "ident", "ident"), (ones, "ones", "ones"), (maskF, "maskF", "maskF"), (maskB, "maskB", "maskB"),
                         (triF, "triF", "triF"), (triB, "triB", "triB"), (j1, "j1", "j1"), (ngT, "norm_gT", "ngT"), (fgT, "final_gT", "fgT")):
            fw.dma("sp", t[:], I[n], writes=[rn])
        fw.op("dve", lambda E: E.tensor_copy(onesb[:], ones[:]), ["ones"], ["onesb"])

        def cast_eng():
            rr["cast"] += 1
            return ("dve", "pool", "act")[rr["cast"] % 3]

        def copy_op(eng, out, in_, r, w):
            if eng == "act":
                fw.op("act", lambda E: E.copy(out, in_), r, w)
            else:
                fw.op(eng, lambda E: E.tensor_copy(out, in_), r, w)

        def load_w_bf16(es, dst, dname, src, KC, N, tag):
            CH = min(N, 2048)
            stg = [SB(es, f"stg_{tag}_{i}", [128, CH]) for i in range(2)]
            k = 0
            for kc in range(KC):
                for n0 in range(0, N, CH):
                    s = stg[k % 2]; sn = f"stg_{tag}_{k % 2}"
                    fw.dma("sp", s[:], src[kc * 128:(kc + 1) * 128, n0:n0 + CH], writes=[sn])
                    copy_op(cast_eng(), dst[:, kc, n0:n0 + CH], s[:], [sn], [dname])
                    k += 1

        def phase_mod():
            with ExitStack() as es:
                cT = SB(es, "cT", [128, 8, NB]); sc = SB(es, "sc", [128, 8, NB]); abT = SB(es, "abT", [128, 4, 24])
                wt = [SB(es, f"adaw{i}", [128, 8, 128]) for i in range(2)]
                fw.dma("sp", cT[:], I["cT"], writes=["cT"])
                fw.dma("sp", abT[:], I["ada_bT"], writes=["abT"])
                fw.op("act", lambda E: E.activation(sc[:], cT[:], AF.Silu), ["cT"], ["sc"])
                k = 0
                for i in range(4):
                    for m in range(24):
                        w = wt[k % 2]; wn = f"adaw{k % 2}"
                        src = I["ada_w"][i].rearrange("(kc p) n -> p kc n", p=128)[:, :, m * 128:(m + 1) * 128]
                        fw.dma("sp" if k % 2 == 0 else "act", w[:], src, writes=[wn])
                        for kc in range(8):
                            fw.op("pe", lambda E, w=w, kc=kc: E.matmul(PS[0][:, 0:NB], w[:, kc, :], sc[:, kc, :],
                                                                       start=(kc == 0), stop=(kc == 7)), [wn, "sc"], ["ps0"])
                        fw.op("dve", lambda E, i=i, m=m: E.tensor_scalar(MOD[:, i, m, :], PS[0][:, 0:NB], abT[:, i, m:m + 1], None, ALU.add),
                              ["ps0", "abT"], ["MOD"])
                        k += 1
                for i in range(4):
                    for b in range(NB):
                        fw.op("dve", lambda E, i=i, b=b: E.scalar_tensor_tensor(S1[:, i, :, b], MOD[:, i, 8:16, b], 1.0, ngT[:, i, :], ALU.add, ALU.mult),
                              ["MOD", "ngT"], ["S1"])

        def phase_in():
            with ExitStack() as es:
                xi = [SB(es, f"xi{i}", [128, D]) for i in range(2)]
                xo = [SB(es, f"xo{i}", [128, 8, 128]) for i in range(2)]
                for tt in range(NT // 128):
                    a = xi[tt % 2]; an = f"xi{tt % 2}"; o = xo[tt % 2]; on = f"xo{tt % 2}"
                    fw.dma("sp", a[:], I["x"][tt * 128:(tt + 1) * 128, :], writes=[an])
                    for h in range(2):
                        p = PS[h]; pn = PSN[h]
                        for q in range(4):
                            kc = h * 4 + q
                            fw.op("pe", lambda E, p=p, q=q, kc=kc, a=a: E.transpose(p[:, q * 128:(q + 1) * 128], a[:, kc * 128:(kc + 1) * 128], ident[:]),
                                  [an, "ident"], [pn])
                        copy_op("dve" if h == 0 else "act", o[:, h * 4:(h + 1) * 4, :].rearrange("p a b -> p (a b)"), p[:], [pn], [on])
                    fw.dma("act", XT[:, :, tt * 128:(tt + 1) * 128].rearrange("k p t -> p k t"), o[:], reads=[on], writes=["XT"])

        def norm_block(XB, xbn, HB, hbn, tmps, li, b, final=False):
            sq, rs = tmps
            for kc in range(8):
                fw.op("act", lambda E, kc=kc: E.activation(sq[:, kc % 2, :], XB[:, kc, :], AF.Square), [xbn], [f"sq{kc % 2}"])
                fw.op("pe", lambda E, kc=kc: E.matmul(PS[7][:], ones[:], sq[:, kc % 2, :], start=(kc == 0), stop=(kc == 7)),
                      [f"sq{kc % 2}", "ones"], ["ps7"])
            fw.op("act", lambda E: E.activation(rs[:], PS[7][:], AF.Sqrt, bias=1e-6, scale=1.0 / D), ["ps7"], ["rs"])
            fw.op("dve", lambda E: E.reciprocal(rs[:], rs[:]), ["rs"], ["rs"])
            for kc in range(8):
                eng = "pool"
                if final:
                    fw.op("dve", lambda E, kc=kc: E.scalar_tensor_tensor(HB[:, kc, :], XB[:, kc, :], fgT[:, kc:kc + 1], rs[:], ALU.mult, ALU.mult),
                          [xbn, "rs", "fgT"], [hbn])
                else:
                    fw.op("dve", lambda E, kc=kc: E.scalar_tensor_tensor(sq[:, 2 + kc % 2, :], XB[:, kc, :], S1[:, li, kc, b:b + 1], rs[:], ALU.mult, ALU.mult),
                          [xbn, "rs", "S1"], [f"sq{2 + kc % 2}"])
                    fw.op(eng, lambda E, kc=kc: E.tensor_scalar(HB[:, kc, :], sq[:, 2 + kc % 2, :], MOD[:, li, kc, b:b + 1], None, ALU.add),
                          [f"sq{2 + kc % 2}", "MOD"], [hbn])

        def phase_inproj(li, w_src, NOUT, dst_a, dst_b):
            NM = NOUT // 128
            with ExitStack() as es:
                W = SB(es, "Win", [128, 8, NOUT], BF16)
                load_w_bf16(es, W, "Win", w_src, 8, NOUT, "win")
                XBs = [SB(es, f"XB{i}", [128, 8, TB]) for i in range(2)]
                HB = SB(es, "HB", [128, 8, TB], BF16)
                sq = SB(es, "sq", [128, 4, TB]); rs = SB(es, "rs", [128, TB])
                ob = [SB(es, f"ob{i}", [128, TB], BF16) for i in range(4)]
                fw.dma("sp", XBs[0][:], XT[:, :, 0:TB].rearrange("k p t -> p k t"), reads=["XT"], writes=["XB0"])
                for tb in range(NTB):
                    XB = XBs[tb % 2]; xbn = f"XB{tb % 2}"; b = tb // (NTB // NB)
                    if tb + 1 < NTB:
                        fw.dma("sp", XBs[(tb + 1) % 2][:], XT[:, :, (tb + 1) * TB:(tb + 2) * TB].rearrange("k p t -> p k t"),
                               reads=["XT"], writes=[f"XB{(tb + 1) % 2}"])
                    norm_block(XB, xbn, HB, "HB", (sq, rs), li, b)
                    for mt in range(NM):
                        p = PS[mt % 4]; pn = PSN[mt % 4]
                        for kc in range(8):
                            fw.op("pe", lambda E, p=p, kc=kc, mt=mt: E.matmul(p[:], W[:, kc, mt * 128:(mt + 1) * 128], HB[:, kc, :],
                                                                              start=(kc == 0), stop=(kc == 7)), ["Win", "HB"], [pn])
                        o = ob[mt % 4]; on = f"ob{mt % 4}"
                        if mt < NM // 2:
                            copy_op("dve" if mt % 2 == 0 else "act", o[:], p[:], [pn], [on])
                            fw.dma("act", dst_a[mt][:, tb * TB:(tb + 1) * TB], o[:], reads=[on], writes=["dst_a"])
                        else:
                            fw.op("act", lambda E, o=o, p=p: E.activation(o[:], p[:], AF.Silu), [pn], [on])
                            fw.dma("act", dst_b[mt - NM // 2][:, tb * TB:(tb + 1) * TB], o[:], reads=[on], writes=["dst_b"])

        YT = scratch("YT", [8, 128, NT], F32)

        def phase_s5_ssm(j):
            with ExitStack() as es:
                lre = SB(es, "lre", [128, 2, 32]); lim = SB(es, "lim", [128, 2, 32]); ldt = SB(es, "ldt", [128, 2, 32])
                fw.dma("sp", lre[:], I["s5_lre"][:, j], writes=["lre"])
                fw.dma("sp", lim[:], I["s5_lim"][:, j], writes=["lim"])
                fw.dma("sp", ldt[:], I["s5_ldt"][:, j], writes=["ldt"])
                dT = SB(es, "dT", [128, 8])
                fw.dma("sp", dT[:], I["s5_dT"][:, j], writes=["dT"])
                tn = ["dt", "th", "rmag", "ar", "ai", "k1", "k2", "k3", "den", "zre", "zim", "nzim", "t1s", "t2s"]
                T = {n: SB(es, "s5t_" + n, [128, 2, 32]) for n in tn}
                ki = SB(es, "s5t_ki", [128, 2, 32], I32)

                def sm(eng, fn, r, w):
                    fw.op(eng, fn, ["s5t_" + x if x in T else x for x in r], ["s5t_" + x if x in T else x for x in w])

                sm("act", lambda E: E.activation(T["dt"][:], ldt[:], AF.Exp), ["ldt"], ["dt"])
                sm("dve", lambda E: E.tensor_tensor(T["th"][:], lim[:], T["dt"][:], ALU.mult), ["lim", "dt"], ["th"])
                sm("dve", lambda E: E.tensor_tensor(T["k1"][:], lre[:], T["dt"][:], ALU.mult), ["lre", "dt"], ["k1"])
                sm("act", lambda E: E.activation(T["rmag"][:], T["k1"][:], AF.Exp), ["k1"], ["rmag"])
                for dst, sh in (("ai", 0.0), ("ar", float(np.pi / 2))):
                    sm("dve", lambda E, sh=sh: E.tensor_scalar(T["k2"][:], T["th"][:], sh, None, ALU.add), ["th"], ["k2"])
                    sm("dve", lambda E: E.tensor_scalar(ki[:], T["k2"][:], 1.0 / TWO_PI, None, ALU.mult), ["k2"], ["s5t_ki"])
                    sm("dve", lambda E: E.tensor_copy(T["k3"][:], ki[:]), ["s5t_ki"], ["k3"])
                    sm("dve", lambda E: E.scalar_tensor_tensor(T["k2"][:], T["k3"][:], -TWO_PI, T["k2"][:], ALU.mult, ALU.add), ["k3", "k2"], ["k2"])
                    sm("act", lambda E, dst=dst: E.activation(T[dst][:], T["k2"][:], AF.Sin), ["k2"], [dst])
                sm("dve", lambda E: E.tensor_tensor(T["ar"][:], T["ar"][:], T["rmag"][:], ALU.mult), ["ar", "rmag"], ["ar"])
                sm("dve", lambda E: E.tensor_tensor(T["ai"][:], T["ai"][:], T["rmag"][:], ALU.mult), ["ai", "rmag"], ["ai"])
                sm("dve", lambda E: E.tensor_scalar(T["k1"][:], T["ar"][:], -1.0, None, ALU.add), ["ar"], ["k1"])
                sm("dve", lambda E: E.tensor_tensor(T["den"][:], lre[:], lre[:], ALU.mult), ["lre"], ["den"])
                sm("dve", lambda E: E.tensor_tensor(T["k2"][:], lim[:], lim[:], ALU.mult), ["lim"], ["k2"])
                sm("dve", lambda E: E.tensor_tensor(T["den"][:], T["den"][:], T["k2"][:], ALU.add), ["den", "k2"], ["den"])
                sm("dve", lambda E: E.reciprocal(T["den"][:], T["den"][:]), ["den"], ["den"])
                sm("dve", lambda E: E.tensor_tensor(T["t1s"][:], T["k1"][:], lre[:], ALU.mult), ["k1", "lre"], ["t1s"])
                sm("dve", lambda E: E.tensor_tensor(T["t2s"][:], T["ai"][:], lim[:], ALU.mult), ["ai", "lim"], ["t2s"])
                sm("dve", lambda E: E.tensor_tensor(T["zre"][:], T["t1s"][:], T["t2s"][:], ALU.add), ["t1s", "t2s"], ["zre"])
                sm("dve", lambda E: E.tensor_tensor(T["zre"][:], T["zre"][:], T["den"][:], ALU.mult), ["zre", "den"], ["zre"])
                sm("dve", lambda E: E.tensor_tensor(T["t1s"][:], T["ai"][:], lre[:], ALU.mult), ["ai", "lre"], ["t1s"])
                sm("dve", lambda E: E.tensor_tensor(T["t2s"][:], T["k1"][:], lim[:], ALU.mult), ["k1", "lim"], ["t2s"])
                sm("dve", lambda E: E.tensor_tensor(T["zim"][:], T["t1s"][:], T["t2s"][:], ALU.subtract), ["t1s", "t2s"], ["zim"])
                sm("dve", lambda E: E.tensor_tensor(T["zim"][:], T["zim"][:], T["den"][:], ALU.mult), ["zim", "den"], ["zim"])
                sm("dve", lambda E: E.tensor_scalar(T["nzim"][:], T["zim"][:], -1.0, None, ALU.mult), ["zim"], ["nzim"])

                B4 = SB(es, "B4", [128, 2, 4 * 128], BF16)
                C4 = SB(es, "C4", [128, 2, 4 * 128], BF16)
                stg = [SB(es, f"s5stg{i}", [128, 4 * 128]) for i in range(4)]
                tC = SB(es, "tC", [128, 128])
                cosT = SB(es, "cosT", [128, 4, TB]); sinT = SB(es, "sinT", [128, 4, TB]); RM = SB(es, "RM", [128, 4, TB])
                ph = SB(es, "ph", [128, TB]); phk = SB(es, "phk", [128, TB]); phi = SB(es, "phi", [128, TB], I32)
                Us = [SB(es, f"Uc{i}", [128, NT], BF16) for i in range(2)]
                YACC = SB(es, "YACC", [128, NT])
                Gc = SB(es, "Gc", [128, NT], BF16)
                tmps = [{n: SB(es, f"w{q}_{n}", [128, TB]) for n in ("t1", "t2", "t3", "t4", "pre", "pim", "sre", "sim")} for q in range(2)]
                u2 = [SB(es, f"u2_{q}", [128, 2, TB]) for q in range(2)]
                sbf = [SB(es, f"sbf{q}", [128, 2, TB], BF16) for q in range(2)]
                carry = SB(es, "carry", [128, 4, NB, 4])
                it = 0
                for d in range(2):
                    for cc in range(8):
                        U = Us[it % 2]; un = f"Uc{it % 2}"
                        fw.dma("sp", U[:], UT[cc], reads=["dst_a"], writes=[un])
                        for q, key in enumerate(("s5_bre", "s5_bim", "s5_cre", "s5_cim")):
                            fw.dma("sp", stg[q][:], I[key][j, d][:, cc * 512:(cc + 1) * 512], writes=[f"s5stg{q}"])
                        fw.op("act", lambda E: E.copy(B4[:, 0, :], stg[0][:]), ["s5stg0"], ["B4"])
                        fw.op("act", lambda E: E.copy(B4[:, 1, :], stg[1][:]), ["s5stg1"], ["B4"])
                        for pi in range(4):
                            st = 4 * cc + pi
                            sl = slice(pi * 128, (pi + 1) * 128)
                            zr = T["zre"][:, d, st:st + 1]; zi = T["zim"][:, d, st:st + 1]; nzi = T["nzim"][:, d, st:st + 1]
                            fw.op("dve", lambda E, sl=sl, zi=zi: E.tensor_scalar(tC[:], stg[3][:, sl], zi, None, ALU.mult), ["s5stg3", "s5t_zim"], ["tC"])
                            fw.op("dve", lambda E, sl=sl, zr=zr: E.scalar_tensor_tensor(C4[:, 0, sl], stg[2][:, sl], zr, tC[:], ALU.mult, ALU.subtract),
                                  ["s5stg2", "s5t_zre", "tC"], ["C4"])
                            fw.op("dve", lambda E, sl=sl, zr=zr: E.tensor_scalar(tC[:], stg[3][:, sl], zr, None, ALU.mult), ["s5stg3", "s5t_zre"], ["tC"])
                            fw.op("dve", lambda E, sl=sl, nzi=nzi: E.scalar_tensor_tensor(C4[:, 1, sl], stg[2][:, sl], nzi, tC[:], ALU.mult, ALU.subtract),
                                  ["s5stg2", "s5t_nzim", "tC"], ["C4"])
                            th = T["th"][:, d, st:st + 1]
                            fw.op("pool", lambda E, th=th: E.tensor_scalar(ph[:], j1[:], th, None, ALU.mult), ["j1", "s5t_th"], ["ph"])
                            for dstT, dn, sh in ((sinT, "sinT", 0.0), (cosT, "cosT", float(np.pi / 2))):
                                fw.op("pool", lambda E, sh=sh: E.tensor_scalar(phk[:], ph[:], sh, None, ALU.add), ["ph"], ["phk"])
                                fw.op("pool", lambda E: E.tensor_scalar(phi[:], phk[:], 1.0 / TWO_PI, None, ALU.mult), ["phk"], ["phi"])
                                fw.op("pool", lambda E: E.tensor_copy(ph[:] if False else tmps[0]["t1"][:], phi[:]), ["phi"], ["w0_t1"])
                                fw.op("dve", lambda E: E.scalar_tensor_tensor(phk[:], tmps[0]["t1"][:], -TWO_PI, phk[:], ALU.mult, ALU.add), ["w0_t1", "phk"], ["phk"])
                                fw.op("act", lambda E, dstT=dstT, pi=pi: E.activation(dstT[:, pi, :], phk[:], AF.Sin), ["phk"], [dn])
                            rm = T["rmag"][:, d, st:st + 1]
                            fw.op("pool", lambda E, pi=pi, rm=rm: E.tensor_scalar(RM[:, pi, :], j1[:], 0.0, rm, ALU.mult, ALU.add), ["j1", "s5t_rmag"], ["RM"])
                        fw.op("pool", lambda E: E.memset(carry[:], 0.0), [], ["carry"])
                        if d == 0:
                            fw.op("pool", lambda E, U=U, cc=cc: E.tensor_scalar(YACC[:], U[:], dT[:, cc:cc + 1], None, ALU.mult), [un, "dT"], ["YACC"])
                        else:
                            fw.dma("sp", YACC[:], YT[cc], reads=["YT"], writes=["YACC"])
                        k = 0
                        for b in range(NB):
                            for chi in range(4):
                                ch = chi if d == 0 else 3 - chi
                                T0 = b * L + ch * TB
                                if d == 0:
                                    rhs = U[:, T0:T0 + TB]; yv = YACC[:, T0:T0 + TB]
                                else:
                                    rhs = rev_ap(U[:], T0 + TB - 1, TB); yv = rev_ap(YACC[:], T0 + TB - 1, TB)
                                py = PS[4 + (k // 4) % 2]; pyn = PSN[4 + (k // 4) % 2]
                                for pi in range(4):
                                    q = k % 2
                                    W = tmps[q]; wn = lambda n, q=q: f"w{q}_{n}"
                                    pa = PS[2 * q]; pan = PSN[2 * q]; pb = PS[2 * q + 1]; pbn = PSN[2 * q + 1]
                                    sl = slice(pi * 128, (pi + 1) * 128)
                                    fw.op("pe", lambda E, pa=pa, sl=sl, rhs=rhs: E.matmul(pa[:], B4[:, 0, sl], rhs, start=True, stop=True), ["B4", un], [pan])
                                    fw.op("pe", lambda E, pb=pb, sl=sl, rhs=rhs: E.matmul(pb[:], B4[:, 1, sl], rhs, start=True, stop=True), ["B4", un], [pbn])
                                    cs = cosT[:, pi, :]; sn = sinT[:, pi, :]
                                    fw.op("dve", lambda E, W=W, pa=pa, cs=cs: E.tensor_tensor(W["t1"][:], pa[:], cs, ALU.mult), [pan, "cosT"], [wn("t1")])
                                    fw.op("dve", lambda E, W=W, pb=pb, sn=sn: E.tensor_tensor(W["t2"][:], pb[:], sn, ALU.mult), [pbn, "sinT"], [wn("t2")])
                                    fw.op("dve", lambda E, W=W, pb=pb, cs=cs: E.tensor_tensor(W["t3"][:], pb[:], cs, ALU.mult), [pbn, "cosT"], [wn("t3")])
                                    fw.op("dve", lambda E, W=W, pa=pa, sn=sn: E.tensor_tensor(W["t4"][:], pa[:], sn, ALU.mult), [pan, "sinT"], [wn("t4")])
                                    fw.op("pool", lambda E, W=W: E.tensor_tensor(W["pre"][:], W["t1"][:], W["t2"][:], ALU.add), [wn("t1"), wn("t2")], [wn("pre")])
                                    fw.op("pool", lambda E, W=W: E.tensor_tensor(W["pim"][:], W["t3"][:], W["t4"][:], ALU.subtract), [wn("t3"), wn("t4")], [wn("pim")])
                                    fw.op("dve", lambda E, W=W, pi=pi, b=b: E.tensor_tensor_scan(W["sre"][:], RM[:, pi, :], W["pre"][:], carry[:, pi, b, 0:1], ALU.mult, ALU.add),
                                          ["RM", wn("pre"), "carry"], [wn("sre")])
                                    fw.op("dve", lambda E, W=W, pi=pi, b=b: E.tensor_tensor_scan(W["sim"][:], RM[:, pi, :], W["pim"][:], carry[:, pi, b, 1:2], ALU.mult, ALU.add),
                                          ["RM", wn("pim"), "carry"], [wn("sim")])
                                    fw.op("pool", lambda E, W=W, cs=cs: E.tensor_tensor(W["t1"][:], W["sre"][:], cs, ALU.mult), [wn("sre"), "cosT"], [wn("t1")])
                                    fw.op("pool", lambda E, W=W, sn=sn: E.tensor_tensor(W["t2"][:], W["sim"][:], sn, ALU.mult), [wn("sim"), "sinT"], [wn("t2")])
                                    fw.op("pool", lambda E, W=W, sn=sn: E.tensor_tensor(W["t3"][:], W["sre"][:], sn, ALU.mult), [wn("sre"), "sinT"], [wn("t3")])
                                    fw.op("pool", lambda E, W=W, cs=cs: E.tensor_tensor(W["t4"][:], W["sim"][:], cs, ALU.mult), [wn("sim"), "cosT"], [wn("t4")])
                                    uu = u2[q]; uun = f"u2_{q}"
                                    fw.op("dve", lambda E, W=W, uu=uu: E.tensor_tensor(uu[:, 0, :], W["t1"][:], W["t2"][:], ALU.subtract), [wn("t1"), wn("t2")], [uun])
                                    fw.op("dve", lambda E, W=W, uu=uu: E.tensor_tensor(uu[:, 1, :], W["t3"][:], W["t4"][:], ALU.add), [wn("t3"), wn("t4")], [uun])
                                    sb_ = sbf[q]; sbn = f"sbf{q}"
                                    fw.op("act", lambda E, sb_=sb_, uu=uu: E.copy(sb_[:], uu[:]), [uun], [sbn])
                                    fw.op("pool", lambda E, uu=uu, pi=pi, b=b: E.tensor_copy(carry[:, pi, b, 0:2], uu[:, :, TB - 1]), [uun], ["carry"])
                                    fw.op("pe", lambda E, py=py, sl=sl, sb_=sb_, pi=pi: E.matmul(py[:], C4[:, 0, sl], sb_[:, 0, :], start=(pi == 0), stop=False), ["C4", sbn], [pyn])
                                    fw.op("pe", lambda E, py=py, sl=sl, sb_=sb_, pi=pi: E.matmul(py[:], C4[:, 1, sl], sb_[:, 1, :], start=False, stop=(pi == 3)), ["C4", sbn], [pyn])
                                    k += 1
                                fw.op("dve", lambda E, py=py, yv=yv: E.tensor_tensor(yv, py[:], yv, ALU.add), [pyn, "YACC"], ["YACC"])
                                if "CARRYD" in dbg and d == 0 and cc == 0:
                                    CD2 = nc.dram_tensor(f"CARRYD_b{b}c{chi}", [128, 32], F32, kind="ExternalOutput").ap()
                                    fw.dma("sp", CD2, carry[:].rearrange("p a b c -> p (a b c)"), reads=["carry"], writes=["CARRYD"])
                                    if b == 1 and chi == 1:
                                        UD3 = nc.dram_tensor("CARRYD_U", [128, NT], BF16, kind="ExternalOutput").ap()
                                        fw.dma("sp", UD3, U[:], reads=[un], writes=["CARRYD"])
                                        for nm in ("t1", "pre", "sre"):
                                            UD4 = nc.dram_tensor("CARRYD_" + nm, [128, TB], F32, kind="ExternalOutput").ap()
                                            fw.dma("sp", UD4, tmps[(k - 1) % 2][nm][:], reads=[f"w{(k - 1) % 2}_{nm}"], writes=["CARRYD"])
                                        UD5 = nc.dram_tensor("CARRYD_cos", [128, 4 * TB], F32, kind="ExternalOutput").ap()
                                        fw.dma("sp", UD5, cosT[:].rearrange("p a b -> p (a b)"), reads=["cosT"], writes=["CARRYD"])
                                        UD6 = nc.dram_tensor("CARRYD_RM", [128, 4 * TB], F32, kind="ExternalOutput").ap()
                                        fw.dma("sp", UD6, RM[:].rearrange("p a b -> p (a b)"), reads=["RM"], writes=["CARRYD"])
                                    UD2 = nc.dram_tensor(f"CARRYDU_b{b}c{chi}", [128, 2 * TB], F32, kind="ExternalOutput").ap()
                                    fw.dma("sp", UD2, u2[(k - 1) % 2][:].rearrange("p a b -> p (a b)"), reads=[f"u2_{(k - 1) % 2}"], writes=["CARRYD"])
                        if "CARRYD" in dbg and d == 0 and cc < 2:
                            CD = nc.dram_tensor(f"CARRYD{cc}", [128, 32], F32, kind="ExternalOutput").ap()
                            fw.dma("sp", CD, carry[:].rearrange("p a b c -> p (a b c)"), reads=["carry"], writes=["CARRYD"])
                        if d == 0:
                            fw.dma("sp", YT[cc], YACC[:], reads=["YACC"], writes=["YT"])
                        else:
                            fw.op("act", lambda E: E.activation(Gc[:], YACC[:], AF.Gelu), ["YACC"], ["Gc"])
                            fw.dma("act", GT[cc], Gc[:], reads=["Gc"], writes=["GT"])
                        it += 1

        def phase_s5_out(j, li):
            with ExitStack() as es:
                Wg = SB(es, "Wg", [128, 8, D], BF16); Wo = SB(es, "Wo", [128, 8, D], BF16)
                load_w_bf16(es, Wg, "Wg", I["s5_w_glu"][j], 8, D, "wg")
                load_w_bf16(es, Wo, "Wo", I["s5_w_out"][j], 8, D, "wo")
                bg = SB(es, "bg", [128, 8])
                fw.dma("sp", bg[:], I["s5_b_gluT"][:, j], writes=["bg"])
                G = SB(es, "Gb", [128, 8, TB], BF16); SZ = SB(es, "SZb", [128, 8, TB], BF16); XB = SB(es, "XBo", [128, 8, TB])
                Y2 = SB(es, "Y2", [128, 8, TB], BF16)
                sg = [SB(es, f"sg{i}", [128, TB]) for i in range(2)]
                for tb in range(NTB):
                    b = tb // (NTB // NB); ts_ = slice(tb * TB, (tb + 1) * TB)
                    fw.dma("sp", G[:], GT[:, :, ts_].rearrange("k p t -> p k t"), reads=["GT"], writes=["Gb"])
                    fw.dma("sp", SZ[:], SZT[0:8, :, ts_].rearrange("k p t -> p k t"), reads=["dst_b"], writes=["SZb"])
                    fw.dma("sp", XB[:], XT[:, :, ts_].rearrange("k p t -> p k t"), reads=["XT"], writes=["XBo"])
                    for mt in range(8):
                        p = PS[mt % 4]; pn = PSN[mt % 4]; s_ = sg[mt % 2]; sn = f"sg{mt % 2}"
                        for kc in range(8):
                            fw.op("pe", lambda E, p=p, kc=kc, mt=mt: E.matmul(p[:], Wg[:, kc, mt * 128:(mt + 1) * 128], G[:, kc, :], start=(kc == 0), stop=(kc == 7)),
                                  ["Wg", "Gb"], [pn])
                        fw.op("act", lambda E, p=p, s_=s_, mt=mt: E.activation(s_[:], p[:], AF.Sigmoid, bias=bg[:, mt:mt + 1]), [pn, "bg"], [sn])
                        fw.op("dve", lambda E, s_=s_, mt=mt: E.tensor_tensor(s_[:], s_[:], G[:, mt, :], ALU.mult), [sn, "Gb"], [sn])
                        fw.op("pool", lambda E, s_=s_, mt=mt: E.tensor_tensor(Y2[:, mt, :], s_[:], SZ[:, mt, :], ALU.mult), [sn, "SZb"], ["Y2"])
                    for mt in range(8):
                        p = PS[4 + mt % 4]; pn = PSN[4 + mt % 4]
                        for kc in range(8):
                            fw.op("pe", lambda E, p=p, kc=kc, mt=mt: E.matmul(p[:], Wo[:, kc, mt * 128:(mt + 1) * 128], Y2[:, kc, :], start=(kc == 0), stop=(kc == 7)),
                                  ["Wo", "Y2"], [pn])
                        fw.op("dve", lambda E, p=p, mt=mt, b=b: E.scalar_tensor_tensor(XB[:, mt, :], p[:], MOD[:, li, 16 + mt, b:b + 1], XB[:, mt, :], ALU.mult, ALU.add),
                              [pn, "MOD", "XBo"], ["XBo"])
                    fw.dma("act", XT[:, :, ts_].rearrange("k p t -> p k t"), XB[:], reads=["XBo"], writes=["XT"])

        def phase_ml_qkv(j, GATES):
            with ExitStack() as es:
                cw = SB(es, "cw", [128, 16, 5]); cb = SB(es, "cb", [128, 16]); bgr = SB(es, "bgr", [128, 16])
                fw.dma("sp", cw[:], I["ml_convT"][:, j], writes=["cw"])
                fw.dma("sp", cb[:], I["ml_convbT"][:, j], writes=["cb"])
                fw.dma("sp", bgr[:], I["ml_bg_rep"][:, j], writes=["bgr"])
                Wq = SB(es, "Wq", [128, 1, 2048], BF16); Wk = SB(es, "Wk", [128, 1, 2048], BF16); Wv = SB(es, "Wv", [128, 1, 2048], BF16)
                load_w_bf16(es, Wq, "Wq", I["ml_wq_bd"][j], 1, 2048, "wq")
                load_w_bf16(es, Wk, "Wk", I["ml_wk_bd"][j], 1, 2048, "wk")
                load_w_bf16(es, Wv, "Wv", I["ml_wv_bd"][j], 1, 2048, "wv")
                wgs = SB(es, "wgs", [128, 768]); wg = SB(es, "wgb", [128, 768], BF16)
                fw.dma("sp", wgs[:], I["ml_wg"][j], writes=["wgs"])
                fw.op("dve", lambda E: E.tensor_copy(wg[:], wgs[:]), ["wgs"], ["wgb"])
                XMH = [SB(es, f"XMH{i}", [128, L + 4], BF16) for i in range(2)]
                for i in range(2):
                    fw.op("pool", lambda E, i=i: E.memset(XMH[i][:], 0.0), [], [f"XMH{i}"])
                acc = SB(es, "cacc", [128, L]); XC = SB(es, "XCc", [128, L], BF16)
                QTB = SB(es, "QTB", [128, L], BF16); KTB = SB(es, "KTB", [128, L], BF16); VTB = SB(es, "VTB", [128, L], BF16)
                KTK = SB(es, "KTK", [128, 16, 128], BF16); VTK = SB(es, "VTK", [128, 16, 128], BF16)
                it = 0
                for cc in range(16):
                    csl = slice(cc * 128, (cc + 1) * 128)
                    for b in range(NB):
                        xm = XMH[it % 2]; xn = f"XMH{it % 2}"; bs = slice(b * L, (b + 1) * L)
                        fw.dma("sp", xm[:, 2:2 + L], XMT[cc][:, bs], reads=["dst_a"], writes=[xn])
                        ce = "dve"
                        fw.op(ce, lambda E, xm=xm, cc=cc: E.tensor_scalar(acc[:], xm[:, 0:L], cw[:, cc, 0:1], cb[:, cc:cc + 1], ALU.mult, ALU.add), [xn, "cw", "cb"], ["cacc"])
                        for kk in range(1, 5):
                            fw.op(ce, lambda E, xm=xm, cc=cc, kk=kk: E.scalar_tensor_tensor(acc[:], xm[:, kk:kk + L], cw[:, cc, kk:kk + 1], acc[:], ALU.mult, ALU.add),
                                  [xn, "cw", "cacc"], ["cacc"])
                        fw.op("act", lambda E: E.activation(XC[:], acc[:], AF.Silu), ["cacc"], ["XCc"])
                        fw.dma("act", XCT[cc][:, bs], XC[:], reads=["XCc"], writes=["XCT"])
                        for q4 in range(4):
                            qs = slice(q4 * TB, (q4 + 1) * TB)
                            for wi, (Wm, wn, src, srn, dst, dn) in enumerate(((Wq, "Wq", XC[:, qs], "XCc", QTB, "QTB"), (Wk, "Wk", XC[:, qs], "XCc", KTB, "KTB"),
                                                                           (Wv, "Wv", xm[:, 2 + q4 * TB:2 + (q4 + 1) * TB], xn, VTB, "VTB"))):
                                p = PS[wi]; pn = PSN[wi]
                                fw.op("pe", lambda E, p=p, Wm=Wm, src=src, csl=csl: E.matmul(p[:], Wm[:, 0, csl], src, start=True, stop=True), [wn, srn], [pn])
                                copy_op("dve" if wi != 1 else "act", dst[:, qs], p[:], [pn], [dn])
                        fw.dma("act", QT[cc][:, bs], QTB[:], reads=["QTB"], writes=["QT"])
                        fw.dma("act", KT[cc][:, bs], KTB[:], reads=["KTB"], writes=["KT"])
                        for t4 in range(4):
                            pk = PS[3]; pv = PS[4]
                            for tq in range(4):
                                tt = t4 * 4 + tq; tsl = slice(tt * 128, (tt + 1) * 128); osl = slice(tq * 128, (tq + 1) * 128)
                                fw.op("pe", lambda E, tsl=tsl, osl=osl, csl=csl: E.matmul(pk[:, osl], XC[:, tsl], Wk[:, 0, csl], start=True, stop=True), ["XCc", "Wk"], ["ps3"])
                                fw.op("pe", lambda E, tsl=tsl, osl=osl, xm=xm, tt=tt, csl=csl: E.matmul(pv[:, osl], xm[:, 2 + tt * 128:2 + (tt + 1) * 128], Wv[:, 0, csl], start=True, stop=True),
                                      [xn, "Wv"], ["ps4"])
                                gsl = slice((b * 16 + tt) * 16, (b * 16 + tt + 1) * 16)
                                for wi, (src, srn) in enumerate(((QTB, "QTB"), (KTB, "KTB"), (VTB, "VTB"))):
                                    fw.op("pe", lambda E, src=src, tsl=tsl, gsl=gsl, wi=wi, cc=cc, b=b, tt=tt: E.matmul(
                                        PS[6][:, gsl], src[:, tsl], wg[:, (wi * 16 + cc) * 16:(wi * 16 + cc + 1) * 16],
                                        start=(cc == 0 and wi == 0 and b == 0 and tt == 0), stop=(cc == 15 and wi == 2)), [srn, "wgb"], ["ps6"])
                            copy_op("dve", KTK[:, t4 * 4:(t4 + 1) * 4, :].rearrange("p a b -> p (a b)"), pk[:], ["ps3"], ["KTK"])
                            copy_op("act", VTK[:, t4 * 4:(t4 + 1) * 4, :].rearrange("p a b -> p (a b)"), pv[:], ["ps4"], ["VTK"])
                        fw.dma("act", KTOK[bs, csl].rearrange("(t p) c -> p t c", p=128), KTK[:], reads=["KTK"], writes=["KTOK"])
                        fw.dma("act", VTOK[bs, csl].rearrange("(t p) c -> p t c", p=128), VTK[:], reads=["VTK"], writes=["VTOK"])
                        it += 1
                fw.op("dve", lambda E: E.tensor_tensor(GATES[:], PS[6][:].rearrange("p (a b) -> p a b", b=16),
                                                       bgr[:].unsqueeze(1).to_broadcast([128, 32, 16]), ALU.add), ["ps6", "bgr"], ["GATES"])

        def phase_ml_attn(j, GATES):
            SC = float(512 ** -0.5)
            with ExitStack() as es:
                def G3(name, last=16, dt=F32):
                    return SB(es, name, [128, 32, last], dt)
                E1 = G3("E1"); LF = G3("LF"); BWF = G3("BWF"); BWB = G3("BWB"); TOT = G3("TOT"); OFF = G3("OFF")
                fw.op("act", lambda E: E.activation(E1[:], GATES[:], AF.Exp, scale=-1.0), ["GATES"], ["E1"])
                fw.op("act", lambda E: E.activation(E1[:], E1[:], AF.Ln, bias=1.0), ["E1"], ["E1"])
                fw.op("dve", lambda E: E.tensor_scalar(LF[:], E1[:], -1.0, None, ALU.mult), ["E1"], ["LF"])
                LFf = LF[:].rearrange("p a b -> p (a b)")
                fw.op("pe", lambda E: E.matmul(PS[0][:], triF[:], LFf, start=True, stop=True), ["triF", "LF"], ["ps0"])
                fw.op("pe", lambda E: E.matmul(PS[1][:], triB[:], LFf, start=True, stop=True), ["triB", "LF"], ["ps1"])
                fw.op("pe", lambda E: E.matmul(PS[2][:], ones[:], LFf, start=True, stop=True), ["ones", "LF"], ["ps2"])
                fw.op("dve", lambda E: E.tensor_copy(BWF[:].rearrange("p a b -> p (a b)"), PS[0][:]), ["ps0"], ["BWF"])
                fw.op("act", lambda E: E.copy(BWB[:].rearrange("p a b -> p (a b)"), PS[1][:]), ["ps1"], ["BWB"])
                fw.op("dve", lambda E: E.tensor_copy(TOT[:].rearrange("p a b -> p (a b)"), PS[2][:]), ["ps2"], ["TOT"])
                for (BW, bwn, fwd) in ((BWF, "BWF", True), (BWB, "BWB", False)):
                    fw.op("pool", lambda E: E.memset(OFF[:], 0.0), [], ["OFF"])
                    for b in range(NB):
                        rng = range(1, 16) if fwd else range(14, -1, -1)
                        for kt in rng:
                            g = b * 16 + kt; gp = g - 1 if fwd else g + 1
                            fw.op("pool", lambda E, g=g, gp=gp: E.tensor_tensor(OFF[:, g, :], OFF[:, gp, :], TOT[:, gp, :], ALU.add), ["OFF", "TOT"], ["OFF"])
                    fw.op("pool", lambda E, BW=BW: E.tensor_tensor(BW[:], BW[:], OFF[:], ALU.add), [bwn, "OFF"], [bwn])
                AALL = SB(es, "AALL", [128, 2, 32, 4]); CM = SB(es, "CM", [128, 2, 32, 4]); TM = SB(es, "TM", [128, 2, 32, 4])
                PM = SB(es, "PM", [128, 2, 32, 4]); MM = SB(es, "MM", [128, 2, 32, 4]); EM = SB(es, "EM", [128, 2, 32, 4])
                fw.op("dve", lambda E: E.tensor_tensor(AALL[:, 0], GATES[:, :, 0:4], BWF[:, :, 4:8], ALU.subtract), ["GATES", "BWF"], ["AALL"])
                fw.op("dve", lambda E: E.tensor_tensor(AALL[:, 1], GATES[:, :, 8:12], BWB[:, :, 12:16], ALU.subtract), ["GATES", "BWB"], ["AALL"])
                DG = [SB(es, f"DG{i}", [128, 4, 128]) for i in range(2)]
                AM = [SB(es, f"AM{i}", [128, 4, 128]) for i in range(2)]
                identb = ident[:].unsqueeze(1).to_broadcast([128, 4, 128])
                k = 0
                for d in range(2):
                    mk = maskF if d == 0 else maskB
                    mkn = "maskF" if d == 0 else "maskB"
                    for g in range(32):
                        q = k % 2; dg = DG[q]; dgn = f"DG{q}"; am = AM[q]; amn = f"AM{q}"; p = PS[q]; pn = PSN[q]
                        fw.op("pool", lambda E, dg=dg, d=d, g=g: E.tensor_tensor(dg[:], identb, AALL[:, d, g, :].unsqueeze(2).to_broadcast([128, 4, 128]), ALU.mult),
                              ["ident", "AALL"], [dgn])
                        fw.op("pe", lambda E, p=p, dg=dg: E.matmul(p[:], ones[:], dg[:].rearrange("p a b -> p (a b)"), start=True, stop=True), ["ones", dgn], [pn])
                        p3 = p[:].rearrange("p (a b) -> p a b", b=128)
                        fw.op("dve", lambda E, am=am, p3=p3, mk=mk: E.tensor_tensor(am[:], p3, mk[:].unsqueeze(1).to_broadcast([128, 4, 128]), ALU.add), [pn, mkn], [amn])
                        fw.op("dve", lambda E, am=am, d=d, g=g: E.tensor_reduce(CM[:, d, g, :], am[:], AX.X, ALU.max), [amn], ["CM"])
                        fw.op("dve", lambda E, p3=p3, d=d, g=g: E.tensor_reduce(TM[:, d, g, :], p3, AX.X, ALU.max), [pn], ["TM"])
                        k += 1
                fw.op("pool", lambda E: E.memset(PM[:], 0.0), [], ["PM"])
                for d in range(2):
                    for b in range(NB):
                        rng = range(1, 16) if d == 0 else range(14, -1, -1)
                        for kt in rng:
                            g = b * 16 + kt; gp = g - 1 if d == 0 else g + 1
                            fw.op("dve", lambda E, d=d, g=g, gp=gp: E.tensor_tensor(PM[:, d, g, :], PM[:, d, gp, :], TM[:, d, gp, :], ALU.max), ["PM", "TM"], ["PM"])
                fw.op("dve", lambda E: E.tensor_tensor(MM[:], CM[:], PM[:], ALU.max), ["CM", "PM"], ["MM"])
                fw.op("dve", lambda E: E.tensor_tensor(EM[:, 0], MM[:, 0], BWF[:, :, 4:8], ALU.add), ["MM", "BWF"], ["EM"])
                fw.op("dve", lambda E: E.tensor_tensor(EM[:, 1], MM[:, 1], BWB[:, :, 12:16], ALU.add), ["MM", "BWB"], ["EM"])
                fw.op("act", lambda E: E.activation(EM[:], EM[:], AF.Exp, scale=-1.0), ["EM"], ["EM"])

                QH = SB(es, "QH", [128, 4, L], BF16); KH = SB(es, "KH", [128, 4, L], BF16); VH = SB(es, "VH", [128, 16, 512], BF16)
                HACC = SB(es, "HACC", [128, 16, 512]); MBt = SB(es, "MBt", [128, L]); HNF = SB(es, "HNF", [128, 4, L], BF16)
                WT = [SB(es, f"WT{i}", [128, TB]) for i in range(2)]
                PT = [SB(es, f"PT{i}", [128, TB], BF16) for i in range(2)]
                sm4 = SB(es, "sm4", [128, 8, 4]); st6 = SB(es, "st6", [128, 6]); mv = SB(es, "mv", [128, 2]); hn = SB(es, "hnt", [128, 512])
                for b in range(NB):
                    bs = slice(b * L, (b + 1) * L)
                    for hd in range(4):
                        fw.dma("sp", QH[:], QT[hd * 4:(hd + 1) * 4, :, bs].rearrange("k p t -> p k t"), reads=["QT"], writes=["QH"])
                        fw.dma("sp", KH[:], KT[hd * 4:(hd + 1) * 4, :, bs].rearrange("k p t -> p k t"), reads=["KT"], writes=["KH"])
                        fw.dma("sp", VH[:], VTOK[bs, hd * 512:(hd + 1) * 512].rearrange("(t p) e -> p t e", p=128), reads=["VTOK"], writes=["VH"])
                        for d in range(2):
                            tmask = triF if d == 0 else triB
                            tmn = "triF" if d == 0 else "triB"
                            for q4 in range(4):
                                dg = DG[q4 % 2]; dgn = f"DG{q4 % 2}"
                                g0 = b * 16 + q4 * 4
                                fw.op("pool", lambda E, dg=dg, d=d, g0=g0, hd=hd: E.tensor_tensor(dg[:], identb, MM[:, d, g0:g0 + 4, hd:hd + 1].to_broadcast([128, 4, 128]), ALU.mult),
                                      ["ident", "MM"], [dgn])
                                fw.op("pe", lambda E, dg=dg: E.matmul(PS[7][:], ones[:], dg[:].rearrange("p a b -> p (a b)"), start=True, stop=True), ["ones", dgn], ["ps7"])
                                fw.op("act", lambda E, q4=q4: E.copy(MBt[:, q4 * TB:(q4 + 1) * TB], PS[7][:]), ["ps7"], ["MBt"])
                            kk = 0
                            for Q in range(4):
                                keys = range(0, 4 * Q + 4) if d == 0 else range(4 * Q, 16)
                                qsl = slice(Q * TB, (Q + 1) * TB)
                                rs_started = [False]
                                for tk in keys:
                                    w = kk % 2; ps_s = PS[5 + w]; psn = PSN[5 + w]; wt = WT[w]; wtn = f"WT{w}"; pt = PT[w]; ptn = f"PT{w}"
                                    ksl = slice(tk * 128, (tk + 1) * 128)
                                    for kc in range(4):
                                        fw.op("pe", lambda E, ps_s=ps_s, kc=kc, ksl=ksl, qsl=qsl: E.matmul(ps_s[:], KH[:, kc, ksl], QH[:, kc, qsl], start=(kc == 0), stop=(kc == 3)),
                                              ["KH", "QH"], [psn])
                                    acol = AALL[:, d, b * 16 + tk, hd:hd + 1]
                                    fw.op("act", lambda E, wt=wt, qsl=qsl, acol=acol: E.activation(wt[:], MBt[:, qsl], AF.Exp, bias=acol, scale=-1.0), ["MBt", "AALL"], [wtn])
                                    fw.op("dve", lambda E, pt=pt, ps_s=ps_s, wt=wt: E.scalar_tensor_tensor(pt[:], ps_s[:], SC, wt[:], ALU.mult, ALU.mult), [psn, wtn], [ptn])
                                    for qs in range(4):
                                        tq = 4 * Q + qs
                                        valid = (tk <= tq) if d == 0 else (tk >= tq)
                                        if not valid:
                                            continue
                                        sub = slice(qs * 128, (qs + 1) * 128)
                                        if tk == tq:
                                            fw.op("pool", lambda E, pt=pt, sub=sub, tmask=tmask: E.tensor_tensor(pt[:, sub], pt[:, sub], tmask[:], ALU.mult), [ptn, tmn], [ptn])
                                        first = (tk == 0) if d == 0 else (tk == tq)
                                        last = (tk == tq) if d == 0 else (tk == 15)
                                        fw.op("pe", lambda E, qs=qs, pt=pt, sub=sub, tk=tk, first=first, last=last: E.matmul(PS[qs][:], pt[:, sub], VH[:, tk, :], start=first, stop=last),
                                              [ptn, "VH"], [PSN[qs]])
                                        rfirst = not rs_started[0]
                                        rs_started[0] = True
                                        fw.op("pe", lambda E, qs=qs, pt=pt, sub=sub, rfirst=rfirst, last=last: E.matmul(PS[4][:, qs:qs + 1], pt[:, sub], onesb[:, 0:1], start=rfirst, stop=last),
                                              [ptn, "onesb"], ["ps4"])
                                    kk += 1
                                for qs in range(4):
                                    tq = 4 * Q + qs; g = b * 16 + tq
                                    c0 = sm4[:, qs, 0:1]; c1 = sm4[:, qs, 1:2]
                                    fw.op("dve", lambda E, qs=qs, c1=c1: E.tensor_scalar(c1, PS[4][:, qs:qs + 1], -1.0, None, ALU.mult), ["ps4"], ["sm4"])
                                    fw.op("dve", lambda E, qs=qs, c0=c0, c1=c1: E.tensor_tensor(c0, PS[4][:, qs:qs + 1], c1, ALU.max), ["ps4", "sm4"], ["sm4"])
                                    fw.op("dve", lambda E, c0=c0, d=d, g=g, hd=hd: E.tensor_tensor(c0, c0, EM[:, d, g, hd:hd + 1], ALU.max), ["sm4", "EM"], ["sm4"])
                                    fw.op("dve", lambda E, c0=c0, c1=c1: E.reciprocal(c1, c0), ["sm4"], ["sm4"])
                                    if d == 0:
                                        fw.op("act", lambda E, qs=qs, tq=tq, c1=c1: E.activation(HACC[:, tq, :], PS[qs][:], AF.Copy, scale=c1), [PSN[qs], "sm4"], ["HACC"])
                                    else:
                                        fw.op("dve", lambda E, qs=qs, tq=tq, c1=c1: E.scalar_tensor_tensor(HACC[:, tq, :], PS[qs][:], c1, HACC[:, tq, :], ALU.mult, ALU.add),
                                              [PSN[qs], "sm4", "HACC"], ["HACC"])
                        for tt in range(16):
                            fw.op("dve", lambda E, tt=tt: E.bn_stats(st6[:], HACC[:, tt, :]), ["HACC"], ["st6"])
                            fw.op("dve", lambda E: E.bn_aggr(mv[:], st6[:]), ["st6"], ["mv"])
                            fw.op("act", lambda E: E.activation(mv[:, 1:2], mv[:, 1:2], AF.Sqrt, bias=1e-5, scale=1.0), ["mv"], ["mv"])
                            fw.op("dve", lambda E: E.reciprocal(mv[:, 1:2], mv[:, 1:2]), ["mv"], ["mv"])
                            fw.op("dve", lambda E, tt=tt: E.tensor_scalar(hn[:], HACC[:, tt, :], mv[:, 0:1], mv[:, 1:2], ALU.subtract, ALU.mult), ["HACC", "mv"], ["hnt"])
                            p = PS[5 + tt % 2]; pn = PSN[5 + tt % 2]
                            for es_ in range(4):
                                fw.op("pe", lambda E, p=p, es_=es_: E.transpose(p[:, es_ * 128:(es_ + 1) * 128], hn[:, es_ * 128:(es_ + 1) * 128], ident[:]), ["hnt", "ident"], [pn])
                            fw.op("act", lambda E, p=p, tt=tt: E.copy(HNF[:, :, tt * 128:(tt + 1) * 128], p[:].rearrange("p (a b) -> p a b", b=128)), [pn], ["HNF"])
                        fw.dma("act", HNT[hd * 4:(hd + 1) * 4, :, bs].rearrange("k p t -> p k t"), HNF[:], reads=["HNF"], writes=["HNT"])

        def phase_ml_out(j, li):
            with ExitStack() as es:
                Wo = SB(es, "Wo2", [128, 16, D], BF16)
                load_w_bf16(es, Wo, "Wo2", I["ml_w_out"][j], 16, D, "wo2")
                gn = SB(es, "gn", [128, 16]); sk = SB(es, "sk", [128, 16])
                fw.dma("sp", gn[:], I["ml_gnT"][:, j], writes=["gn"])
                fw.dma("sp", sk[:], I["ml_skipT"][:, j], writes=["sk"])
                HN = SB(es, "HNb", [128, 16, TB], BF16); XCb = SB(es, "XCb", [128, 16, TB], BF16); SZ = SB(es, "SZb2", [128, 16, TB], BF16)
                Y = SB(es, "Yb", [128, 16, TB], BF16); XB = SB(es, "XBo2", [128, 8, TB])
                tm = [SB(es, f"tm{i}", [128, TB]) for i in range(2)]
                for tb in range(NTB):
                    b = tb // (NTB // NB); ts_ = slice(tb * TB, (tb + 1) * TB)
                    fw.dma("sp", HN[:], HNT[:, :, ts_].rearrange("k p t -> p k t"), reads=["HNT"], writes=["HNb"])
                    fw.dma("sp", XCb[:], XCT[:, :, ts_].rearrange("k p t -> p k t"), reads=["XCT"], writes=["XCb"])
                    fw.dma("sp", SZ[:], SZT[:, :, ts_].rearrange("k p t -> p k t"), reads=["dst_b"], writes=["SZb2"])
                    fw.dma("sp", XB[:], XT[:, :, ts_].rearrange("k p t -> p k t"), reads=["XT"], writes=["XBo2"])
                    for cc in range(16):
                        t_ = tm[cc % 2]; tn_ = f"tm{cc % 2}"
                        fw.op("pool", lambda E, t_=t_, cc=cc: E.tensor_scalar(t_[:], XCb[:, cc, :], sk[:, cc:cc + 1], None, ALU.mult), ["XCb", "sk"], [tn_])
                        fw.op("dve", lambda E, t_=t_, cc=cc: E.scalar_tensor_tensor(t_[:], HN[:, cc, :], gn[:, cc:cc + 1], t_[:], ALU.mult, ALU.add), ["HNb", "gn", tn_], [tn_])
                        fw.op("dve" if cc % 2 else "pool", lambda E, t_=t_, cc=cc: E.tensor_tensor(Y[:, cc, :], t_[:], SZ[:, cc, :], ALU.mult), [tn_, "SZb2"], ["Yb"])
                    for mt in range(8):
                        p = PS[mt % 4]; pn = PSN[mt % 4]
                        for kc in range(16):
                            fw.op("pe", lambda E, p=p, kc=kc, mt=mt: E.matmul(p[:], Wo[:, kc, mt * 128:(mt + 1) * 128], Y[:, kc, :], start=(kc == 0), stop=(kc == 15)),
                                  ["Wo2", "Yb"], [pn])
                        fw.op("dve", lambda E, p=p, mt=mt, b=b: E.scalar_tensor_tensor(XB[:, mt, :], p[:], MOD[:, li, 16 + mt, b:b + 1], XB[:, mt, :], ALU.mult, ALU.add),
                              [pn, "MOD", "XBo2"], ["XBo2"])
                    fw.dma("act", XT[:, :, ts_].rearrange("k p t -> p k t"), XB[:], reads=["XBo2"], writes=["XT"])

        def phase_final():
            with ExitStack() as es:
                XB = SB(es, "XBf", [128, 8, TB]); FO = SB(es, "FO", [128, 8, TB])
                sq = SB(es, "sq", [128, 4, TB]); rs = SB(es, "rs", [128, TB])
                ot = [SB(es, f"ot{i}", [128, D]) for i in range(2)]
                k = 0
                for tb in range(NTB):
                    ts_ = slice(tb * TB, (tb + 1) * TB)
                    fw.dma("sp", XB[:], XT[:, :, ts_].rearrange("k p t -> p k t"), reads=["XT"], writes=["XBf"])
                    norm_block(XB, "XBf", FO, "FO", (sq, rs), 0, 0, final=True)
                    for t4 in range(4):
                        o = ot[k % 2]; on = f"ot{k % 2}"
                        for h in range(2):
                            p = PS[h]; pn = PSN[h]
                            for q in range(4):
                                kc = h * 4 + q
                                fw.op("pe", lambda E, p=p, q=q, kc=kc, t4=t4: E.transpose(p[:, q * 128:(q + 1) * 128], FO[:, kc, t4 * 128:(t4 + 1) * 128], ident[:]),
                                      ["FO", "ident"], [pn])
                            copy_op("dve" if h == 0 else "act", o[:, h * 512:(h + 1) * 512], p[:], [pn], [on])
                        r0 = tb * TB + t4 * 128
                        fw.dma("sp", OUT[r0:r0 + 128, :], o[:], reads=[on], writes=["OUT"])
                        k += 1

        stages = []
        stages.append(("mod", phase_mod))
        stages.append(("in", phase_in))
        for li in range(4):
            j = li // 2
            if li % 2 == 0:
                stages.append((f"inproj{li}", lambda li=li, j=j: phase_inproj(li, I["s5_w_in"][j], 2 * D, UT, SZT)))
                stages.append((f"ssm{li}", lambda j=j: phase_s5_ssm(j)))
                stages.append((f"s5out{li}", lambda li=li, j=j: phase_s5_out(j, li)))
            else:
                stages.append((f"inproj{li}", lambda li=li, j=j: phase_inproj(li, I["ml_w_in"][j], 4 * D, XMT, SZT)))
                stages.append((f"qkv{li}", lambda j=j: phase_ml_qkv(j, GATES)))
                stages.append((f"attn{li}", lambda j=j: phase_ml_attn(j, GATES)))
                stages.append((f"mlout{li}", lambda li=li, j=j: phase_ml_out(j, li)))
        stages.append(("final", phase_final))
        GATES = SB(top, "GATES", [128, 32, 16])
        for name, fn in stages:
            if dbg_stop and name in dbg_stop:
                break
            fn()
            fw.barrier()
        if "MODD" in dbg:
            MODD = nc.dram_tensor("MODD", [128, 4 * 24 * NB], F32, kind="ExternalOutput").ap()
            fw.dma("sp", MODD, MOD[:].rearrange("p a b c -> p (a b c)"), reads=["MOD"], writes=["MODD"])
        if "GATESD" in dbg:
            GD = nc.dram_tensor("GATESD", [128, 512], F32, kind="ExternalOutput").ap()
            fw.dma("sp", GD, GATES[:].rearrange("p a b -> p (a b)"), reads=["GATES"], writes=["GATESD"])
        fw.finish(["OUT", "MODD", "GATESD", "XT", "dst_a", "dst_b", "GT", "YT", "XCT", "QT", "KT", "KTOK", "VTOK", "HNT"], "sp")
        fw.run_block()
    print("n_inst", fw.n_inst, {e: len(fw.prog[e]) for e in fw.prog})
    return nc


def _prep_shared(inp):
    f = np.float32
    S = {}
    S["ada_w"] = np.ascontiguousarray(inp["ada_w"], dtype=f)
    S["ada_bT"] = np.ascontiguousarray(inp["ada_b"].reshape(4, 24, 128).transpose(2, 0, 1), dtype=f)
    S["norm_gT"] = np.ascontiguousarray(inp["norm_g"].reshape(4, 8, 128).transpose(2, 0, 1), dtype=f)
    S["final_gT"] = np.ascontiguousarray(inp["final_g"].reshape(8, 128).T, dtype=f)
    S["ident"] = np.eye(128, dtype=f)
    S["ones"] = np.ones((128, 128), f)
    S["j1"] = np.ascontiguousarray(np.broadcast_to(np.arange(1, TB + 1, dtype=f), (128, TB)))
    t = np.arange(128)[:, None]; s = np.arange(128)[None, :]
    S["maskF"] = np.where(s <= t, 0.0, -30000.0).astype(f)
    S["maskB"] = np.where(s >= t, 0.0, -30000.0).astype(f)
    S["triF"] = (t <= s).astype(f)
    S["triB"] = (t >= s).astype(f)
    for k in ("s5_w_in", "s5_w_glu", "s5_w_out", "ml_w_in", "ml_w_out"):
        S[k] = np.ascontiguousarray(inp[k], dtype=f)
    S["s5_b_gluT"] = np.ascontiguousarray(inp["s5_b_glu"].reshape(2, 8, 128).transpose(2, 0, 1), dtype=f)
    S["s5_dT"] = np.ascontiguousarray(inp["s5_d"].reshape(2, 8, 128).transpose(2, 0, 1), dtype=f)

    def st_layout(a):
        a = a.reshape(2, 2, 32, 2, 64)
        return np.ascontiguousarray(a.transpose(3, 4, 0, 1, 2).reshape(128, 2, 2, 32), dtype=f)
    S["s5_lre"] = st_layout(inp["s5_lam_re"])
    S["s5_lim"] = st_layout(inp["s5_lam_im"])
    S["s5_ldt"] = st_layout(np.broadcast_to(inp["s5_log_dt"][..., None], (2, 2, 64, 64)))

    def b_layout(bm):
        out = np.zeros((2, 2, 128, 32, 128), f)
        for st in range(32):
            pi = st % 4
            for gl in range(2):
                g = 2 * st + gl
                k0 = (2 * pi + gl) * 16
                out[:, :, k0:k0 + 16, st, gl * 64:(gl + 1) * 64] = bm[:, :, g].transpose(0, 1, 3, 2)
        return out.reshape(2, 2, 128, 32 * 128)

    def c_layout(cm):
        out = np.zeros((2, 2, 128, 32, 128), f)
        for st in range(32):
            pi = st % 4
            for gl in range(2):
                g = 2 * st + gl
                m0 = (2 * pi + gl) * 16
                out[:, :, gl * 64:(gl + 1) * 64, st, m0:m0 + 16] = cm[:, :, g].transpose(0, 1, 3, 2)
        return out.reshape(2, 2, 128, 32 * 128)
    S["s5_bre"] = b_layout(np.asarray(inp["s5_b_re"], f)); S["s5_bim"] = b_layout(np.asarray(inp["s5_b_im"], f))
    S["s5_cre"] = c_layout(np.asarray(inp["s5_c_re"], f)); S["s5_cim"] = c_layout(np.asarray(inp["s5_c_im"], f))
    S["ml_convT"] = np.ascontiguousarray(inp["ml_conv_w"].reshape(2, 5, 16, 128).transpose(3, 0, 2, 1), dtype=f)
    S["ml_convbT"] = np.ascontiguousarray(inp["ml_conv_b"].reshape(2, 16, 128).transpose(2, 0, 1), dtype=f)

    def bd_layout(w):
        out = np.zeros((2, 128, 16, 128), f)
        wr = np.asarray(w, f).reshape(2, 16, 32, 4, 4)
        for n in range(32):
            out[:, 4 * n:4 * n + 4, :, 4 * n:4 * n + 4] = wr[:, :, n].transpose(0, 2, 1, 3)
        return out.reshape(2, 128, 16 * 128)
    S["ml_wq_bd"] = bd_layout(inp["ml_w_q"]); S["ml_wk_bd"] = bd_layout(inp["ml_w_k"]); S["ml_wv_bd"] = bd_layout(inp["ml_w_v"])
    wg = np.asarray(inp["ml_w_gates"], f).reshape(2, 3, 16, 128, 16)
    S["ml_wg"] = np.ascontiguousarray(wg.transpose(0, 3, 1, 2, 4).reshape(2, 128, 768))
    S["ml_bg_rep"] = np.ascontiguousarray(np.broadcast_to(np.asarray(inp["ml_b_gates"], f)[None], (128, 2, 16)))
    S["ml_gnT"] = np.ascontiguousarray(inp["ml_gn_w"].reshape(2, 16, 128).transpose(2, 0, 1), dtype=f)
    S["ml_skipT"] = np.ascontiguousarray(inp["ml_skip"].reshape(2, 16, 128).transpose(2, 0, 1), dtype=f)
    return S


_NC_CACHE = {}


def kernel(**inputs):
    inp = {k: np.asarray(v) for k, v in inputs.items()}
    S = _prep_shared(inp)
    x = np.asarray(inp["x"], np.float32); c = np.asarray(inp["c"], np.float32)
    in_maps = []
    for core in range(NCORES):
        m = dict(S)
        m["x"] = np.ascontiguousarray(x[core * NB:(core + 1) * NB].reshape(NT, D))
        cc = c[core * NB:(core + 1) * NB]
        m["cT"] = np.ascontiguousarray(cc.reshape(NB, 8, 128).transpose(2, 1, 0))
        in_maps.append(m)
    if "nc" not in _NC_CACHE:
        _NC_CACHE["nc"] = build_program()
    res = run_bass_kernel_spmd(_NC_CACHE["nc"], in_maps, core_ids=list(range(NCORES)))
    out = np.concatenate([r["out"].reshape(NB, L, D) for r in res.results], axis=0)
    return out.astype(np.float32)
```

```python
import numpy as np
from contextlib import ExitStack
import concourse.bass as bass
import concourse.mybir as mybir
from concourse.ap import AP
from concourse.bass_utils import run_bass_kernel_spmd

F32 = mybir.dt.float32
BF16 = mybir.dt.bfloat16
I32 = mybir.dt.int32
ALU = mybir.AluOpType
AF = mybir.ActivationFunctionType
AX = mybir.AxisListType

NCORES = 8
D = 1024
L = 2048
NB = 2
NT = NB * L
TB = 512
NTB = NT // TB
SEM_LIMIT = 30000
N_DMA_SEMS = 12
TWO_PI = float(2 * np.pi)
SERIAL_DMA = False


class FW:
    ENGS = ("pe", "act", "dve", "pool", "sp")
    same_engine_sync = True

    def __init__(self, nc):
        self.nc = nc
        self.prog = {e: [] for e in self.ENGS}
        self.cur_sem = {}
        self.cnt = {}
        self.nsem = 0
        for e in self.ENGS:
            self._new_sem(e)
        self.waited = {}
        self.res = {}
        self.dma_sems = {}
        self.dma_rr = {}
        for e in ("sp", "act", "pool"):
            self.dma_sems[e] = [[self._alloc_sem(f"dma_{e}_{i}"), 0] for i in range(N_DMA_SEMS)]
            self.dma_rr[e] = 0
        self.n_inst = 0
        self.last_dma = {}

    def _alloc_sem(self, name):
        self.nsem += 1
        return self.nc.alloc_semaphore(name=f"{name}_{self.nsem}")

    def _new_sem(self, e):
        self.cur_sem[e] = self._alloc_sem(f"cnt_{e}")
        self.cnt[e] = 0

    def _need(self, eng, tok, waits):
        if tok is None:
            return
        sem, val, owner = tok
        if self.waited.get((eng, id(sem)), 0) >= val:
            return
        if owner == eng and (eng == "pe" or not self.same_engine_sync):
            return
        old = waits.get(id(sem))
        if old is None or old[1] < val:
            waits[id(sem)] = (sem, val)

    def _deps(self, eng, reads, writes):
        waits = {}
        for r in reads:
            ent = self.res.get(r)
            if ent is not None:
                self._need(eng, ent[0], waits)
        for w in writes:
            ent = self.res.get(w)
            if ent is not None:
                self._need(eng, ent[0], waits)
                for t in ent[1]:
                    self._need(eng, t, waits)
        return waits

    def _emit_waits(self, eng, waits):
        for sid, (sem, val) in waits.items():
            self.waited[(eng, sid)] = val
            self.prog[eng].append(lambda E, sem=sem, val=val: E.wait_ge(sem, val))
            self.n_inst += 1

    def _update(self, tok, reads, writes):
        for r in reads:
            ent = self.res.setdefault(r, [None, []])
            ent[1].append(tok)
            if len(ent[1]) > 48:
                best = {}
                for t in ent[1]:
                    k = id(t[0])
                    if k not in best or best[k][1] < t[1]:
                        best[k] = t
                ent[1] = list(best.values())
        for w in writes:
            self.res[w] = [tok, []]

    def op(self, eng, fn, reads=(), writes=()):
        reads = [r for r in reads if r is not None]
        writes = [w for w in writes if w is not None]
        waits = self._deps(eng, reads, writes)
        self._emit_waits(eng, waits)
        if self.cnt[eng] >= SEM_LIMIT:
            self._new_sem(eng)
        sem = self.cur_sem[eng]
        self.cnt[eng] += 1
        val = self.cnt[eng]
        self.prog[eng].append(lambda E, fn=fn, sem=sem: fn(E).then_inc(sem, 1))
        self.n_inst += 1
        tok = (sem, val, eng)
        self._update(tok, reads, writes)
        return tok

    def dma(self, q, out, in_, reads=(), writes=()):
        reads = [r for r in reads if r is not None]
        writes = [w for w in writes if w is not None]
        waits = self._deps(q, reads, writes)
        slot = self.dma_sems[q][self.dma_rr[q] % N_DMA_SEMS]
        self.dma_rr[q] += 1
        sem, used = slot
        if used > 0 and self.waited.get((q, id(sem)), 0) < used:
            old = waits.get(id(sem))
            if old is None or old[1] < used:
                waits[id(sem)] = (sem, used)
        if SERIAL_DMA and self.last_dma.get(q) is not None:
            ps_, pv_ = self.last_dma[q]
            if self.waited.get((q, id(ps_)), 0) < pv_:
                old = waits.get(id(ps_))
                if old is None or old[1] < pv_:
                    waits[id(ps_)] = (ps_, pv_)
        self._emit_waits(q, waits)
        slot[1] = used + 16
        self.last_dma[q] = (sem, slot[1])
        val = slot[1]
        self.prog[q].append(lambda E, out=out, in_=in_, sem=sem: E.dma_start(out=out, in_=in_).then_inc(sem, 16))
        self.n_inst += 1
        tok = (sem, val, "dma")
        self._update(tok, reads, writes)
        return tok

    def barrier(self):
        for eng in self.ENGS:
            waits = {}
            for other in self.ENGS:
                if other == eng or self.cnt[other] == 0:
                    continue
                sem = self.cur_sem[other]
                if self.waited.get((eng, id(sem)), 0) < self.cnt[other]:
                    waits[id(sem)] = (sem, self.cnt[other])
            for q in self.dma_sems:
                for sem, used in self.dma_sems[q]:
                    if used > 0 and self.waited.get((eng, id(sem)), 0) < used:
                        waits[id(sem)] = (sem, used)
            self._emit_waits(eng, waits)

    def finish(self, names, eng="sp"):
        waits = {}
        for n in names:
            ent = self.res.get(n)
            if ent is not None:
                self._need(eng, ent[0], waits)
        self._emit_waits(eng, waits)

    def run_block(self):
        nc = self.nc
        with nc.Block() as block:
            @block.tensor
            def _(E):
                for f in self.prog["pe"]:
                    f(E)

            @block.scalar
            def _(E):
                for f in self.prog["act"]:
                    f(E)

            @block.vector
            def _(E):
                for f in self.prog["dve"]:
                    f(E)

            @block.gpsimd
            def _(E):
                for f in self.prog["pool"]:
                    f(E)

            @block.sync
            def _(E):
                for f in self.prog["sp"]:
                    f(E)


def rev_ap(ap2d, start, n):
    a = list(ap2d.ap)
    return AP(ap2d.tensor, ap2d.offset + start * a[-1][0], [list(a[0]), [-a[-1][0], n]])


INPUT_SPECS = [
    ("x", [NT, D]), ("cT", [128, 8, NB]), ("ada_w", [4, D, 3 * D]), ("ada_bT", [128, 4, 24]),
    ("norm_gT", [128, 4, 8]), ("final_gT", [128, 8]), ("ident", [128, 128]), ("ones", [128, 128]),
    ("j1", [128, TB]), ("maskF", [128, 128]), ("maskB", [128, 128]), ("triF", [128, 128]), ("triB", [128, 128]),
    ("s5_w_in", [2, D, 2 * D]), ("s5_w_glu", [2, D, D]), ("s5_w_out", [2, D, D]),
    ("s5_b_gluT", [128, 2, 8]), ("s5_dT", [128, 2, 8]),
    ("s5_lre", [128, 2, 2, 32]), ("s5_lim", [128, 2, 2, 32]), ("s5_ldt", [128, 2, 2, 32]),
    ("s5_bre", [2, 2, 128, 32 * 128]), ("s5_bim", [2, 2, 128, 32 * 128]),
    ("s5_cre", [2, 2, 128, 32 * 128]), ("s5_cim", [2, 2, 128, 32 * 128]),
    ("ml_w_in", [2, D, 4 * D]), ("ml_w_out", [2, 2 * D, D]),
    ("ml_convT", [128, 2, 16, 5]), ("ml_convbT", [128, 2, 16]),
    ("ml_wq_bd", [2, 128, 16 * 128]), ("ml_wk_bd", [2, 128, 16 * 128]), ("ml_wv_bd", [2, 128, 16 * 128]),
    ("ml_wg", [2, 128, 3 * 16 * 16]), ("ml_bg_rep", [128, 2, 16]),
    ("ml_gnT", [128, 2, 16]), ("ml_skipT", [128, 2, 16]),
]


def build_program(dbg=(), dbg_stop=()):
    nc = bass.Bass("TRN2", target_bir_lowering=False)
    I = {}
    for name, shape in INPUT_SPECS:
        I[name] = nc.dram_tensor(name, shape, F32, kind="ExternalInput").ap()
    OUT = nc.dram_tensor("out", [NT, D], F32, kind="ExternalOutput").ap()

    def scratch(name, shape, dt):
        kind = "ExternalOutput" if name in dbg else "Internal"
        return nc.dram_tensor(name, shape, dt, kind=kind).ap()

    XT = scratch("XT", [8, 128, NT], F32)
    UT = scratch("UT", [8, 128, NT], BF16)
    SZT = scratch("SZT", [16, 128, NT], BF16)
    GT = scratch("GT", [8, 128, NT], BF16)
    XMT = scratch("XMT", [16, 128, NT], BF16)
    XCT = scratch("XCT", [16, 128, NT], BF16)
    QT = scratch("QT", [16, 128, NT], BF16)
    KT = scratch("KT", [16, 128, NT], BF16)
    KTOK = scratch("KTOK", [NT, 2 * D], BF16)
    VTOK = scratch("VTOK", [NT, 2 * D], BF16)
    HNT = scratch("HNT", [16, 128, NT], BF16)

    fw = FW(nc)
    rr = {"cast": 0, "ev": 0}

    with ExitStack() as top:
        def SB(es, name, shape, dt=F32):
            rr["sb"] = rr.get("sb", 0) + 1
            return es.enter_context(nc.sbuf_tensor(f"sb{rr['sb']}_{name}", shape, dt))

        PS = [top.enter_context(nc.psum_tensor(f"ps{i}", [128, 512], F32)) for i in range(8)]
        PSN = [f"ps{i}" for i in range(8)]

        ident = SB(top, "ident", [128, 128]); ones = SB(top, "ones", [128, 128])
        onesb = SB(top, "onesb", [128, 128], BF16)
        maskF = SB(top, "maskF", [128, 128]); maskB = SB(top, "maskB", [128, 128])
        triF = SB(top, "triF", [128, 128]); triB = SB(top, "triB", [128, 128])
        j1 = SB(top, "j1", [128, TB])
        MOD = SB(top, "MOD", [128, 4, 24, NB])
        S1 = SB(top, "S1", [128, 4, 8, NB])
        ngT = SB(top, "ngT", [128, 4, 8]); fgT = SB(top, "fgT", [128, 8])
        for t, n, rn in ((ident, "ident", "ident"), (ones, "ones", "ones"), (maskF, "maskF", "maskF"), (maskB, "maskB", "maskB"),
                         (triF, "triF", "triF"), (triB, "triB", "triB"), (j1, "j1", "j1"), (ngT, "norm_gT", "ngT"), (fgT, "final_gT", "fgT")):
            fw.dma("sp", t[:], I[n], writes=[rn])
        fw.op("dve", lambda E: E.tensor_copy(onesb[:], ones[:]), ["ones"], ["onesb"])

        def cast_eng():
            rr["cast"] += 1
            return ("dve", "pool", "act")[rr["cast"] % 3]

        def copy_op(eng, out, in_, r, w):
            if eng == "act":
                fw.op("act", lambda E: E.copy(out, in_), r, w)
            else:
                fw.op(eng, lambda E: E.tensor_copy(out, in_), r, w)

        def load_w_bf16(es, dst, dname, src, KC, N, tag):
            CH = min(N, 2048)
            stg = [SB(es, f"stg_{tag}_{i}", [128, CH]) for i in range(2)]
            k = 0
            for kc in range(KC):
                for n0 in range(0, N, CH):
                    s = stg[k % 2]; sn = f"stg_{tag}_{k % 2}"
                    fw.dma("sp", s[:], src[kc * 128:(kc + 1) * 128, n0:n0 + CH], writes=[sn])
                    copy_op(cast_eng(), dst[:, kc, n0:n0 + CH], s[:], [sn], [dname])
                    k += 1

        def phase_mod():
            with ExitStack() as es:
                cT = SB(es, "cT", [128, 8, NB]); sc = SB(es, "sc", [128, 8, NB]); abT = SB(es, "abT", [128, 4, 24])
                wt = [SB(es, f"adaw{i}", [128, 8, 128]) for i in range(2)]
                fw.dma("sp", cT[:], I["cT"], writes=["cT"])
                fw.dma("sp", abT[:], I["ada_bT"], writes=["abT"])
                fw.op("act", lambda E: E.activation(sc[:], cT[:], AF.Silu), ["cT"], ["sc"])
                k = 0
                for i in range(4):
                    for m in range(24):
                        w = wt[k % 2]; wn = f"adaw{k % 2}"
                        src = I["ada_w"][i].rearrange("(kc p) n -> p kc n", p=128)[:, :, m * 128:(m + 1) * 128]
                        fw.dma("sp" if k % 2 == 0 else "act", w[:], src, writes=[wn])
                        for kc in range(8):
                            fw.op("pe", lambda E, w=w, kc=kc: E.matmul(PS[0][:, 0:NB], w[:, kc, :], sc[:, kc, :],
                                                                       start=(kc == 0), stop=(kc == 7)), [wn, "sc"], ["ps0"])
                        fw.op("dve", lambda E, i=i, m=m: E.tensor_scalar(MOD[:, i, m, :], PS[0][:, 0:NB], abT[:, i, m:m + 1], None, ALU.add),
                              ["ps0", "abT"], ["MOD"])
                        k += 1
                for i in range(4):
                    for b in range(NB):
                        fw.op("dve", lambda E, i=i, b=b: E.scalar_tensor_tensor(S1[:, i, :, b], MOD[:, i, 8:16, b], 1.0, ngT[:, i, :], ALU.add, ALU.mult),
                              ["MOD", "ngT"], ["S1"])

        def phase_in():
            with ExitStack() as es:
                xi = [SB(es, f"xi{i}", [128, D]) for i in range(2)]
                xo = [SB(es, f"xo{i}", [128, 8, 128]) for i in range(2)]
                for tt in range(NT // 128):
                    a = xi[tt % 2]; an = f"xi{tt % 2}"; o = xo[tt % 2]; on = f"xo{tt % 2}"
                    fw.dma("sp", a[:], I["x"][tt * 128:(tt + 1) * 128, :], writes=[an])
                    for h in range(2):
                        p = PS[h]; pn = PSN[h]
                        for q in range(4):
                            kc = h * 4 + q
                            fw.op("pe", lambda E, p=p, q=q, kc=kc, a=a: E.transpose(p[:, q * 128:(q + 1) * 128], a[:, kc * 128:(kc + 1) * 128], ident[:]),
                                  [an, "ident"], [pn])
                        copy_op("dve" if h == 0 else "act", o[:, h * 4:(h + 1) * 4, :].rearrange("p a b -> p (a b)"), p[:], [pn], [on])
                    fw.dma("act", XT[:, :, tt * 128:(tt + 1) * 128].rearrange("k p t -> p k t"), o[:], reads=[on], writes=["XT"])

        def norm_block(XB, xbn, HB, hbn, tmps, li, b, final=False):
            sq, rs = tmps
            for kc in range(8):
                fw.op("act", lambda E, kc=kc: E.activation(sq[:, kc % 2, :], XB[:, kc, :], AF.Square), [xbn], [f"sq{kc % 2}"])
                fw.op("pe", lambda E, kc=kc: E.matmul(PS[7][:], ones[:], sq[:, kc % 2, :], start=(kc == 0), stop=(kc == 7)),
                      [f"sq{kc % 2}", "ones"], ["ps7"])
            fw.op("act", lambda E: E.activation(rs[:], PS[7][:], AF.Sqrt, bias=1e-6, scale=1.0 / D), ["ps7"], ["rs"])
            fw.op("dve", lambda E: E.reciprocal(rs[:], rs[:]), ["rs"], ["rs"])
            for kc in range(8):
                eng = "pool"
                if final:
                    fw.op("dve", lambda E, kc=kc: E.scalar_tensor_tensor(HB[:, kc, :], XB[:, kc, :], fgT[:, kc:kc + 1], rs[:], ALU.mult, ALU.mult),
                          [xbn, "rs", "fgT"], [hbn])
                else:
                    fw.op("dve", lambda E, kc=kc: E.scalar_tensor_tensor(sq[:, 2 + kc % 2, :], XB[:, kc, :], S1[:, li, kc, b:b + 1], rs[:], ALU.mult, ALU.mult),
                          [xbn, "rs", "S1"], [f"sq{2 + kc % 2}"])
                    fw.op(eng, lambda E, kc=kc: E.tensor_scalar(HB[:, kc, :], sq[:, 2 + kc % 2, :], MOD[:, li, kc, b:b + 1], None, ALU.add),
                          [f"sq{2 + kc % 2}", "MOD"], [hbn])

        def phase_inproj(li, w_src, NOUT, dst_a, dst_b):
            NM = NOUT // 128
            with ExitStack() as es:
                W = SB(es, "Win", [128, 8, NOUT], BF16)
                load_w_bf16(es, W, "Win", w_src, 8, NOUT, "win")
                XBs = [SB(es, f"XB{i}", [128, 8, TB]) for i in range(2)]
                HB = SB(es, "HB", [128, 8, TB], BF16)
                sq = SB(es, "sq", [128, 4, TB]); rs = SB(es, "rs", [128, TB])
                ob = [SB(es, f"ob{i}", [128, TB], BF16) for i in range(4)]
                fw.dma("sp", XBs[0][:], XT[:, :, 0:TB].rearrange("k p t -> p k t"), reads=["XT"], writes=["XB0"])
                for tb in range(NTB):
                    XB = XBs[tb % 2]; xbn = f"XB{tb % 2}"; b = tb // (NTB // NB)
                    if tb + 1 < NTB:
                        fw.dma("sp", XBs[(tb + 1) % 2][:], XT[:, :, (tb + 1) * TB:(tb + 2) * TB].rearrange("k p t -> p k t"),
                               reads=["XT"], writes=[f"XB{(tb + 1) % 2}"])
                    norm_block(XB, xbn, HB, "HB", (sq, rs), li, b)
                    for mt in range(NM):
                        p = PS[mt % 4]; pn = PSN[mt % 4]
                        for kc in range(8):
                            fw.op("pe", lambda E, p=p, kc=kc, mt=mt: E.matmul(p[:], W[:, kc, mt * 128:(mt + 1) * 128], HB[:, kc, :],
                                                                              start=(kc == 0), stop=(kc == 7)), ["Win", "HB"], [pn])
                        o = ob[mt % 4]; on = f"ob{mt % 4}"
                        if mt < NM // 2:
                            copy_op("dve" if mt % 2 == 0 else "act", o[:], p[:], [pn], [on])
                            fw.dma("act", dst_a[mt][:, tb * TB:(tb + 1) * TB], o[:], reads=[on], writes=["dst_a"])
                        else:
                            fw.op("act", lambda E, o=o, p=p: E.activation(o[:], p[:], AF.Silu), [pn], [on])
                            fw.dma("act", dst_b[mt - NM // 2][:, tb * TB:(tb + 1) * TB], o[:], reads=[on], writes=["dst_b"])

        YT = scratch("YT", [8, 128, NT], F32)

        def phase_s5_ssm(j):
            with ExitStack() as es:
                lre = SB(es, "lre", [128, 2, 32]); lim = SB(es, "lim", [128, 2, 32]); ldt = SB(es, "ldt", [128, 2, 32])
                fw.dma("sp", lre[:], I["s5_lre"][:, j], writes=["lre"])
                fw.dma("sp", lim[:], I["s5_lim"][:, j], writes=["lim"])
                fw.dma("sp", ldt[:], I["s5_ldt"][:, j], writes=["ldt"])
                dT = SB(es, "dT", [128, 8])
                fw.dma("sp", dT[:], I["s5_dT"][:, j], writes=["dT"])
                tn = ["dt", "th", "rmag", "ar", "ai", "k1", "k2", "k3", "den", "zre", "zim", "nzim", "t1s", "t2s"]
                T = {n: SB(es, "s5t_" + n, [128, 2, 32]) for n in tn}
                ki = SB(es, "s5t_ki", [128, 2, 32], I32)

                def sm(eng, fn, r, w):
                    fw.op(eng, fn, ["s5t_" + x if x in T else x for x in r], ["s5t_" + x if x in T else x for x in w])

                sm("act", lambda E: E.activation(T["dt"][:], ldt[:], AF.Exp), ["ldt"], ["dt"])
                sm("dve", lambda E: E.tensor_tensor(T["th"][:], lim[:], T["dt"][:], ALU.mult), ["lim", "dt"], ["th"])
                sm("dve", lambda E: E.tensor_tensor(T["k1"][:], lre[:], T["dt"][:], ALU.mult), ["lre", "dt"], ["k1"])
                sm("act", lambda E: E.activation(T["rmag"][:], T["k1"][:], AF.Exp), ["k1"], ["rmag"])
                for dst, sh in (("ai", 0.0), ("ar", float(np.pi / 2))):
                    sm("dve", lambda E, sh=sh: E.tensor_scalar(T["k2"][:], T["th"][:], sh, None, ALU.add), ["th"], ["k2"])
                    sm("dve", lambda E: E.tensor_scalar(ki[:], T["k2"][:], 1.0 / TWO_PI, None, ALU.mult), ["k2"], ["s5t_ki"])
                    sm("dve", lambda E: E.tensor_copy(T["k3"][:], ki[:]), ["s5t_ki"], ["k3"])
                    sm("dve", lambda E: E.scalar_tensor_tensor(T["k2"][:], T["k3"][:], -TWO_PI, T["k2"][:], ALU.mult, ALU.add), ["k3", "k2"], ["k2"])
                    sm("act", lambda E, dst=dst: E.activation(T[dst][:], T["k2"][:], AF.Sin), ["k2"], [dst])
                sm("dve", lambda E: E.tensor_tensor(T["ar"][:], T["ar"][:], T["rmag"][:], ALU.mult), ["ar", "rmag"], ["ar"])
                sm("dve", lambda E: E.tensor_tensor(T["ai"][:], T["ai"][:], T["rmag"][:], ALU.mult), ["ai", "rmag"], ["ai"])
                sm("dve", lambda E: E.tensor_scalar(T["k1"][:], T["ar"][:], -1.0, None, ALU.add), ["ar"], ["k1"])
                sm("dve", lambda E: E.tensor_tensor(T["den"][:], lre[:], lre[:], ALU.mult), ["lre"], ["den"])
                sm("dve", lambda E: E.tensor_tensor(T["k2"][:], lim[:], lim[:], ALU.mult), ["lim"], ["k2"])
                sm("dve", lambda E: E.tensor_tensor(T["den"][:], T["den"][:], T["k2"][:], ALU.add), ["den", "k2"], ["den"])
                sm("dve", lambda E: E.reciprocal(T["den"][:], T["den"][:]), ["den"], ["den"])
                sm("dve", lambda E: E.tensor_tensor(T["t1s"][:], T["k1"][:], lre[:], ALU.mult), ["k1", "lre"], ["t1s"])
                sm("dve", lambda E: E.tensor_tensor(T["t2s"][:], T["ai"][:], lim[:], ALU.mult), ["ai", "lim"], ["t2s"])
                sm("dve", lambda E: E.tensor_tensor(T["zre"][:], T["t1s"][:], T["t2s"][:], ALU.add), ["t1s", "t2s"], ["zre"])
                sm("dve", lambda E: E.tensor_tensor(T["zre"][:], T["zre"][:], T["den"][:], ALU.mult), ["zre", "den"], ["zre"])
                sm("dve", lambda E: E.tensor_tensor(T["t1s"][:], T["ai"][:], lre[:], ALU.mult), ["ai", "lre"], ["t1s"])
                sm("dve", lambda E: E.tensor_tensor(T["t2s"][:], T["k1"][:], lim[:], ALU.mult), ["k1", "lim"], ["t2s"])
                sm("dve", lambda E: E.tensor_tensor(T["zim"][:], T["t1s"][:], T["t2s"][:], ALU.subtract), ["t1s", "t2s"], ["zim"])
                sm("dve", lambda E: E.tensor_tensor(T["zim"][:], T["zim"][:], T["den"][:], ALU.mult), ["zim", "den"], ["zim"])
                sm("dve", lambda E: E.tensor_scalar(T["nzim"][:], T["zim"][:], -1.0, None, ALU.mult), ["zim"], ["nzim"])

                B4 = SB(es, "B4", [128, 2, 4 * 128], BF16)
                C4 = SB(es, "C4", [128, 2, 4 * 128], BF16)
                stg = [SB(es, f"s5stg{i}", [128, 4 * 128]) for i in range(4)]
                tC = SB(es, "tC", [128, 128])
                cosT = SB(es, "cosT", [128, 4, TB]); sinT = SB(es, "sinT", [128, 4, TB]); RM = SB(es, "RM", [128, 4, TB])
                ph = SB(es, "ph", [128, TB]); phk = SB(es, "phk", [128, TB]); phi = SB(es, "phi", [128, TB], I32)
                Us = [SB(es, f"Uc{i}", [128, NT], BF16) for i in range(2)]
                YACC = SB(es, "YACC", [128, NT])
                Gc = SB(es, "Gc", [128, NT], BF16)
                tmps = [{n: SB(es, f"w{q}_{n}", [128, TB]) for n in ("t1", "t2", "t3", "t4", "pre", "pim", "sre", "sim")} for q in range(3)]
                u2 = [SB(es, f"u2_{q}", [128, 2, TB]) for q in range(3)]
                sbf = [SB(es, f"sbf{q}", [128, 2, TB], BF16) for q in range(3)]
                carry = SB(es, "carry", [128, 4, NB, 4])
                it = 0
                for d in range(2):
                    for cc in range(8):
                        U = Us[it % 2]; un = f"Uc{it % 2}"
                        fw.dma("sp", U[:], UT[cc], reads=["dst_a"], writes=[un])
                        for q, key in enumerate(("s5_bre", "s5_bim", "s5_cre", "s5_cim")):
                            fw.dma("sp", stg[q][:], I[key][j, d][:, cc * 512:(cc + 1) * 512], writes=[f"s5stg{q}"])
                        fw.op("act", lambda E: E.copy(B4[:, 0, :], stg[0][:]), ["s5stg0"], ["B4"])
                        fw.op("act", lambda E: E.copy(B4[:, 1, :], stg[1][:]), ["s5stg1"], ["B4"])
                        for pi in range(4):
                            st = 4 * cc + pi
                            sl = slice(pi * 128, (pi + 1) * 128)
                            zr = T["zre"][:, d, st:st + 1]; zi = T["zim"][:, d, st:st + 1]; nzi = T["nzim"][:, d, st:st + 1]
                            fw.op("dve", lambda E, sl=sl, zi=zi: E.tensor_scalar(tC[:], stg[3][:, sl], zi, None, ALU.mult), ["s5stg3", "s5t_zim"], ["tC"])
                            fw.op("dve", lambda E, sl=sl, zr=zr: E.scalar_tensor_tensor(C4[:, 0, sl], stg[2][:, sl], zr, tC[:], ALU.mult, ALU.subtract),
                                  ["s5stg2", "s5t_zre", "tC"], ["C4"])
                            fw.op("dve", lambda E, sl=sl, zr=zr: E.tensor_scalar(tC[:], stg[3][:, sl], zr, None, ALU.mult), ["s5stg3", "s5t_zre"], ["tC"])
                            fw.op("dve", lambda E, sl=sl, nzi=nzi: E.scalar_tensor_tensor(C4[:, 1, sl], stg[2][:, sl], nzi, tC[:], ALU.mult, ALU.subtract),
                                  ["s5stg2", "s5t_nzim", "tC"], ["C4"])
                            th = T["th"][:, d, st:st + 1]
                            fw.op("pool", lambda E, th=th: E.tensor_scalar(ph[:], j1[:], th, None, ALU.mult), ["j1", "s5t_th"], ["ph"])
                            for dstT, dn, sh in ((sinT, "sinT", 0.0), (cosT, "cosT", float(np.pi / 2))):
                                fw.op("pool", lambda E, sh=sh: E.tensor_scalar(phk[:], ph[:], sh, None, ALU.add), ["ph"], ["phk"])
                                fw.op("pool", lambda E: E.tensor_scalar(phi[:], phk[:], 1.0 / TWO_PI, None, ALU.mult), ["phk"], ["phi"])
                                fw.op("pool", lambda E: E.tensor_copy(ph[:] if False else tmps[0]["t1"][:], phi[:]), ["phi"], ["w0_t1"])
                                fw.op("dve", lambda E: E.scalar_tensor_tensor(phk[:], tmps[0]["t1"][:], -TWO_PI, phk[:], ALU.mult, ALU.add), ["w0_t1", "phk"], ["phk"])
                                fw.op("act", lambda E, dstT=dstT, pi=pi: E.activation(dstT[:, pi, :], phk[:], AF.Sin), ["phk"], [dn])
                            rm = T["rmag"][:, d, st:st + 1]
                            fw.op("pool", lambda E, pi=pi, rm=rm: E.tensor_scalar(RM[:, pi, :], j1[:], 0.0, rm, ALU.mult, ALU.add), ["j1", "s5t_rmag"], ["RM"])
                        fw.op("pool", lambda E: E.memset(carry[:], 0.0), [], ["carry"])
                        if d == 0:
                            fw.op("pool", lambda E, U=U, cc=cc: E.tensor_scalar(YACC[:], U[:], dT[:, cc:cc + 1], None, ALU.mult), [un, "dT"], ["YACC"])
                        else:
                            fw.dma("sp", YACC[:], YT[cc], reads=["YT"], writes=["YACC"])
                        k = 0
                        for b in range(NB):
                            for chi in range(4):
                                ch = chi if d == 0 else 3 - chi
                                T0 = b * L + ch * TB
                                if d == 0:
                                    rhs = U[:, T0:T0 + TB]; yv = YACC[:, T0:T0 + TB]
                                else:
                                    rhs = rev_ap(U[:], T0 + TB - 1, TB); yv = rev_ap(YACC[:], T0 + TB - 1, TB)
                                py = PS[6 + (k // 4) % 2]; pyn = PSN[6 + (k // 4) % 2]
                                for pi in range(4):
                                    q = k % 3
                                    W = tmps[q]; wn = lambda n, q=q: f"w{q}_{n}"
                                    pa = PS[2 * q]; pan = PSN[2 * q]; pb = PS[2 * q + 1]; pbn = PSN[2 * q + 1]
                                    sl = slice(pi * 128, (pi + 1) * 128)
                                    fw.op("pe", lambda E, pa=pa, sl=sl, rhs=rhs: E.matmul(pa[:], B4[:, 0, sl], rhs, start=True, stop=True), ["B4", un], [pan])
                                    fw.op("pe", lambda E, pb=pb, sl=sl, rhs=rhs: E.matmul(pb[:], B4[:, 1, sl], rhs, start=True, stop=True), ["B4", un], [pbn])
                                    cs = cosT[:, pi, :]; sn = sinT[:, pi, :]
                                    fw.op("dve", lambda E, W=W, pa=pa, cs=cs: E.tensor_tensor(W["t1"][:], pa[:], cs, ALU.mult), [pan, "cosT"], [wn("t1")])
                                    fw.op("dve", lambda E, W=W, pb=pb, sn=sn: E.tensor_tensor(W["t2"][:], pb[:], sn, ALU.mult), [pbn, "sinT"], [wn("t2")])
                                    fw.op("dve", lambda E, W=W, pb=pb, cs=cs: E.tensor_tensor(W["t3"][:], pb[:], cs, ALU.mult), [pbn, "cosT"], [wn("t3")])
                                    fw.op("dve", lambda E, W=W, pa=pa, sn=sn: E.tensor_tensor(W["t4"][:], pa[:], sn, ALU.mult), [pan, "sinT"], [wn("t4")])
                                    fw.op("pool", lambda E, W=W: E.tensor_tensor(W["pre"][:], W["t1"][:], W["t2"][:], ALU.add), [wn("t1"), wn("t2")], [wn("pre")])
                                    fw.op("pool", lambda E, W=W: E.tensor_tensor(W["pim"][:], W["t3"][:], W["t4"][:], ALU.subtract), [wn("t3"), wn("t4")], [wn("pim")])
                                    fw.op("dve", lambda E, W=W, pi=pi, b=b: E.tensor_tensor_scan(W["sre"][:], RM[:, pi, :], W["pre"][:], carry[:, pi, b, 0:1], ALU.mult, ALU.add),
                                          ["RM", wn("pre"), "carry"], [wn("sre")])
                                    fw.op("dve", lambda E, W=W, pi=pi, b=b: E.tensor_tensor_scan(W["sim"][:], RM[:, pi, :], W["pim"][:], carry[:, pi, b, 1:2], ALU.mult, ALU.add),
                                          ["RM", wn("pim"), "carry"], [wn("sim")])
                                    fw.op("pool", lambda E, W=W, cs=cs: E.tensor_tensor(W["t1"][:], W["sre"][:], cs, ALU.mult), [wn("sre"), "cosT"], [wn("t1")])
                                    fw.op("pool", lambda E, W=W, sn=sn: E.tensor_tensor(W["t2"][:], W["sim"][:], sn, ALU.mult), [wn("sim"), "sinT"], [wn("t2")])
                                    fw.op("pool", lambda E, W=W, sn=sn: E.tensor_tensor(W["t3"][:], W["sre"][:], sn, ALU.mult), [wn("sre"), "sinT"], [wn("t3")])
                                    fw.op("pool", lambda E, W=W, cs=cs: E.tensor_tensor(W["t4"][:], W["sim"][:], cs, ALU.mult), [wn("sim"), "cosT"], [wn("t4")])
                                    uu = u2[q]; uun = f"u2_{q}"
                                    fw.op("dve", lambda E, W=W, uu=uu: E.tensor_tensor(uu[:, 0, :], W["t1"][:], W["t2"][:], ALU.subtract), [wn("t1"), wn("t2")], [uun])
                                    fw.op("dve", lambda E, W=W, uu=uu: E.tensor_tensor(uu[:, 1, :], W["t3"][:], W["t4"][:], ALU.add), [wn("t3"), wn("t4")], [uun])
                                    sb_ = sbf[q]; sbn = f"sbf{q}"
                                    fw.op("act", lambda E, sb_=sb_, uu=uu: E.copy(sb_[:], uu[:]), [uun], [sbn])
                                    fw.op("pool", lambda E, uu=uu, pi=pi, b=b: E.tensor_copy(carry[:, pi, b, 0:2], uu[:, :, TB - 1]), [uun], ["carry"])
                                    fw.op("pe", lambda E, py=py, sl=sl, sb_=sb_, pi=pi: E.matmul(py[:], C4[:, 0, sl], sb_[:, 0, :], start=(pi == 0), stop=False), ["C4", sbn], [pyn])
                                    fw.op("pe", lambda E, py=py, sl=sl, sb_=sb_, pi=pi: E.matmul(py[:], C4[:, 1, sl], sb_[:, 1, :], start=False, stop=(pi == 3)), ["C4", sbn], [pyn])
                                    k += 1
                                fw.op("dve", lambda E, py=py, yv=yv: E.tensor_tensor(yv, py[:], yv, ALU.add), [pyn, "YACC"], ["YACC"])
                                if "CARRYD" in dbg and d == 0 and cc == 0:
                                    CD2 = nc.dram_tensor(f"CARRYD_b{b}c{chi}", [128, 32], F32, kind="ExternalOutput").ap()
                                    fw.dma("sp", CD2, carry[:].rearrange("p a b c -> p (a b c)"), reads=["carry"], writes=["CARRYD"])
                                    if b == 1 and chi == 1:
                                        UD3 = nc.dram_tensor("CARRYD_U", [128, NT], BF16, kind="ExternalOutput").ap()
                                        fw.dma("sp", UD3, U[:], reads=[un], writes=["CARRYD"])
                                        for nm in ("t1", "pre", "sre"):
                                            UD4 = nc.dram_tensor("CARRYD_" + nm, [128, TB], F32, kind="ExternalOutput").ap()
                                            fw.dma("sp", UD4, tmps[(k - 1) % 3][nm][:], reads=[f"w{(k - 1) % 3}_{nm}"], writes=["CARRYD"])
                                        UD5 = nc.dram_tensor("CARRYD_cos", [128, 4 * TB], F32, kind="ExternalOutput").ap()
                                        fw.dma("sp", UD5, cosT[:].rearrange("p a b -> p (a b)"), reads=["cosT"], writes=["CARRYD"])
                                        UD6 = nc.dram_tensor("CARRYD_RM", [128, 4 * TB], F32, kind="ExternalOutput").ap()
                                        fw.dma("sp", UD6, RM[:].rearrange("p a b -> p (a b)"), reads=["RM"], writes=["CARRYD"])
                                    UD2 = nc.dram_tensor(f"CARRYDU_b{b}c{chi}", [128, 2 * TB], F32, kind="ExternalOutput").ap()
                                    fw.dma("sp", UD2, u2[(k - 1) % 3][:].rearrange("p a b -> p (a b)"), reads=[f"u2_{(k - 1) % 3}"], writes=["CARRYD"])
                        if "CARRYD" in dbg and d == 0 and cc < 2:
                            CD = nc.dram_tensor(f"CARRYD{cc}", [128, 32], F32, kind="ExternalOutput").ap()
                            fw.dma("sp", CD, carry[:].rearrange("p a b c -> p (a b c)"), reads=["carry"], writes=["CARRYD"])
                        if d == 0:
                            fw.dma("sp", YT[cc], YACC[:], reads=["YACC"], writes=["YT"])
                        else:
                            fw.op("act", lambda E: E.activation(Gc[:], YACC[:], AF.Gelu), ["YACC"], ["Gc"])
                            fw.dma("act", GT[cc], Gc[:], reads=["Gc"], writes=["GT"])
                        it += 1

        def phase_s5_out(j, li):
            with ExitStack() as es:
                Wg = SB(es, "Wg", [128, 8, D], BF16); Wo = SB(es, "Wo", [128, 8, D], BF16)
                load_w_bf16(es, Wg, "Wg", I["s5_w_glu"][j], 8, D, "wg")
                load_w_bf16(es, Wo, "Wo", I["s5_w_out"][j], 8, D, "wo")
                bg = SB(es, "bg", [128, 8])
                fw.dma("sp", bg[:], I["s5_b_gluT"][:, j], writes=["bg"])
                G = SB(es, "Gb", [128, 8, TB], BF16); SZ = SB(es, "SZb", [128, 8, TB], BF16); XB = SB(es, "XBo", [128, 8, TB])
                Y2 = SB(es, "Y2", [128, 8, TB], BF16)
                sg = [SB(es, f"sg{i}", [128, TB]) for i in range(2)]
                for tb in range(NTB):
                    b = tb // (NTB // NB); ts_ = slice(tb * TB, (tb + 1) * TB)
                    fw.dma("sp", G[:], GT[:, :, ts_].rearrange("k p t -> p k t"), reads=["GT"], writes=["Gb"])
                    fw.dma("sp", SZ[:], SZT[0:8, :, ts_].rearrange("k p t -> p k t"), reads=["dst_b"], writes=["SZb"])
                    fw.dma("sp", XB[:], XT[:, :, ts_].rearrange("k p t -> p k t"), reads=["XT"], writes=["XBo"])
                    for mt in range(8):
                        p = PS[mt % 4]; pn = PSN[mt % 4]; s_ = sg[mt % 2]; sn = f"sg{mt % 2}"
                        for kc in range(8):
                            fw.op("pe", lambda E, p=p, kc=kc, mt=mt: E.matmul(p[:], Wg[:, kc, mt * 128:(mt + 1) * 128], G[:, kc, :], start=(kc == 0), stop=(kc == 7)),
                                  ["Wg", "Gb"], [pn])
                        fw.op("act", lambda E, p=p, s_=s_, mt=mt: E.activation(s_[:], p[:], AF.Sigmoid, bias=bg[:, mt:mt + 1]), [pn, "bg"], [sn])
                        fw.op("dve", lambda E, s_=s_, mt=mt: E.tensor_tensor(s_[:], s_[:], G[:, mt, :], ALU.mult), [sn, "Gb"], [sn])
                        fw.op("pool", lambda E, s_=s_, mt=mt: E.tensor_tensor(Y2[:, mt, :], s_[:], SZ[:, mt, :], ALU.mult), [sn, "SZb"], ["Y2"])
                    for mt in range(8):
                        p = PS[4 + mt % 4]; pn = PSN[4 + mt % 4]
                        for kc in range(8):
                            fw.op("pe", lambda E, p=p, kc=kc, mt=mt: E.matmul(p[:], Wo[:, kc, mt * 128:(mt + 1) * 128], Y2[:, kc, :], start=(kc == 0), stop=(kc == 7)),
                                  ["Wo", "Y2"], [pn])
                        fw.op("dve", lambda E, p=p, mt=mt, b=b: E.scalar_tensor_tensor(XB[:, mt, :], p[:], MOD[:, li, 16 + mt, b:b + 1], XB[:, mt, :], ALU.mult, ALU.add),
                              [pn, "MOD", "XBo"], ["XBo"])
                    fw.dma("act", XT[:, :, ts_].rearrange("k p t -> p k t"), XB[:], reads=["XBo"], writes=["XT"])

        def phase_ml_qkv(j, GATES):
            with ExitStack() as es:
                cw = SB(es, "cw", [128, 16, 5]); cb = SB(es, "cb", [128, 16]); bgr = SB(es, "bgr", [128, 16])
                fw.dma("sp", cw[:], I["ml_convT"][:, j], writes=["cw"])
                fw.dma("sp", cb[:], I["ml_convbT"][:, j], writes=["cb"])
                fw.dma("sp", bgr[:], I["ml_bg_rep"][:, j], writes=["bgr"])
                Wq = SB(es, "Wq", [128, 1, 2048], BF16); Wk = SB(es, "Wk", [128, 1, 2048], BF16); Wv = SB(es, "Wv", [128, 1, 2048], BF16)
                load_w_bf16(es, Wq, "Wq", I["ml_wq_bd"][j], 1, 2048, "wq")
                load_w_bf16(es, Wk, "Wk", I["ml_wk_bd"][j], 1, 2048, "wk")
                load_w_bf16(es, Wv, "Wv", I["ml_wv_bd"][j], 1, 2048, "wv")
                wgs = SB(es, "wgs", [128, 768]); wg = SB(es, "wgb", [128, 768], BF16)
                fw.dma("sp", wgs[:], I["ml_wg"][j], writes=["wgs"])
                fw.op("dve", lambda E: E.tensor_copy(wg[:], wgs[:]), ["wgs"], ["wgb"])
                XMH = [SB(es, f"XMH{i}", [128, L + 4], BF16) for i in range(2)]
                for i in range(2):
                    fw.op("pool", lambda E, i=i: E.memset(XMH[i][:], 0.0), [], [f"XMH{i}"])
                acc = SB(es, "cacc", [128, L]); XC = SB(es, "XCc", [128, L], BF16)
                QTB = SB(es, "QTB", [128, L], BF16); KTB = SB(es, "KTB", [128, L], BF16); VTB = SB(es, "VTB", [128, L], BF16)
                KTK = SB(es, "KTK", [128, 16, 128], BF16); VTK = SB(es, "VTK", [128, 16, 128], BF16)
                it = 0
                for cc in range(16):
                    csl = slice(cc * 128, (cc + 1) * 128)
                    for b in range(NB):
                        xm = XMH[it % 2]; xn = f"XMH{it % 2}"; bs = slice(b * L, (b + 1) * L)
                        fw.dma("sp", xm[:, 2:2 + L], XMT[cc][:, bs], reads=["dst_a"], writes=[xn])
                        ce = "dve"
                        fw.op(ce, lambda E, xm=xm, cc=cc: E.tensor_scalar(acc[:], xm[:, 0:L], cw[:, cc, 0:1], cb[:, cc:cc + 1], ALU.mult, ALU.add), [xn, "cw", "cb"], ["cacc"])
                        for kk in range(1, 5):
                            fw.op(ce, lambda E, xm=xm, cc=cc, kk=kk: E.scalar_tensor_tensor(acc[:], xm[:, kk:kk + L], cw[:, cc, kk:kk + 1], acc[:], ALU.mult, ALU.add),
                                  [xn, "cw", "cacc"], ["cacc"])
                        fw.op("act", lambda E: E.activation(XC[:], acc[:], AF.Silu), ["cacc"], ["XCc"])
                        fw.dma("act", XCT[cc][:, bs], XC[:], reads=["XCc"], writes=["XCT"])
                        for q4 in range(4):
                            qs = slice(q4 * TB, (q4 + 1) * TB)
                            for wi, (Wm, wn, src, srn, dst, dn) in enumerate(((Wq, "Wq", XC[:, qs], "XCc", QTB, "QTB"), (Wk, "Wk", XC[:, qs], "XCc", KTB, "KTB"),
                                                                           (Wv, "Wv", xm[:, 2 + q4 * TB:2 + (q4 + 1) * TB], xn, VTB, "VTB"))):
                                p = PS[wi]; pn = PSN[wi]
                                fw.op("pe", lambda E, p=p, Wm=Wm, src=src, csl=csl: E.matmul(p[:], Wm[:, 0, csl], src, start=True, stop=True), [wn, srn], [pn])
                                copy_op("dve" if wi != 1 else "act", dst[:, qs], p[:], [pn], [dn])
                        fw.dma("act", QT[cc][:, bs], QTB[:], reads=["QTB"], writes=["QT"])
                        fw.dma("act", KT[cc][:, bs], KTB[:], reads=["KTB"], writes=["KT"])
                        for t4 in range(4):
                            pk = PS[3]; pv = PS[4]
                            for tq in range(4):
                                tt = t4 * 4 + tq; tsl = slice(tt * 128, (tt + 1) * 128); osl = slice(tq * 128, (tq + 1) * 128)
                                fw.op("pe", lambda E, tsl=tsl, osl=osl, csl=csl: E.matmul(pk[:, osl], XC[:, tsl], Wk[:, 0, csl], start=True, stop=True), ["XCc", "Wk"], ["ps3"])
                                fw.op("pe", lambda E, tsl=tsl, osl=osl, xm=xm, tt=tt, csl=csl: E.matmul(pv[:, osl], xm[:, 2 + tt * 128:2 + (tt + 1) * 128], Wv[:, 0, csl], start=True, stop=True),
                                      [xn, "Wv"], ["ps4"])
                                gsl = slice((b * 16 + tt) * 16, (b * 16 + tt + 1) * 16)
                                for wi, (src, srn) in enumerate(((QTB, "QTB"), (KTB, "KTB"), (VTB, "VTB"))):
                                    fw.op("pe", lambda E, src=src, tsl=tsl, gsl=gsl, wi=wi, cc=cc, b=b, tt=tt: E.matmul(
                                        PS[6][:, gsl], src[:, tsl], wg[:, (wi * 16 + cc) * 16:(wi * 16 + cc + 1) * 16],
                                        start=(cc == 0 and wi == 0 and b == 0 and tt == 0), stop=(cc == 15 and wi == 2)), [srn, "wgb"], ["ps6"])
                            copy_op("dve", KTK[:, t4 * 4:(t4 + 1) * 4, :].rearrange("p a b -> p (a b)"), pk[:], ["ps3"], ["KTK"])
                            copy_op("act", VTK[:, t4 * 4:(t4 + 1) * 4, :].rearrange("p a b -> p (a b)"), pv[:], ["ps4"], ["VTK"])
                        fw.dma("act", KTOK[bs, csl].rearrange("(t p) c -> p t c", p=128), KTK[:], reads=["KTK"], writes=["KTOK"])
                        fw.dma("act", VTOK[bs, csl].rearrange("(t p) c -> p t c", p=128), VTK[:], reads=["VTK"], writes=["VTOK"])
                        it += 1
                fw.op("dve", lambda E: E.tensor_tensor(GATES[:], PS[6][:].rearrange("p (a b) -> p a b", b=16),
                                                       bgr[:].unsqueeze(1).to_broadcast([128, 32, 16]), ALU.add), ["ps6", "bgr"], ["GATES"])

        def phase_ml_attn(j, GATES):
            SC = float(512 ** -0.5)
            with ExitStack() as es:
                def G3(name, last=16, dt=F32):
                    return SB(es, name, [128, 32, last], dt)
                E1 = G3("E1"); LF = G3("LF"); BWF = G3("BWF"); BWB = G3("BWB"); TOT = G3("TOT"); OFF = G3("OFF")
                fw.op("act", lambda E: E.activation(E1[:], GATES[:], AF.Exp, scale=-1.0), ["GATES"], ["E1"])
                fw.op("act", lambda E: E.activation(E1[:], E1[:], AF.Ln, bias=1.0), ["E1"], ["E1"])
                fw.op("dve", lambda E: E.tensor_scalar(LF[:], E1[:], -1.0, None, ALU.mult), ["E1"], ["LF"])
                LFf = LF[:].rearrange("p a b -> p (a b)")
                fw.op("pe", lambda E: E.matmul(PS[0][:], triF[:], LFf, start=True, stop=True), ["triF", "LF"], ["ps0"])
                fw.op("pe", lambda E: E.matmul(PS[1][:], triB[:], LFf, start=True, stop=True), ["triB", "LF"], ["ps1"])
                fw.op("pe", lambda E: E.matmul(PS[2][:], ones[:], LFf, start=True, stop=True), ["ones", "LF"], ["ps2"])
                fw.op("dve", lambda E: E.tensor_copy(BWF[:].rearrange("p a b -> p (a b)"), PS[0][:]), ["ps0"], ["BWF"])
                fw.op("act", lambda E: E.copy(BWB[:].rearrange("p a b -> p (a b)"), PS[1][:]), ["ps1"], ["BWB"])
                fw.op("dve", lambda E: E.tensor_copy(TOT[:].rearrange("p a b -> p (a b)"), PS[2][:]), ["ps2"], ["TOT"])
                for (BW, bwn, fwd) in ((BWF, "BWF", True), (BWB, "BWB", False)):
                    fw.op("pool", lambda E: E.memset(OFF[:], 0.0), [], ["OFF"])
                    for b in range(NB):
                        rng = range(1, 16) if fwd else range(14, -1, -1)
                        for kt in rng:
                            g = b * 16 + kt; gp = g - 1 if fwd else g + 1
                            fw.op("pool", lambda E, g=g, gp=gp: E.tensor_tensor(OFF[:, g, :], OFF[:, gp, :], TOT[:, gp, :], ALU.add), ["OFF", "TOT"], ["OFF"])
                    fw.op("pool", lambda E, BW=BW: E.tensor_tensor(BW[:], BW[:], OFF[:], ALU.add), [bwn, "OFF"], [bwn])
                AALL = SB(es, "AALL", [128, 2, 32, 4]); CM = SB(es, "CM", [128, 2, 32, 4]); TM = SB(es, "TM", [128, 2, 32, 4])
                PM = SB(es, "PM", [128, 2, 32, 4]); MM = SB(es, "MM", [128, 2, 32, 4]); EM = SB(es, "EM", [128, 2, 32, 4])
                fw.op("dve", lambda E: E.tensor_tensor(AALL[:, 0], GATES[:, :, 0:4], BWF[:, :, 4:8], ALU.subtract), ["GATES", "BWF"], ["AALL"])
                fw.op("dve", lambda E: E.tensor_tensor(AALL[:, 1], GATES[:, :, 8:12], BWB[:, :, 12:16], ALU.subtract), ["GATES", "BWB"], ["AALL"])
                DG = [SB(es, f"DG{i}", [128, 4, 128]) for i in range(2)]
                AM = [SB(es, f"AM{i}", [128, 4, 128]) for i in range(2)]
                identb = ident[:].unsqueeze(1).to_broadcast([128, 4, 128])
                k = 0
                for d in range(2):
                    mk = maskF if d == 0 else maskB
                    mkn = "maskF" if d == 0 else "maskB"
                    for g in range(32):
                        q = k % 2; dg = DG[q]; dgn = f"DG{q}"; am = AM[q]; amn = f"AM{q}"; p = PS[q]; pn = PSN[q]
                        fw.op("pool", lambda E, dg=dg, d=d, g=g: E.tensor_tensor(dg[:], identb, AALL[:, d, g, :].unsqueeze(2).to_broadcast([128, 4, 128]), ALU.mult),
                              ["ident", "AALL"], [dgn])
                        fw.op("pe", lambda E, p=p, dg=dg: E.matmul(p[:], ones[:], dg[:].rearrange("p a b -> p (a b)"), start=True, stop=True), ["ones", dgn], [pn])
                        p3 = p[:].rearrange("p (a b) -> p a b", b=128)
                        fw.op("dve", lambda E, am=am, p3=p3, mk=mk: E.tensor_tensor(am[:], p3, mk[:].unsqueeze(1).to_broadcast([128, 4, 128]), ALU.add), [pn, mkn], [amn])
                        fw.op("dve", lambda E, am=am, d=d, g=g: E.tensor_reduce(CM[:, d, g, :], am[:], AX.X, ALU.max), [amn], ["CM"])
                        fw.op("dve", lambda E, p3=p3, d=d, g=g: E.tensor_reduce(TM[:, d, g, :], p3, AX.X, ALU.max), [pn], ["TM"])
                        k += 1
                fw.op("pool", lambda E: E.memset(PM[:], 0.0), [], ["PM"])
                for d in range(2):
                    for b in range(NB):
                        rng = range(1, 16) if d == 0 else range(14, -1, -1)
                        for kt in rng:
                            g = b * 16 + kt; gp = g - 1 if d == 0 else g + 1
                            fw.op("dve", lambda E, d=d, g=g, gp=gp: E.tensor_tensor(PM[:, d, g, :], PM[:, d, gp, :], TM[:, d, gp, :], ALU.max), ["PM", "TM"], ["PM"])
                fw.op("dve", lambda E: E.tensor_tensor(MM[:], CM[:], PM[:], ALU.max), ["CM", "PM"], ["MM"])
                fw.op("dve", lambda E: E.tensor_tensor(EM[:, 0], MM[:, 0], BWF[:, :, 4:8], ALU.add), ["MM", "BWF"], ["EM"])
                fw.op("dve", lambda E: E.tensor_tensor(EM[:, 1], MM[:, 1], BWB[:, :, 12:16], ALU.add), ["MM", "BWB"], ["EM"])
                fw.op("act", lambda E: E.activation(EM[:], EM[:], AF.Exp, scale=-1.0), ["EM"], ["EM"])

                QH = SB(es, "QH", [128, 4, L], BF16); KH = SB(es, "KH", [128, 4, L], BF16); VH = SB(es, "VH", [128, 16, 512], BF16)
                HACC = SB(es, "HACC", [128, 16, 512]); MBt = SB(es, "MBt", [128, L]); HNF = SB(es, "HNF", [128, 4, L], BF16)
                WT = [SB(es, f"WT{i}", [128, TB]) for i in range(2)]
                PT = [SB(es, f"PT{i}", [128, TB], BF16) for i in range(2)]
                sm4 = SB(es, "sm4", [128, 8, 4]); st6 = SB(es, "st6", [128, 6]); mv = SB(es, "mv", [128, 2]); hn = SB(es, "hnt", [128, 512])
                for b in range(NB):
                    bs = slice(b * L, (b + 1) * L)
                    for hd in range(4):
                        fw.dma("sp", QH[:], QT[hd * 4:(hd + 1) * 4, :, bs].rearrange("k p t -> p k t"), reads=["QT"], writes=["QH"])
                        fw.dma("sp", KH[:], KT[hd * 4:(hd + 1) * 4, :, bs].rearrange("k p t -> p k t"), reads=["KT"], writes=["KH"])
                        fw.dma("sp", VH[:], VTOK[bs, hd * 512:(hd + 1) * 512].rearrange("(t p) e -> p t e", p=128), reads=["VTOK"], writes=["VH"])
                        for d in range(2):
                            tmask = triF if d == 0 else triB
                            tmn = "triF" if d == 0 else "triB"
                            for q4 in range(4):
                                dg = DG[q4 % 2]; dgn = f"DG{q4 % 2}"
                                g0 = b * 16 + q4 * 4
                                fw.op("pool", lambda E, dg=dg, d=d, g0=g0, hd=hd: E.tensor_tensor(dg[:], identb, MM[:, d, g0:g0 + 4, hd:hd + 1].to_broadcast([128, 4, 128]), ALU.mult),
                                      ["ident", "MM"], [dgn])
                                fw.op("pe", lambda E, dg=dg: E.matmul(PS[7][:], ones[:], dg[:].rearrange("p a b -> p (a b)"), start=True, stop=True), ["ones", dgn], ["ps7"])
                                fw.op("act", lambda E, q4=q4: E.copy(MBt[:, q4 * TB:(q4 + 1) * TB], PS[7][:]), ["ps7"], ["MBt"])
                            kk = 0
                            for Q in range(4):
                                keys = range(0, 4 * Q + 4) if d == 0 else range(4 * Q, 16)
                                qsl = slice(Q * TB, (Q + 1) * TB)
                                rs_started = [False]
                                for tk in keys:
                                    w = kk % 2; ps_s = PS[5 + w]; psn = PSN[5 + w]; wt = WT[w]; wtn = f"WT{w}"; pt = PT[w]; ptn = f"PT{w}"
                                    ksl = slice(tk * 128, (tk + 1) * 128)
                                    for kc in range(4):
                                        fw.op("pe", lambda E, ps_s=ps_s, kc=kc, ksl=ksl, qsl=qsl: E.matmul(ps_s[:], KH[:, kc, ksl], QH[:, kc, qsl], start=(kc == 0), stop=(kc == 3)),
                                              ["KH", "QH"], [psn])
                                    acol = AALL[:, d, b * 16 + tk, hd:hd + 1]
                                    fw.op("act", lambda E, wt=wt, qsl=qsl, acol=acol: E.activation(wt[:], MBt[:, qsl], AF.Exp, bias=acol, scale=-1.0), ["MBt", "AALL"], [wtn])
                                    fw.op("dve", lambda E, pt=pt, ps_s=ps_s, wt=wt: E.scalar_tensor_tensor(pt[:], ps_s[:], SC, wt[:], ALU.mult, ALU.mult), [psn, wtn], [ptn])
                                    for qs in range(4):
                                        tq = 4 * Q + qs
                                        valid = (tk <= tq) if d == 0 else (tk >= tq)
                                        if not valid:
                                            continue
                                        sub = slice(qs * 128, (qs + 1) * 128)
                                        if tk == tq:
                                            fw.op("pool", lambda E, pt=pt, sub=sub, tmask=tmask: E.tensor_tensor(pt[:, sub], pt[:, sub], tmask[:], ALU.mult), [ptn, tmn], [ptn])
                                        first = (tk == 0) if d == 0 else (tk == tq)
                                        last = (tk == tq) if d == 0 else (tk == 15)
                                        fw.op("pe", lambda E, qs=qs, pt=pt, sub=sub, tk=tk, first=first, last=last: E.matmul(PS[qs][:], pt[:, sub], VH[:, tk, :], start=first, stop=last),
                                              [ptn, "VH"], [PSN[qs]])
                                        rfirst = not rs_started[0]
                                        rs_started[0] = True
                                        fw.op("pe", lambda E, qs=qs, pt=pt, sub=sub, rfirst=rfirst, last=last: E.matmul(PS[4][:, qs:qs + 1], pt[:, sub], onesb[:, 0:1], start=rfirst, stop=last),
                                              [ptn, "onesb"], ["ps4"])
                                    kk += 1
                                for qs in range(4):
                                    tq = 4 * Q + qs; g = b * 16 + tq
                                    c0 = sm4[:, qs, 0:1]; c1 = sm4[:, qs, 1:2]
                                    fw.op("dve", lambda E, qs=qs, c1=c1: E.tensor_scalar(c1, PS[4][:, qs:qs + 1], -1.0, None, ALU.mult), ["ps4"], ["sm4"])
                                    fw.op("dve", lambda E, qs=qs, c0=c0, c1=c1: E.tensor_tensor(c0, PS[4][:, qs:qs + 1], c1, ALU.max), ["ps4", "sm4"], ["sm4"])
                                    fw.op("dve", lambda E, c0=c0, d=d, g=g, hd=hd: E.tensor_tensor(c0, c0, EM[:, d, g, hd:hd + 1], ALU.max), ["sm4", "EM"], ["sm4"])
                                    fw.op("dve", lambda E, c0=c0, c1=c1: E.reciprocal(c1, c0), ["sm4"], ["sm4"])
                                    if d == 0:
                                        fw.op("act", lambda E, qs=qs, tq=tq, c1=c1: E.activation(HACC[:, tq, :], PS[qs][:], AF.Copy, scale=c1), [PSN[qs], "sm4"], ["HACC"])
                                    else:
                                        fw.op("dve", lambda E, qs=qs, tq=tq, c1=c1: E.scalar_tensor_tensor(HACC[:, tq, :], PS[qs][:], c1, HACC[:, tq, :], ALU.mult, ALU.add),
                                              [PSN[qs], "sm4", "HACC"], ["HACC"])
                        for tt in range(16):
                            fw.op("dve", lambda E, tt=tt: E.bn_stats(st6[:], HACC[:, tt, :]), ["HACC"], ["st6"])
                            fw.op("dve", lambda E: E.bn_aggr(mv[:], st6[:]), ["st6"], ["mv"])
                            fw.op("act", lambda E: E.activation(mv[:, 1:2], mv[:, 1:2], AF.Sqrt, bias=1e-5, scale=1.0), ["mv"], ["mv"])
                            fw.op("dve", lambda E: E.reciprocal(mv[:, 1:2], mv[:, 1:2]), ["mv"], ["mv"])
                            fw.op("dve", lambda E, tt=tt: E.tensor_scalar(hn[:], HACC[:, tt, :], mv[:, 0:1], mv[:, 1:2], ALU.subtract, ALU.mult), ["HACC", "mv"], ["hnt"])
                            p = PS[5 + tt % 2]; pn = PSN[5 + tt % 2]
                            for es_ in range(4):
                                fw.op("pe", lambda E, p=p, es_=es_: E.transpose(p[:, es_ * 128:(es_ + 1) * 128], hn[:, es_ * 128:(es_ + 1) * 128], ident[:]), ["hnt", "ident"], [pn])
                            fw.op("act", lambda E, p=p, tt=tt: E.copy(HNF[:, :, tt * 128:(tt + 1) * 128], p[:].rearrange("p (a b) -> p a b", b=128)), [pn], ["HNF"])
                        fw.dma("act", HNT[hd * 4:(hd + 1) * 4, :, bs].rearrange("k p t -> p k t"), HNF[:], reads=["HNF"], writes=["HNT"])

        def phase_ml_out(j, li):
            with ExitStack() as es:
                Wo = SB(es, "Wo2", [128, 16, D], BF16)
                load_w_bf16(es, Wo, "Wo2", I["ml_w_out"][j], 16, D, "wo2")
                gn = SB(es, "gn", [128, 16]); sk = SB(es, "sk", [128, 16])
                fw.dma("sp", gn[:], I["ml_gnT"][:, j], writes=["gn"])
                fw.dma("sp", sk[:], I["ml_skipT"][:, j], writes=["sk"])
                HN = SB(es, "HNb", [128, 16, TB], BF16); XCb = SB(es, "XCb", [128, 16, TB], BF16); SZ = SB(es, "SZb2", [128, 16, TB], BF16)
                Y = SB(es, "Yb", [128, 16, TB], BF16); XB = SB(es, "XBo2", [128, 8, TB])
                tm = [SB(es, f"tm{i}", [128, TB]) for i in range(2)]
                for tb in range(NTB):
                    b = tb // (NTB // NB); ts_ = slice(tb * TB, (tb + 1) * TB)
                    fw.dma("sp", HN[:], HNT[:, :, ts_].rearrange("k p t -> p k t"), reads=["HNT"], writes=["HNb"])
                    fw.dma("sp", XCb[:], XCT[:, :, ts_].rearrange("k p t -> p k t"), reads=["XCT"], writes=["XCb"])
                    fw.dma("sp", SZ[:], SZT[:, :, ts_].rearrange("k p t -> p k t"), reads=["dst_b"], writes=["SZb2"])
                    fw.dma("sp", XB[:], XT[:, :, ts_].rearrange("k p t -> p k t"), reads=["XT"], writes=["XBo2"])
                    for cc in range(16):
                        t_ = tm[cc % 2]; tn_ = f"tm{cc % 2}"
                        fw.op("pool", lambda E, t_=t_, cc=cc: E.tensor_scalar(t_[:], XCb[:, cc, :], sk[:, cc:cc + 1], None, ALU.mult), ["XCb", "sk"], [tn_])
                        fw.op("dve", lambda E, t_=t_, cc=cc: E.scalar_tensor_tensor(t_[:], HN[:, cc, :], gn[:, cc:cc + 1], t_[:], ALU.mult, ALU.add), ["HNb", "gn", tn_], [tn_])
                        fw.op("dve" if cc % 2 else "pool", lambda E, t_=t_, cc=cc: E.tensor_tensor(Y[:, cc, :], t_[:], SZ[:, cc, :], ALU.mult), [tn_, "SZb2"], ["Yb"])
                    for mt in range(8):
                        p = PS[mt % 4]; pn = PSN[mt % 4]
                        for kc in range(16):
                            fw.op("pe", lambda E, p=p, kc=kc, mt=mt: E.matmul(p[:], Wo[:, kc, mt * 128:(mt + 1) * 128], Y[:, kc, :], start=(kc == 0), stop=(kc == 15)),
                                  ["Wo2", "Yb"], [pn])
                        fw.op("dve", lambda E, p=p, mt=mt, b=b: E.scalar_tensor_tensor(XB[:, mt, :], p[:], MOD[:, li, 16 + mt, b:b + 1], XB[:, mt, :], ALU.mult, ALU.add),
                              [pn, "MOD", "XBo2"], ["XBo2"])
                    fw.dma("act", XT[:, :, ts_].rearrange("k p t -> p k t"), XB[:], reads=["XBo2"], writes=["XT"])

        def phase_final():
            with ExitStack() as es:
                XB = SB(es, "XBf", [128, 8, TB]); FO = SB(es, "FO", [128, 8, TB])
                sq = SB(es, "sq", [128, 4, TB]); rs = SB(es, "rs", [128, TB])
                ot = [SB(es, f"ot{i}", [128, D]) for i in range(2)]
                k = 0
                for tb in range(NTB):
                    ts_ = slice(tb * TB, (tb + 1) * TB)
                    fw.dma("sp", XB[:], XT[:, :, ts_].rearrange("k p t -> p k t"), reads=["XT"], writes=["XBf"])
                    norm_block(XB, "XBf", FO, "FO", (sq, rs), 0, 0, final=True)
                    for t4 in range(4):
                        o = ot[k % 2]; on = f"ot{k % 2}"
                        for h in range(2):
                            p = PS[h]; pn = PSN[h]
                            for q in range(4):
                                kc = h * 4 + q
                                fw.op("pe", lambda E, p=p, q=q, kc=kc, t4=t4: E.transpose(p[:, q * 128:(q + 1) * 128], FO[:, kc, t4 * 128:(t4 + 1) * 128], ident[:]),
                                      ["FO", "ident"], [pn])
                            copy_op("dve" if h == 0 else "act", o[:, h * 512:(h + 1) * 512], p[:], [pn], [on])
                        r0 = tb * TB + t4 * 128
                        fw.dma("sp", OUT[r0:r0 + 128, :], o[:], reads=[on], writes=["OUT"])
                        k += 1

        stages = []
        stages.append(("mod", phase_mod))
        stages.append(("in", phase_in))
        for li in range(4):
            j = li // 2
            if li % 2 == 0:
                stages.append((f"inproj{li}", lambda li=li, j=j: phase_inproj(li, I["s5_w_in"][j], 2 * D, UT, SZT)))
                stages.append((f"ssm{li}", lambda j=j: phase_s5_ssm(j)))
                stages.append((f"s5out{li}", lambda li=li, j=j: phase_s5_out(j, li)))
            else:
                stages.append((f"inproj{li}", lambda li=li, j=j: phase_inproj(li, I["ml_w_in"][j], 4 * D, XMT, SZT)))
                stages.append((f"qkv{li}", lambda j=j: phase_ml_qkv(j, GATES)))
                stages.append((f"attn{li}", lambda j=j: phase_ml_attn(j, GATES)))
                stages.append((f"mlout{li}", lambda li=li, j=j: phase_ml_out(j, li)))
        stages.append(("final", phase_final))
        GATES = SB(top, "GATES", [128, 32, 16])
        for name, fn in stages:
            if dbg_stop and name in dbg_stop:
                break
            fn()
            fw.barrier()
        if "MODD" in dbg:
            MODD = nc.dram_tensor("MODD", [128, 4 * 24 * NB], F32, kind="ExternalOutput").ap()
            fw.dma("sp", MODD, MOD[:].rearrange("p a b c -> p (a b c)"), reads=["MOD"], writes=["MODD"])
        if "GATESD" in dbg:
            GD = nc.dram_tensor("GATESD", [128, 512], F32, kind="ExternalOutput").ap()
            fw.dma("sp", GD, GATES[:].rearrange("p a b -> p (a b)"), reads=["GATES"], writes=["GATESD"])
        fw.finish(["OUT", "MODD", "GATESD", "XT", "dst_a", "dst_b", "GT", "YT", "XCT", "QT", "KT", "KTOK", "VTOK", "HNT"], "sp")
        fw.run_block()
    print("n_inst", fw.n_inst, {e: len(fw.prog[e]) for e in fw.prog})
    return nc


def _prep_shared(inp):
    f = np.float32
    S = {}
    S["ada_w"] = np.ascontiguousarray(inp["ada_w"], dtype=f)
    S["ada_bT"] = np.ascontiguousarray(inp["ada_b"].reshape(4, 24, 128).transpose(2, 0, 1), dtype=f)
    S["norm_gT"] = np.ascontiguousarray(inp["norm_g"].reshape(4, 8, 128).transpose(2, 0, 1), dtype=f)
    S["final_gT"] = np.ascontiguousarray(inp["final_g"].reshape(8, 128).T, dtype=f)
    S["ident"] = np.eye(128, dtype=f)
    S["ones"] = np.ones((128, 128), f)
    S["j1"] = np.ascontiguousarray(np.broadcast_to(np.arange(1, TB + 1, dtype=f), (128, TB)))
    t = np.arange(128)[:, None]; s = np.arange(128)[None, :]
    S["maskF"] = np.where(s <= t, 0.0, -30000.0).astype(f)
    S["maskB"] = np.where(s >= t, 0.0, -30000.0).astype(f)
    S["triF"] = (t <= s).astype(f)
    S["triB"] = (t >= s).astype(f)
    for k in ("s5_w_in", "s5_w_glu", "s5_w_out", "ml_w_in", "ml_w_out"):
        S[k] = np.ascontiguousarray(inp[k], dtype=f)
    S["s5_b_gluT"] = np.ascontiguousarray(inp["s5_b_glu"].reshape(2, 8, 128).transpose(2, 0, 1), dtype=f)
    S["s5_dT"] = np.ascontiguousarray(inp["s5_d"].reshape(2, 8, 128).transpose(2, 0, 1), dtype=f)

    def st_layout(a):
        a = a.reshape(2, 2, 32, 2, 64)
        return np.ascontiguousarray(a.transpose(3, 4, 0, 1, 2).reshape(128, 2, 2, 32), dtype=f)
    S["s5_lre"] = st_layout(inp["s5_lam_re"])
    S["s5_lim"] = st_layout(inp["s5_lam_im"])
    S["s5_ldt"] = st_layout(np.broadcast_to(inp["s5_log_dt"][..., None], (2, 2, 64, 64)))

    def b_layout(bm):
        out = np.zeros((2, 2, 128, 32, 128), f)
        for st in range(32):
            pi = st % 4
            for gl in range(2):
                g = 2 * st + gl
                k0 = (2 * pi + gl) * 16
                out[:, :, k0:k0 + 16, st, gl * 64:(gl + 1) * 64] = bm[:, :, g].transpose(0, 1, 3, 2)
        return out.reshape(2, 2, 128, 32 * 128)

    def c_layout(cm):
        out = np.zeros((2, 2, 128, 32, 128), f)
        for st in range(32):
            pi = st % 4
            for gl in range(2):
                g = 2 * st + gl
                m0 = (2 * pi + gl) * 16
                out[:, :, gl * 64:(gl + 1) * 64, st, m0:m0 + 16] = cm[:, :, g].transpose(0, 1, 3, 2)
        return out.reshape(2, 2, 128, 32 * 128)
    S["s5_bre"] = b_layout(np.asarray(inp["s5_b_re"], f)); S["s5_bim"] = b_layout(np.asarray(inp["s5_b_im"], f))
    S["s5_cre"] = c_layout(np.asarray(inp["s5_c_re"], f)); S["s5_cim"] = c_layout(np.asarray(inp["s5_c_im"], f))
    S["ml_convT"] = np.ascontiguousarray(inp["ml_conv_w"].reshape(2, 5, 16, 128).transpose(3, 0, 2, 1), dtype=f)
    S["ml_convbT"] = np.ascontiguousarray(inp["ml_conv_b"].reshape(2, 16, 128).transpose(2, 0, 1), dtype=f)

    def bd_layout(w):
        out = np.zeros((2, 128, 16, 128), f)
        wr = np.asarray(w, f).reshape(2, 16, 32, 4, 4)
        for n in range(32):
            out[:, 4 * n:4 * n + 4, :, 4 * n:4 * n + 4] = wr[:, :, n].transpose(0, 2, 1, 3)
        return out.reshape(2, 128, 16 * 128)
    S["ml_wq_bd"] = bd_layout(inp["ml_w_q"]); S["ml_wk_bd"] = bd_layout(inp["ml_w_k"]); S["ml_wv_bd"] = bd_layout(inp["ml_w_v"])
    wg = np.asarray(inp["ml_w_gates"], f).reshape(2, 3, 16, 128, 16)
    S["ml_wg"] = np.ascontiguousarray(wg.transpose(0, 3, 1, 2, 4).reshape(2, 128, 768))
    S["ml_bg_rep"] = np.ascontiguousarray(np.broadcast_to(np.asarray(inp["ml_b_gates"], f)[None], (128, 2, 16)))
    S["ml_gnT"] = np.ascontiguousarray(inp["ml_gn_w"].reshape(2, 16, 128).transpose(2, 0, 1), dtype=f)
    S["ml_skipT"] = np.ascontiguousarray(inp["ml_skip"].reshape(2, 16, 128).transpose(2, 0, 1), dtype=f)
    return S


_NC_CACHE = {}


def kernel(**inputs):
    inp = {k: np.asarray(v) for k, v in inputs.items()}
    S = _prep_shared(inp)
    x = np.asarray(inp["x"], np.float32); c = np.asarray(inp["c"], np.float32)
    in_maps = []
    for core in range(NCORES):
        m = dict(S)
        m["x"] = np.ascontiguousarray(x[core * NB:(core + 1) * NB].reshape(NT, D))
        cc = c[core * NB:(core + 1) * NB]
        m["cT"] = np.ascontiguousarray(cc.reshape(NB, 8, 128).transpose(2, 1, 0))
        in_maps.append(m)
    if "nc" not in _NC_CACHE:
        _NC_CACHE["nc"] = build_program()
    res = run_bass_kernel_spmd(_NC_CACHE["nc"], in_maps, core_ids=list(range(NCORES)))
    out = np.concatenate([r["out"].reshape(NB, L, D) for r in res.results], axis=0)
    return out.astype(np.float32)
```

```python
import numpy as np
from contextlib import ExitStack
import concourse.bass as bass
import concourse.mybir as mybir
from concourse.ap import AP
from concourse.bass_utils import run_bass_kernel_spmd

F32 = mybir.dt.float32
BF16 = mybir.dt.bfloat16
I32 = mybir.dt.int32
ALU = mybir.AluOpType
AF = mybir.ActivationFunctionType
AX = mybir.AxisListType

NCORES = 8
D = 1024
L = 2048
NB = 2
NT = NB * L
TB = 512
NTB = NT // TB
SEM_LIMIT = 30000
N_DMA_SEMS = 12
TWO_PI = float(2 * np.pi)
SERIAL_DMA = False


class FW:
    ENGS = ("pe", "act", "dve", "pool", "sp")
    same_engine_sync = True

    def __init__(self, nc):
        self.nc = nc
        self.prog = {e: [] for e in self.ENGS}
        self.cur_sem = {}
        self.cnt = {}
        self.nsem = 0
        for e in self.ENGS:
            self._new_sem(e)
        self.waited = {}
        self.res = {}
        self.dma_sems = {}
        self.dma_rr = {}
        for e in ("sp", "act", "pool"):
            self.dma_sems[e] = [[self._alloc_sem(f"dma_{e}_{i}"), 0] for i in range(N_DMA_SEMS)]
            self.dma_rr[e] = 0
        self.n_inst = 0
        self.last_dma = {}

    def _alloc_sem(self, name):
        self.nsem += 1
        return self.nc.alloc_semaphore(name=f"{name}_{self.nsem}")

    def _new_sem(self, e):
        self.cur_sem[e] = self._alloc_sem(f"cnt_{e}")
        self.cnt[e] = 0

    def _need(self, eng, tok, waits):
        if tok is None:
            return
        sem, val, owner = tok
        if self.waited.get((eng, id(sem)), 0) >= val:
            return
        if owner == eng and (eng == "pe" or not self.same_engine_sync):
            return
        old = waits.get(id(sem))
        if old is None or old[1] < val:
            waits[id(sem)] = (sem, val)

    def _deps(self, eng, reads, writes):
        waits = {}
        for r in reads:
            ent = self.res.get(r)
            if ent is not None:
                self._need(eng, ent[0], waits)
        for w in writes:
            ent = self.res.get(w)
            if ent is not None:
                self._need(eng, ent[0], waits)
                for t in ent[1]:
                    self._need(eng, t, waits)
        return waits

    def _emit_waits(self, eng, waits):
        for sid, (sem, val) in waits.items():
            self.waited[(eng, sid)] = val
            self.prog[eng].append(lambda E, sem=sem, val=val: E.wait_ge(sem, val))
            self.n_inst += 1

    def _update(self, tok, reads, writes):
        for r in reads:
            ent = self.res.setdefault(r, [None, []])
            ent[1].append(tok)
            if len(ent[1]) > 48:
                best = {}
                for t in ent[1]:
                    k = id(t[0])
                    if k not in best or best[k][1] < t[1]:
                        best[k] = t
                ent[1] = list(best.values())
        for w in writes:
            self.res[w] = [tok, []]

    def op(self, eng, fn, reads=(), writes=()):
        reads = [r for r in reads if r is not None]
        writes = [w for w in writes if w is not None]
        waits = self._deps(eng, reads, writes)
        self._emit_waits(eng, waits)
        if self.cnt[eng] >= SEM_LIMIT:
            self._new_sem(eng)
        sem = self.cur_sem[eng]
        self.cnt[eng] += 1
        val = self.cnt[eng]
        self.prog[eng].append(lambda E, fn=fn, sem=sem: fn(E).then_inc(sem, 1))
        self.n_inst += 1
        tok = (sem, val, eng)
        self._update(tok, reads, writes)
        return tok

    def dma(self, q, out, in_, reads=(), writes=()):
        reads = [r for r in reads if r is not None]
        writes = [w for w in writes if w is not None]
        waits = self._deps(q, reads, writes)
        slot = self.dma_sems[q][self.dma_rr[q] % N_DMA_SEMS]
        self.dma_rr[q] += 1
        sem, used = slot
        if used > 0 and self.waited.get((q, id(sem)), 0) < used:
            old = waits.get(id(sem))
            if old is None or old[1] < used:
                waits[id(sem)] = (sem, used)
        if SERIAL_DMA and self.last_dma.get(q) is not None:
            ps_, pv_ = self.last_dma[q]
            if self.waited.get((q, id(ps_)), 0) < pv_:
                old = waits.get(id(ps_))
                if old is None or old[1] < pv_:
                    waits[id(ps_)] = (ps_, pv_)
        self._emit_waits(q, waits)
        slot[1] = used + 16
        self.last_dma[q] = (sem, slot[1])
        val = slot[1]
        self.prog[q].append(lambda E, out=out, in_=in_, sem=sem: E.dma_start(out=out, in_=in_).then_inc(sem, 16))
        self.n_inst += 1
        tok = (sem, val, "dma")
        self._update(tok, reads, writes)
        return tok

    def barrier(self):
        for eng in self.ENGS:
            waits = {}
            for other in self.ENGS:
                if other == eng or self.cnt[other] == 0:
                    continue
                sem = self.cur_sem[other]
                if self.waited.get((eng, id(sem)), 0) < self.cnt[other]:
                    waits[id(sem)] = (sem, self.cnt[other])
            for q in self.dma_sems:
                for sem, used in self.dma_sems[q]:
                    if used > 0 and self.waited.get((eng, id(sem)), 0) < used:
                        waits[id(sem)] = (sem, used)
            self._emit_waits(eng, waits)

    def finish(self, names, eng="sp"):
        waits = {}
        for n in names:
            ent = self.res.get(n)
            if ent is not None:
                self._need(eng, ent[0], waits)
        self._emit_waits(eng, waits)

    def run_block(self):
        nc = self.nc
        with nc.Block() as block:
            @block.tensor
            def _(E):
                for f in self.prog["pe"]:
                    f(E)

            @block.scalar
            def _(E):
                for f in self.prog["act"]:
                    f(E)

            @block.vector
            def _(E):
                for f in self.prog["dve"]:
                    f(E)

            @block.gpsimd
            def _(E):
                for f in self.prog["pool"]:
                    f(E)

            @block.sync
            def _(E):
                for f in self.prog["sp"]:
                    f(E)


def rev_ap(ap2d, start, n):
    a = list(ap2d.ap)
    return AP(ap2d.tensor, ap2d.offset + start * a[-1][0], [list(a[0]), [-a[-1][0], n]])


INPUT_SPECS = [
    ("x", [NT, D]), ("cT", [128, 8, NB]), ("ada_w", [4, D, 3 * D]), ("ada_bT", [128, 4, 24]),
    ("norm_gT", [128, 4, 8]), ("final_gT", [128, 8]), ("ident", [128, 128]), ("ones", [128, 128]),
    ("j1", [128, TB]), ("maskF", [128, 128]), ("maskB", [128, 128]), ("triF", [128, 128]), ("triB", [128, 128]),
    ("s5_w_in", [2, D, 2 * D]), ("s5_w_glu", [2, D, D]), ("s5_w_out", [2, D, D]),
    ("s5_b_gluT", [128, 2, 8]), ("s5_dT", [128, 2, 8]),
    ("s5_lre", [128, 2, 2, 32]), ("s5_lim", [128, 2, 2, 32]), ("s5_ldt", [128, 2, 2, 32]),
    ("s5_bre", [2, 2, 128, 32 * 128]), ("s5_bim", [2, 2, 128, 32 * 128]),
    ("s5_cre", [2, 2, 128, 32 * 128]), ("s5_cim", [2, 2, 128, 32 * 128]),
    ("ml_w_in", [2, D, 4 * D]), ("ml_w_out", [2, 2 * D, D]),
    ("ml_convT", [128, 2, 16, 5]), ("ml_convbT", [128, 2, 16]),
    ("ml_wq_bd", [2, 128, 16 * 128]), ("ml_wk_bd", [2, 128, 16 * 128]), ("ml_wv_bd", [2, 128, 16 * 128]),
    ("ml_wg", [2, 128, 3 * 16 * 16]), ("ml_bg_rep", [128, 2, 16]),
    ("ml_gnT", [128, 2, 16]), ("ml_skipT", [128, 2, 16]),
]


def build_program(dbg=(), dbg_stop=()):
    nc = bass.Bass("TRN2", target_bir_lowering=False)
    I = {}
    for name, shape in INPUT_SPECS:
        I[name] = nc.dram_tensor(name, shape, F32, kind="ExternalInput").ap()
    OUT = nc.dram_tensor("out", [NT, D], F32, kind="ExternalOutput").ap()

    def scratch(name, shape, dt):
        kind = "ExternalOutput" if name in dbg else "Internal"
        return nc.dram_tensor(name, shape, dt, kind=kind).ap()

    XT = scratch("XT", [8, 128, NT], F32)
    UT = scratch("UT", [8, 128, NT], BF16)
    SZT = scratch("SZT", [16, 128, NT], BF16)
    GT = scratch("GT", [8, 128, NT], BF16)
    XMT = scratch("XMT", [16, 128, NT], BF16)
    XCT = scratch("XCT", [16, 128, NT], BF16)
    QT = scratch("QT", [16, 128, NT], BF16)
    KT = scratch("KT", [16, 128, NT], BF16)
    KTOK = scratch("KTOK", [NT, 2 * D], BF16)
    VTOK = scratch("VTOK", [NT, 2 * D], BF16)
    HNT = scratch("HNT", [16, 128, NT], BF16)

    fw = FW(nc)
    rr = {"cast": 0, "ev": 0}

    with ExitStack() as top:
        def SB(es, name, shape, dt=F32):
            rr["sb"] = rr.get("sb", 0) + 1
            return es.enter_context(nc.sbuf_tensor(f"sb{rr['sb']}_{name}", shape, dt))

        PS = [top.enter_context(nc.psum_tensor(f"ps{i}", [128, 512], F32)) for i in range(8)]
        PSN = [f"ps{i}" for i in range(8)]

        ident = SB(top, "ident", [128, 128]); ones = SB(top, "ones", [128, 128])
        onesb = SB(top, "onesb", [128, 128], BF16)
        maskF = SB(top, "maskF", [128, 128]); maskB = SB(top, "maskB", [128, 128])
        triF = SB(top, "triF", [128, 128]); triB = SB(top, "triB", [128, 128])
        j1 = SB(top, "j1", [128, TB])
        MOD = SB(top, "MOD", [128, 4, 24, NB])
        S1 = SB(top, "S1", [128, 4, 8, NB])
        ngT = SB(top, "ngT", [128, 4, 8]); fgT = SB(top, "fgT", [128, 8])
        for t, n, rn in ((ident, "ident", "ident"), (ones, "ones", "ones"), (maskF, "maskF", "maskF"), (maskB, "maskB", "maskB"),
                         (triF, "triF", "triF"), (triB, "triB", "triB"), (j1, "j1", "j1"), (ngT, "norm_gT", "ngT"), (fgT, "final_gT", "fgT")):
            fw.dma("sp", t[:], I[n], writes=[rn])
        fw.op("dve", lambda E: E.tensor_copy(onesb[:], ones[:]), ["ones"], ["onesb"])

        def cast_eng():
            rr["cast"] += 1
            return ("dve", "pool", "act")[rr["cast"] % 3]

        def copy_op(eng, out, in_, r, w):
            if eng == "act":
                fw.op("act", lambda E: E.copy(out, in_), r, w)
            else:
                fw.op(eng, lambda E: E.tensor_copy(out, in_), r, w)

        def load_w_bf16(es, dst, dname, src, KC, N, tag):
            CH = min(N, 2048)
            stg = [SB(es, f"stg_{tag}_{i}", [128, CH]) for i in range(2)]
            k = 0
            for kc in range(KC):
                for n0 in range(0, N, CH):
                    s = stg[k % 2]; sn = f"stg_{tag}_{k % 2}"
                    fw.dma("sp", s[:], src[kc * 128:(kc + 1) * 128, n0:n0 + CH], writes=[sn])
                    copy_op(cast_eng(), dst[:, kc, n0:n0 + CH], s[:], [sn], [dname])
                    k += 1

        def phase_mod():
            with ExitStack() as es:
                cT = SB(es, "cT", [128, 8, NB]); sc = SB(es, "sc", [128, 8, NB]); abT = SB(es, "abT", [128, 4, 24])
                wt = [SB(es, f"adaw{i}", [128, 8, 128]) for i in range(2)]
                fw.dma("sp", cT[:], I["cT"], writes=["cT"])
                fw.dma("sp", abT[:], I["ada_bT"], writes=["abT"])
                fw.op("act", lambda E: E.activation(sc[:], cT[:], AF.Silu), ["cT"], ["sc"])
                k = 0
                for i in range(4):
                    for m in range(24):
                        w = wt[k % 2]; wn = f"adaw{k % 2}"
                        src = I["ada_w"][i].rearrange("(kc p) n -> p kc n", p=128)[:, :, m * 128:(m + 1) * 128]
                        fw.dma("sp" if k % 2 == 0 else "act", w[:], src, writes=[wn])
                        for kc in range(8):
                            fw.op("pe", lambda E, w=w, kc=kc: E.matmul(PS[0][:, 0:NB], w[:, kc, :], sc[:, kc, :],
                                                                       start=(kc == 0), stop=(kc == 7)), [wn, "sc"], ["ps0"])
                        fw.op("dve", lambda E, i=i, m=m: E.tensor_scalar(MOD[:, i, m, :], PS[0][:, 0:NB], abT[:, i, m:m + 1], None, ALU.add),
                              ["ps0", "abT"], ["MOD"])
                        k += 1
                for i in range(4):
                    for b in range(NB):
                        fw.op("dve", lambda E, i=i, b=b: E.scalar_tensor_tensor(S1[:, i, :, b], MOD[:, i, 8:16, b], 1.0, ngT[:, i, :], ALU.add, ALU.mult),
                              ["MOD", "ngT"], ["S1"])

        def phase_in():
            with ExitStack() as es:
                xi = [SB(es, f"xi{i}", [128, D]) for i in range(2)]
                xo = [SB(es, f"xo{i}", [128, 8, 128]) for i in range(2)]
                for tt in range(NT // 128):
                    a = xi[tt % 2]; an = f"xi{tt % 2}"; o = xo[tt % 2]; on = f"xo{tt % 2}"
                    fw.dma("sp", a[:], I["x"][tt * 128:(tt + 1) * 128, :], writes=[an])
                    for h in range(2):
                        p = PS[h]; pn = PSN[h]
                        for q in range(4):
                            kc = h * 4 + q
                            fw.op("pe", lambda E, p=p, q=q, kc=kc, a=a: E.transpose(p[:, q * 128:(q + 1) * 128], a[:, kc * 128:(kc + 1) * 128], ident[:]),
                                  [an, "ident"], [pn])
                        copy_op("dve" if h == 0 else "act", o[:, h * 4:(h + 1) * 4, :].rearrange("p a b -> p (a b)"), p[:], [pn], [on])
                    fw.dma("act", XT[:, :, tt * 128:(tt + 1) * 128].rearrange("k p t -> p k t"), o[:], reads=[on], writes=["XT"])

        def norm_block(XB, xbn, HB, hbn, tmps, li, b, final=False):
            sq, rs = tmps
            for kc in range(8):
                fw.op("act", lambda E, kc=kc: E.activation(sq[:, kc % 2, :], XB[:, kc, :], AF.Square), [xbn], [f"sq{kc % 2}"])
                fw.op("pe", lambda E, kc=kc: E.matmul(PS[7][:], ones[:], sq[:, kc % 2, :], start=(kc == 0), stop=(kc == 7)),
                      [f"sq{kc % 2}", "ones"], ["ps7"])
            fw.op("act", lambda E: E.activation(rs[:], PS[7][:], AF.Sqrt, bias=1e-6, scale=1.0 / D), ["ps7"], ["rs"])
            fw.op("dve", lambda E: E.reciprocal(rs[:], rs[:]), ["rs"], ["rs"])
            for kc in range(8):
                eng = "pool"
                if final:
                    fw.op("dve", lambda E, kc=kc: E.scalar_tensor_tensor(HB[:, kc, :], XB[:, kc, :], fgT[:, kc:kc + 1], rs[:], ALU.mult, ALU.mult),
                          [xbn, "rs", "fgT"], [hbn])
                else:
                    fw.op("dve", lambda E, kc=kc: E.scalar_tensor_tensor(sq[:, 2 + kc % 2, :], XB[:, kc, :], S1[:, li, kc, b:b + 1], rs[:], ALU.mult, ALU.mult),
                          [xbn, "rs", "S1"], [f"sq{2 + kc % 2}"])
                    fw.op(eng, lambda E, kc=kc: E.tensor_scalar(HB[:, kc, :], sq[:, 2 + kc % 2, :], MOD[:, li, kc, b:b + 1], None, ALU.add),
                          [f"sq{2 + kc % 2}", "MOD"], [hbn])

        def phase_inproj(li, w_src, NOUT, dst_a, dst_b):
            NM = NOUT // 128
            with ExitStack() as es:
                W = SB(es, "Win", [128, 8, NOUT], BF16)
                load_w_bf16(es, W, "Win", w_src, 8, NOUT, "win")
                XBs = [SB(es, f"XB{i}", [128, 8, TB]) for i in range(2)]
                HB = SB(es, "HB", [128, 8, TB], BF16)
                sq = SB(es, "sq", [128, 4, TB]); rs = SB(es, "rs", [128, TB])
                ob = [SB(es, f"ob{i}", [128, TB], BF16) for i in range(4)]
                fw.dma("sp", XBs[0][:], XT[:, :, 0:TB].rearrange("k p t -> p k t"), reads=["XT"], writes=["XB0"])
                for tb in range(NTB):
                    XB = XBs[tb % 2]; xbn = f"XB{tb % 2}"; b = tb // (NTB // NB)
                    if tb + 1 < NTB:
                        fw.dma("sp", XBs[(tb + 1) % 2][:], XT[:, :, (tb + 1) * TB:(tb + 2) * TB].rearrange("k p t -> p k t"),
                               reads=["XT"], writes=[f"XB{(tb + 1) % 2}"])
                    norm_block(XB, xbn, HB, "HB", (sq, rs), li, b)
                    for mt in range(NM):
                        p = PS[mt % 4]; pn = PSN[mt % 4]
                        for kc in range(8):
                            fw.op("pe", lambda E, p=p, kc=kc, mt=mt: E.matmul(p[:], W[:, kc, mt * 128:(mt + 1) * 128], HB[:, kc, :],
                                                                              start=(kc == 0), stop=(kc == 7)), ["Win", "HB"], [pn])
                        o = ob[mt % 4]; on = f"ob{mt % 4}"
                        if mt < NM // 2:
                            copy_op("dve" if mt % 2 == 0 else "act", o[:], p[:], [pn], [on])
                            fw.dma("act", dst_a[mt][:, tb * TB:(tb + 1) * TB], o[:], reads=[on], writes=["dst_a"])
                        else:
                            fw.op("act", lambda E, o=o, p=p: E.activation(o[:], p[:], AF.Silu), [pn], [on])
                            fw.dma("act", dst_b[mt - NM // 2][:, tb * TB:(tb + 1) * TB], o[:], reads=[on], writes=["dst_b"])

        YT = scratch("YT", [8, 128, NT], F32)

        def phase_s5_ssm(j):
            with ExitStack() as es:
                lre = SB(es, "lre", [128, 2, 32]); lim = SB(es, "lim", [128, 2, 32]); ldt = SB(es, "ldt", [128, 2, 32])
                fw.dma("sp", lre[:], I["s5_lre"][:, j], writes=["lre"])
                fw.dma("sp", lim[:], I["s5_lim"][:, j], writes=["lim"])
                fw.dma("sp", ldt[:], I["s5_ldt"][:, j], writes=["ldt"])
                dT = SB(es, "dT", [128, 8])
                fw.dma("sp", dT[:], I["s5_dT"][:, j], writes=["dT"])
                tn = ["dt", "th", "rmag", "ar", "ai", "k1", "k2", "k3", "den", "zre", "zim", "nzim", "t1s", "t2s"]
                T = {n: SB(es, "s5t_" + n, [128, 2, 32]) for n in tn}
                ki = SB(es, "s5t_ki", [128, 2, 32], I32)

                def sm(eng, fn, r, w):
                    fw.op(eng, fn, ["s5t_" + x if x in T else x for x in r], ["s5t_" + x if x in T else x for x in w])

                sm("act", lambda E: E.activation(T["dt"][:], ldt[:], AF.Exp), ["ldt"], ["dt"])
                sm("dve", lambda E: E.tensor_tensor(T["th"][:], lim[:], T["dt"][:], ALU.mult), ["lim", "dt"], ["th"])
                sm("dve", lambda E: E.tensor_tensor(T["k1"][:], lre[:], T["dt"][:], ALU.mult), ["lre", "dt"], ["k1"])
                sm("act", lambda E: E.activation(T["rmag"][:], T["k1"][:], AF.Exp), ["k1"], ["rmag"])
                for dst, sh in (("ai", 0.0), ("ar", float(np.pi / 2))):
                    sm("dve", lambda E, sh=sh: E.tensor_scalar(T["k2"][:], T["th"][:], sh, None, ALU.add), ["th"], ["k2"])
                    sm("dve", lambda E: E.tensor_scalar(ki[:], T["k2"][:], 1.0 / TWO_PI, None, ALU.mult), ["k2"], ["s5t_ki"])
                    sm("dve", lambda E: E.tensor_copy(T["k3"][:], ki[:]), ["s5t_ki"], ["k3"])
                    sm("dve", lambda E: E.scalar_tensor_tensor(T["k2"][:], T["k3"][:], -TWO_PI, T["k2"][:], ALU.mult, ALU.add), ["k3", "k2"], ["k2"])
                    sm("act", lambda E, dst=dst: E.activation(T[dst][:], T["k2"][:], AF.Sin), ["k2"], [dst])
                sm("dve", lambda E: E.tensor_tensor(T["ar"][:], T["ar"][:], T["rmag"][:], ALU.mult), ["ar", "rmag"], ["ar"])
                sm("dve", lambda E: E.tensor_tensor(T["ai"][:], T["ai"][:], T["rmag"][:], ALU.mult), ["ai", "rmag"], ["ai"])
                sm("dve", lambda E: E.tensor_scalar(T["k1"][:], T["ar"][:], -1.0, None, ALU.add), ["ar"], ["k1"])
                sm("dve", lambda E: E.tensor_tensor(T["den"][:], lre[:], lre[:], ALU.mult), ["lre"], ["den"])
                sm("dve", lambda E: E.tensor_tensor(T["k2"][:], lim[:], lim[:], ALU.mult), ["lim"], ["k2"])
                sm("dve", lambda E: E.tensor_tensor(T["den"][:], T["den"][:], T["k2"][:], ALU.add), ["den", "k2"], ["den"])
                sm("dve", lambda E: E.reciprocal(T["den"][:], T["den"][:]), ["den"], ["den"])
                sm("dve", lambda E: E.tensor_tensor(T["t1s"][:], T["k1"][:], lre[:], ALU.mult), ["k1", "lre"], ["t1s"])
                sm("dve", lambda E: E.tensor_tensor(T["t2s"][:], T["ai"][:], lim[:], ALU.mult), ["ai", "lim"], ["t2s"])
                sm("dve", lambda E: E.tensor_tensor(T["zre"][:], T["t1s"][:], T["t2s"][:], ALU.add), ["t1s", "t2s"], ["zre"])
                sm("dve", lambda E: E.tensor_tensor(T["zre"][:], T["zre"][:], T["den"][:], ALU.mult), ["zre", "den"], ["zre"])
                sm("dve", lambda E: E.tensor_tensor(T["t1s"][:], T["ai"][:], lre[:], ALU.mult), ["ai", "lre"], ["t1s"])
                sm("dve", lambda E: E.tensor_tensor(T["t2s"][:], T["k1"][:], lim[:], ALU.mult), ["k1", "lim"], ["t2s"])
                sm("dve", lambda E: E.tensor_tensor(T["zim"][:], T["t1s"][:], T["t2s"][:], ALU.subtract), ["t1s", "t2s"], ["zim"])
                sm("dve", lambda E: E.tensor_tensor(T["zim"][:], T["zim"][:], T["den"][:], ALU.mult), ["zim", "den"], ["zim"])
                sm("dve", lambda E: E.tensor_scalar(T["nzim"][:], T["zim"][:], -1.0, None, ALU.mult), ["zim"], ["nzim"])

                B4 = SB(es, "B4", [128, 2, 4 * 128], BF16)
                C4 = SB(es, "C4", [128, 2, 4 * 128], BF16)
                stg = [SB(es, f"s5stg{i}", [128, 4 * 128]) for i in range(4)]
                tC = SB(es, "tC", [128, 128])
                cosT = SB(es, "cosT", [128, 4, TB]); sinT = SB(es, "sinT", [128, 4, TB]); RM = SB(es, "RM", [128, 4, TB])
                ph = SB(es, "ph", [128, TB]); phk = SB(es, "phk", [128, TB]); phi = SB(es, "phi", [128, TB], I32)
                Us = [SB(es, f"Uc{i}", [128, NT], BF16) for i in range(2)]
                YACC = SB(es, "YACC", [128, NT])
                Gc = SB(es, "Gc", [128, NT], BF16)
                tmps = [{n: SB(es, f"w{q}_{n}", [128, TB]) for n in ("t1", "t2", "t3", "t4", "pre", "pim", "sre", "sim")} for q in range(3)]
                u2 = [SB(es, f"u2_{q}", [128, 2, TB]) for q in range(3)]
                sbf = [SB(es, f"sbf{q}", [128, 2, TB], BF16) for q in range(3)]
                carry = SB(es, "carry", [128, 4, NB, 4])
                it = 0
                for d in range(2):
                    for cc in range(8):
                        U = Us[it % 2]; un = f"Uc{it % 2}"
                        fw.dma("sp", U[:], UT[cc], reads=["dst_a"], writes=[un])
                        for q, key in enumerate(("s5_bre", "s5_bim", "s5_cre", "s5_cim")):
                            fw.dma("sp", stg[q][:], I[key][j, d][:, cc * 512:(cc + 1) * 512], writes=[f"s5stg{q}"])
                        fw.op("act", lambda E: E.copy(B4[:, 0, :], stg[0][:]), ["s5stg0"], ["B4"])
                        fw.op("act", lambda E: E.copy(B4[:, 1, :], stg[1][:]), ["s5stg1"], ["B4"])
                        for pi in range(4):
                            st = 4 * cc + pi
                            sl = slice(pi * 128, (pi + 1) * 128)
                            zr = T["zre"][:, d, st:st + 1]; zi = T["zim"][:, d, st:st + 1]; nzi = T["nzim"][:, d, st:st + 1]
                            fw.op("dve", lambda E, sl=sl, zi=zi: E.tensor_scalar(tC[:], stg[3][:, sl], zi, None, ALU.mult), ["s5stg3", "s5t_zim"], ["tC"])
                            fw.op("dve", lambda E, sl=sl, zr=zr: E.scalar_tensor_tensor(C4[:, 0, sl], stg[2][:, sl], zr, tC[:], ALU.mult, ALU.subtract),
                                  ["s5stg2", "s5t_zre", "tC"], ["C4"])
                            fw.op("dve", lambda E, sl=sl, zr=zr: E.tensor_scalar(tC[:], stg[3][:, sl], zr, None, ALU.mult), ["s5stg3", "s5t_zre"], ["tC"])
                            fw.op("dve", lambda E, sl=sl, nzi=nzi: E.scalar_tensor_tensor(C4[:, 1, sl], stg[2][:, sl], nzi, tC[:], ALU.mult, ALU.subtract),
                                  ["s5stg2", "s5t_nzim", "tC"], ["C4"])
                            th = T["th"][:, d, st:st + 1]
                            fw.op("pool", lambda E, th=th: E.tensor_scalar(ph[:], j1[:], th, None, ALU.mult), ["j1", "s5t_th"], ["ph"])
                            for dstT, dn, sh in ((sinT, "sinT", 0.0), (cosT, "cosT", float(np.pi / 2))):
                                fw.op("pool", lambda E, sh=sh: E.tensor_scalar(phk[:], ph[:], sh, None, ALU.add), ["ph"], ["phk"])
                                fw.op("pool", lambda E: E.tensor_scalar(phi[:], phk[:], 1.0 / TWO_PI, None, ALU.mult), ["phk"], ["phi"])
                                fw.op("pool", lambda E: E.tensor_copy(ph[:] if False else tmps[0]["t1"][:], phi[:]), ["phi"], ["w0_t1"])
                                fw.op("dve", lambda E: E.scalar_tensor_tensor(phk[:], tmps[0]["t1"][:], -TWO_PI, phk[:], ALU.mult, ALU.add), ["w0_t1", "phk"], ["phk"])
                                fw.op("act", lambda E, dstT=dstT, pi=pi: E.activation(dstT[:, pi, :], phk[:], AF.Sin), ["phk"], [dn])
                            rm = T["rmag"][:, d, st:st + 1]
                            fw.op("pool", lambda E, pi=pi, rm=rm: E.tensor_scalar(RM[:, pi, :], j1[:], 0.0, rm, ALU.mult, ALU.add), ["j1", "s5t_rmag"], ["RM"])
                        fw.op("pool", lambda E: E.memset(carry[:], 0.0), [], [f"carry{p_}_{b_}" for p_ in range(4) for b_ in range(NB)])
                        if d == 0:
                            fw.op("pool", lambda E, U=U, cc=cc: E.tensor_scalar(YACC[:], U[:], dT[:, cc:cc + 1], None, ALU.mult), [un, "dT"], ["YACC"])
                        else:
                            fw.dma("sp", YACC[:], YT[cc], reads=["YT"], writes=["YACC"])
                        k = 0
                        for b in range(NB):
                            for chi in range(4):
                                ch = chi if d == 0 else 3 - chi
                                T0 = b * L + ch * TB
                                if d == 0:
                                    rhs = U[:, T0:T0 + TB]; yv = YACC[:, T0:T0 + TB]
                                else:
                                    rhs = rev_ap(U[:], T0 + TB - 1, TB); yv = rev_ap(YACC[:], T0 + TB - 1, TB)
                                py = PS[6 + (k // 4) % 2]; pyn = PSN[6 + (k // 4) % 2]
                                for pi in range(4):
                                    q = k % 3
                                    W = tmps[q]; wn = lambda n, q=q: f"w{q}_{n}"
                                    pa = PS[2 * q]; pan = PSN[2 * q]; pb = PS[2 * q + 1]; pbn = PSN[2 * q + 1]
                                    sl = slice(pi * 128, (pi + 1) * 128)
                                    fw.op("pe", lambda E, pa=pa, sl=sl, rhs=rhs: E.matmul(pa[:], B4[:, 0, sl], rhs, start=True, stop=True), ["B4", un], [pan])
                                    fw.op("pe", lambda E, pb=pb, sl=sl, rhs=rhs: E.matmul(pb[:], B4[:, 1, sl], rhs, start=True, stop=True), ["B4", un], [pbn])
                                    cs = cosT[:, pi, :]; sn = sinT[:, pi, :]
                                    fw.op("dve", lambda E, W=W, pa=pa, cs=cs: E.tensor_tensor(W["t1"][:], pa[:], cs, ALU.mult), [pan, "cosT"], [wn("t1")])
                                    fw.op("dve", lambda E, W=W, pb=pb, sn=sn: E.tensor_tensor(W["t2"][:], pb[:], sn, ALU.mult), [pbn, "sinT"], [wn("t2")])
                                    fw.op("dve", lambda E, W=W, pb=pb, cs=cs: E.tensor_tensor(W["t3"][:], pb[:], cs, ALU.mult), [pbn, "cosT"], [wn("t3")])
                                    fw.op("dve", lambda E, W=W, pa=pa, sn=sn: E.tensor_tensor(W["t4"][:], pa[:], sn, ALU.mult), [pan, "sinT"], [wn("t4")])
                                    fw.op("dve", lambda E, W=W: E.tensor_tensor(W["pre"][:], W["t1"][:], W["t2"][:], ALU.add), [wn("t1"), wn("t2")], [wn("pre")])
                                    fw.op("dve", lambda E, W=W: E.tensor_tensor(W["pim"][:], W["t3"][:], W["t4"][:], ALU.subtract), [wn("t3"), wn("t4")], [wn("pim")])
                                    fw.op("dve", lambda E, W=W, pi=pi, b=b: E.tensor_tensor_scan(W["sre"][:], RM[:, pi, :], W["pre"][:], carry[:, pi, b, 0:1], ALU.mult, ALU.add),
                                          ["RM", wn("pre"), f"carry{pi}_{b}"], [wn("sre")])
                                    fw.op("dve", lambda E, W=W, pi=pi, b=b: E.tensor_tensor_scan(W["sim"][:], RM[:, pi, :], W["pim"][:], carry[:, pi, b, 1:2], ALU.mult, ALU.add),
                                          ["RM", wn("pim"), f"carry{pi}_{b}"], [wn("sim")])
                                    fw.op("pool", lambda E, W=W, cs=cs: E.tensor_tensor(W["t1"][:], W["sre"][:], cs, ALU.mult), [wn("sre"), "cosT"], [wn("t1")])
                                    fw.op("pool", lambda E, W=W, sn=sn: E.tensor_tensor(W["t2"][:], W["sim"][:], sn, ALU.mult), [wn("sim"), "sinT"], [wn("t2")])
                                    fw.op("pool", lambda E, W=W, sn=sn: E.tensor_tensor(W["t3"][:], W["sre"][:], sn, ALU.mult), [wn("sre"), "sinT"], [wn("t3")])
                                    fw.op("pool", lambda E, W=W, cs=cs: E.tensor_tensor(W["t4"][:], W["sim"][:], cs, ALU.mult), [wn("sim"), "cosT"], [wn("t4")])
                                    uu = u2[q]; uun = f"u2_{q}"
                                    fw.op("dve", lambda E, W=W, uu=uu: E.tensor_tensor(uu[:, 0, :], W["t1"][:], W["t2"][:], ALU.subtract), [wn("t1"), wn("t2")], [uun])
                                    fw.op("dve", lambda E, W=W, uu=uu: E.tensor_tensor(uu[:, 1, :], W["t3"][:], W["t4"][:], ALU.add), [wn("t3"), wn("t4")], [uun])
                                    sb_ = sbf[q]; sbn = f"sbf{q}"
                                    fw.op("act", lambda E, sb_=sb_, uu=uu: E.copy(sb_[:], uu[:]), [uun], [sbn])
                                    fw.op("pool", lambda E, uu=uu, pi=pi, b=b: E.tensor_copy(carry[:, pi, b, 0:2], uu[:, :, TB - 1]), [uun], [f"carry{pi}_{b}"])
                                    fw.op("pe", lambda E, py=py, sl=sl, sb_=sb_, pi=pi: E.matmul(py[:], C4[:, 0, sl], sb_[:, 0, :], start=(pi == 0), stop=False), ["C4", sbn], [pyn])
                                    fw.op("pe", lambda E, py=py, sl=sl, sb_=sb_, pi=pi: E.matmul(py[:], C4[:, 1, sl], sb_[:, 1, :], start=False, stop=(pi == 3)), ["C4", sbn], [pyn])
                                    k += 1
                                fw.op("dve", lambda E, py=py, yv=yv: E.tensor_tensor(yv, py[:], yv, ALU.add), [pyn, "YACC"], ["YACC"])
                                if "CARRYD" in dbg and d == 0 and cc == 0:
                                    CD2 = nc.dram_tensor(f"CARRYD_b{b}c{chi}", [128, 32], F32, kind="ExternalOutput").ap()
                                    fw.dma("sp", CD2, carry[:].rearrange("p a b c -> p (a b c)"), reads=["carry"], writes=["CARRYD"])
                                    if b == 1 and chi == 1:
                                        UD3 = nc.dram_tensor("CARRYD_U", [128, NT], BF16, kind="ExternalOutput").ap()
                                        fw.dma("sp", UD3, U[:], reads=[un], writes=["CARRYD"])
                                        for nm in ("t1", "pre", "sre"):
                                            UD4 = nc.dram_tensor("CARRYD_" + nm, [128, TB], F32, kind="ExternalOutput").ap()
                                            fw.dma("sp", UD4, tmps[(k - 1) % 3][nm][:], reads=[f"w{(k - 1) % 3}_{nm}"], writes=["CARRYD"])
                                        UD5 = nc.dram_tensor("CARRYD_cos", [128, 4 * TB], F32, kind="ExternalOutput").ap()
                                        fw.dma("sp", UD5, cosT[:].rearrange("p a b -> p (a b)"), reads=["cosT"], writes=["CARRYD"])
                                        UD6 = nc.dram_tensor("CARRYD_RM", [128, 4 * TB], F32, kind="ExternalOutput").ap()
                                        fw.dma("sp", UD6, RM[:].rearrange("p a b -> p (a b)"), reads=["RM"], writes=["CARRYD"])
                                    UD2 = nc.dram_tensor(f"CARRYDU_b{b}c{chi}", [128, 2 * TB], F32, kind="ExternalOutput").ap()
                                    fw.dma("sp", UD2, u2[(k - 1) % 3][:].rearrange("p a b -> p (a b)"), reads=[f"u2_{(k - 1) % 3}"], writes=["CARRYD"])
                        if "CARRYD" in dbg and d == 0 and cc < 2:
                            CD = nc.dram_tensor(f"CARRYD{cc}", [128, 32], F32, kind="ExternalOutput").ap()
                            fw.dma("sp", CD, carry[:].rearrange("p a b c -> p (a b c)"), reads=["carry"], writes=["CARRYD"])
                        if d == 0:
                            fw.dma("sp", YT[cc], YACC[:], reads=["YACC"], writes=["YT"])
                        else:
                            fw.op("act", lambda E: E.activation(Gc[:], YACC[:], AF.Gelu), ["YACC"], ["Gc"])
                            fw.dma("act", GT[cc], Gc[:], reads=["Gc"], writes=["GT"])
                        it += 1

        def phase_s5_out(j, li):
            with ExitStack() as es:
                Wg = SB(es, "Wg", [128, 8, D], BF16); Wo = SB(es, "Wo", [128, 8, D], BF16)
                load_w_bf16(es, Wg, "Wg", I["s5_w_glu"][j], 8, D, "wg")
                load_w_bf16(es, Wo, "Wo", I["s5_w_out"][j], 8, D, "wo")
                bg = SB(es, "bg", [128, 8])
                fw.dma("sp", bg[:], I["s5_b_gluT"][:, j], writes=["bg"])
                G = SB(es, "Gb", [128, 8, TB], BF16); SZ = SB(es, "SZb", [128, 8, TB], BF16); XB = SB(es, "XBo", [128, 8, TB])
                Y2 = SB(es, "Y2", [128, 8, TB], BF16)
                sg = [SB(es, f"sg{i}", [128, TB]) for i in range(2)]
                for tb in range(NTB):
                    b = tb // (NTB // NB); ts_ = slice(tb * TB, (tb + 1) * TB)
                    fw.dma("sp", G[:], GT[:, :, ts_].rearrange("k p t -> p k t"), reads=["GT"], writes=["Gb"])
                    fw.dma("sp", SZ[:], SZT[0:8, :, ts_].rearrange("k p t -> p k t"), reads=["dst_b"], writes=["SZb"])
                    fw.dma("sp", XB[:], XT[:, :, ts_].rearrange("k p t -> p k t"), reads=["XT"], writes=["XBo"])
                    for mt in range(8):
                        p = PS[mt % 4]; pn = PSN[mt % 4]; s_ = sg[mt % 2]; sn = f"sg{mt % 2}"
                        for kc in range(8):
                            fw.op("pe", lambda E, p=p, kc=kc, mt=mt: E.matmul(p[:], Wg[:, kc, mt * 128:(mt + 1) * 128], G[:, kc, :], start=(kc == 0), stop=(kc == 7)),
                                  ["Wg", "Gb"], [pn])
                        fw.op("act", lambda E, p=p, s_=s_, mt=mt: E.activation(s_[:], p[:], AF.Sigmoid, bias=bg[:, mt:mt + 1]), [pn, "bg"], [sn])
                        fw.op("dve", lambda E, s_=s_, mt=mt: E.tensor_tensor(s_[:], s_[:], G[:, mt, :], ALU.mult), [sn, "Gb"], [sn])
                        fw.op("pool", lambda E, s_=s_, mt=mt: E.tensor_tensor(Y2[:, mt, :], s_[:], SZ[:, mt, :], ALU.mult), [sn, "SZb"], ["Y2"])
                    for mt in range(8):
                        p = PS[4 + mt % 4]; pn = PSN[4 + mt % 4]
                        for kc in range(8):
                            fw.op("pe", lambda E, p=p, kc=kc, mt=mt: E.matmul(p[:], Wo[:, kc, mt * 128:(mt + 1) * 128], Y2[:, kc, :], start=(kc == 0), stop=(kc == 7)),
                                  ["Wo", "Y2"], [pn])
                        fw.op("dve", lambda E, p=p, mt=mt, b=b: E.scalar_tensor_tensor(XB[:, mt, :], p[:], MOD[:, li, 16 + mt, b:b + 1], XB[:, mt, :], ALU.mult, ALU.add),
                              [pn, "MOD", "XBo"], ["XBo"])
                    fw.dma("act", XT[:, :, ts_].rearrange("k p t -> p k t"), XB[:], reads=["XBo"], writes=["XT"])

        def phase_ml_qkv(j, GATES):
            with ExitStack() as es:
                cw = SB(es, "cw", [128, 16, 5]); cb = SB(es, "cb", [128, 16]); bgr = SB(es, "bgr", [128, 16])
                fw.dma("sp", cw[:], I["ml_convT"][:, j], writes=["cw"])
                fw.dma("sp", cb[:], I["ml_convbT"][:, j], writes=["cb"])
                fw.dma("sp", bgr[:], I["ml_bg_rep"][:, j], writes=["bgr"])
                Wq = SB(es, "Wq", [128, 1, 2048], BF16); Wk = SB(es, "Wk", [128, 1, 2048], BF16); Wv = SB(es, "Wv", [128, 1, 2048], BF16)
                load_w_bf16(es, Wq, "Wq", I["ml_wq_bd"][j], 1, 2048, "wq")
                load_w_bf16(es, Wk, "Wk", I["ml_wk_bd"][j], 1, 2048, "wk")
                load_w_bf16(es, Wv, "Wv", I["ml_wv_bd"][j], 1, 2048, "wv")
                wgs = SB(es, "wgs", [128, 768]); wg = SB(es, "wgb", [128, 768], BF16)
                fw.dma("sp", wgs[:], I["ml_wg"][j], writes=["wgs"])
                fw.op("dve", lambda E: E.tensor_copy(wg[:], wgs[:]), ["wgs"], ["wgb"])
                XMH = [SB(es, f"XMH{i}", [128, L + 4], BF16) for i in range(2)]
                for i in range(2):
                    fw.op("pool", lambda E, i=i: E.memset(XMH[i][:], 0.0), [], [f"XMH{i}"])
                acc = SB(es, "cacc", [128, L]); XC = SB(es, "XCc", [128, L], BF16)
                QTB = SB(es, "QTB", [128, L], BF16); KTB = SB(es, "KTB", [128, L], BF16); VTB = SB(es, "VTB", [128, L], BF16)
                KTK = SB(es, "KTK", [128, 16, 128], BF16); VTK = SB(es, "VTK", [128, 16, 128], BF16)
                it = 0
                for cc in range(16):
                    csl = slice(cc * 128, (cc + 1) * 128)
                    for b in range(NB):
                        xm = XMH[it % 2]; xn = f"XMH{it % 2}"; bs = slice(b * L, (b + 1) * L)
                        fw.dma("sp", xm[:, 2:2 + L], XMT[cc][:, bs], reads=["dst_a"], writes=[xn])
                        ce = "dve"
                        fw.op(ce, lambda E, xm=xm, cc=cc: E.tensor_scalar(acc[:], xm[:, 0:L], cw[:, cc, 0:1], cb[:, cc:cc + 1], ALU.mult, ALU.add), [xn, "cw", "cb"], ["cacc"])
                        for kk in range(1, 5):
                            fw.op(ce, lambda E, xm=xm, cc=cc, kk=kk: E.scalar_tensor_tensor(acc[:], xm[:, kk:kk + L], cw[:, cc, kk:kk + 1], acc[:], ALU.mult, ALU.add),
                                  [xn, "cw", "cacc"], ["cacc"])
                        fw.op("act", lambda E: E.activation(XC[:], acc[:], AF.Silu), ["cacc"], ["XCc"])
                        fw.dma("act", XCT[cc][:, bs], XC[:], reads=["XCc"], writes=["XCT"])
                        for q4 in range(4):
                            qs = slice(q4 * TB, (q4 + 1) * TB)
                            for wi, (Wm, wn, src, srn, dst, dn) in enumerate(((Wq, "Wq", XC[:, qs], "XCc", QTB, "QTB"), (Wk, "Wk", XC[:, qs], "XCc", KTB, "KTB"),
                                                                           (Wv, "Wv", xm[:, 2 + q4 * TB:2 + (q4 + 1) * TB], xn, VTB, "VTB"))):
                                p = PS[wi]; pn = PSN[wi]
                                fw.op("pe", lambda E, p=p, Wm=Wm, src=src, csl=csl: E.matmul(p[:], Wm[:, 0, csl], src, start=True, stop=True), [wn, srn], [pn])
                                copy_op("dve" if wi != 1 else "act", dst[:, qs], p[:], [pn], [dn])
                        fw.dma("act", QT[cc][:, bs], QTB[:], reads=["QTB"], writes=["QT"])
                        fw.dma("act", KT[cc][:, bs], KTB[:], reads=["KTB"], writes=["KT"])
                        for t4 in range(4):
                            pk = PS[3]; pv = PS[4]
                            for tq in range(4):
                                tt = t4 * 4 + tq; tsl = slice(tt * 128, (tt + 1) * 128); osl = slice(tq * 128, (tq + 1) * 128)
                                fw.op("pe", lambda E, tsl=tsl, osl=osl, csl=csl: E.matmul(pk[:, osl], XC[:, tsl], Wk[:, 0, csl], start=True, stop=True), ["XCc", "Wk"], ["ps3"])
                                fw.op("pe", lambda E, tsl=tsl, osl=osl, xm=xm, tt=tt, csl=csl: E.matmul(pv[:, osl], xm[:, 2 + tt * 128:2 + (tt + 1) * 128], Wv[:, 0, csl], start=True, stop=True),
                                      [xn, "Wv"], ["ps4"])
                                gsl = slice((b * 16 + tt) * 16, (b * 16 + tt + 1) * 16)
                                for wi, (src, srn) in enumerate(((QTB, "QTB"), (KTB, "KTB"), (VTB, "VTB"))):
                                    fw.op("pe", lambda E, src=src, tsl=tsl, gsl=gsl, wi=wi, cc=cc, b=b, tt=tt: E.matmul(
                                        PS[6][:, gsl], src[:, tsl], wg[:, (wi * 16 + cc) * 16:(wi * 16 + cc + 1) * 16],
                                        start=(cc == 0 and wi == 0 and b == 0 and tt == 0), stop=(cc == 15 and wi == 2)), [srn, "wgb"], ["ps6"])
                            copy_op("dve", KTK[:, t4 * 4:(t4 + 1) * 4, :].rearrange("p a b -> p (a b)"), pk[:], ["ps3"], ["KTK"])
                            copy_op("act", VTK[:, t4 * 4:(t4 + 1) * 4, :].rearrange("p a b -> p (a b)"), pv[:], ["ps4"], ["VTK"])
                        fw.dma("act", KTOK[bs, csl].rearrange("(t p) c -> p t c", p=128), KTK[:], reads=["KTK"], writes=["KTOK"])
                        fw.dma("act", VTOK[bs, csl].rearrange("(t p) c -> p t c", p=128), VTK[:], reads=["VTK"], writes=["VTOK"])
                        it += 1
                fw.op("dve", lambda E: E.tensor_tensor(GATES[:], PS[6][:].rearrange("p (a b) -> p a b", b=16),
                                                       bgr[:].unsqueeze(1).to_broadcast([128, 32, 16]), ALU.add), ["ps6", "bgr"], ["GATES"])

        def phase_ml_attn(j, GATES):
            SC = float(512 ** -0.5)
            with ExitStack() as es:
                def G3(name, last=16, dt=F32):
                    return SB(es, name, [128, 32, last], dt)
                E1 = G3("E1"); LF = G3("LF"); BWF = G3("BWF"); BWB = G3("BWB"); TOT = G3("TOT"); OFF = G3("OFF")
                fw.op("act", lambda E: E.activation(E1[:], GATES[:], AF.Exp, scale=-1.0), ["GATES"], ["E1"])
                fw.op("act", lambda E: E.activation(E1[:], E1[:], AF.Ln, bias=1.0), ["E1"], ["E1"])
                fw.op("dve", lambda E: E.tensor_scalar(LF[:], E1[:], -1.0, None, ALU.mult), ["E1"], ["LF"])
                LFf = LF[:].rearrange("p a b -> p (a b)")
                fw.op("pe", lambda E: E.matmul(PS[0][:], triF[:], LFf, start=True, stop=True), ["triF", "LF"], ["ps0"])
                fw.op("pe", lambda E: E.matmul(PS[1][:], triB[:], LFf, start=True, stop=True), ["triB", "LF"], ["ps1"])
                fw.op("pe", lambda E: E.matmul(PS[2][:], ones[:], LFf, start=True, stop=True), ["ones", "LF"], ["ps2"])
                fw.op("dve", lambda E: E.tensor_copy(BWF[:].rearrange("p a b -> p (a b)"), PS[0][:]), ["ps0"], ["BWF"])
                fw.op("act", lambda E: E.copy(BWB[:].rearrange("p a b -> p (a b)"), PS[1][:]), ["ps1"], ["BWB"])
                fw.op("dve", lambda E: E.tensor_copy(TOT[:].rearrange("p a b -> p (a b)"), PS[2][:]), ["ps2"], ["TOT"])
                for (BW, bwn, fwd) in ((BWF, "BWF", True), (BWB, "BWB", False)):
                    fw.op("pool", lambda E: E.memset(OFF[:], 0.0), [], ["OFF"])
                    for b in range(NB):
                        rng = range(1, 16) if fwd else range(14, -1, -1)
                        for kt in rng:
                            g = b * 16 + kt; gp = g - 1 if fwd else g + 1
                            fw.op("pool", lambda E, g=g, gp=gp: E.tensor_tensor(OFF[:, g, :], OFF[:, gp, :], TOT[:, gp, :], ALU.add), ["OFF", "TOT"], ["OFF"])
                    fw.op("pool", lambda E, BW=BW: E.tensor_tensor(BW[:], BW[:], OFF[:], ALU.add), [bwn, "OFF"], [bwn])
                AALL = SB(es, "AALL", [128, 2, 32, 4]); CM = SB(es, "CM", [128, 2, 32, 4]); TM = SB(es, "TM", [128, 2, 32, 4])
                PM = SB(es, "PM", [128, 2, 32, 4]); MM = SB(es, "MM", [128, 2, 32, 4]); EM = SB(es, "EM", [128, 2, 32, 4])
                fw.op("dve", lambda E: E.tensor_tensor(AALL[:, 0], GATES[:, :, 0:4], BWF[:, :, 4:8], ALU.subtract), ["GATES", "BWF"], ["AALL"])
                fw.op("dve", lambda E: E.tensor_tensor(AALL[:, 1], GATES[:, :, 8:12], BWB[:, :, 12:16], ALU.subtract), ["GATES", "BWB"], ["AALL"])
                DG = [SB(es, f"DG{i}", [128, 4, 128]) for i in range(2)]
                AM = [SB(es, f"AM{i}", [128, 4, 128]) for i in range(2)]
                identb = ident[:].unsqueeze(1).to_broadcast([128, 4, 128])
                k = 0
                for d in range(2):
                    mk = maskF if d == 0 else maskB
                    mkn = "maskF" if d == 0 else "maskB"
                    for g in range(32):
                        q = k % 2; dg = DG[q]; dgn = f"DG{q}"; am = AM[q]; amn = f"AM{q}"; p = PS[q]; pn = PSN[q]
                        fw.op("pool", lambda E, dg=dg, d=d, g=g: E.tensor_tensor(dg[:], identb, AALL[:, d, g, :].unsqueeze(2).to_broadcast([128, 4, 128]), ALU.mult),
                              ["ident", "AALL"], [dgn])
                        fw.op("pe", lambda E, p=p, dg=dg: E.matmul(p[:], ones[:], dg[:].rearrange("p a b -> p (a b)"), start=True, stop=True), ["ones", dgn], [pn])
                        p3 = p[:].rearrange("p (a b) -> p a b", b=128)
                        fw.op("dve", lambda E, am=am, p3=p3, mk=mk: E.tensor_tensor(am[:], p3, mk[:].unsqueeze(1).to_broadcast([128, 4, 128]), ALU.add), [pn, mkn], [amn])
                        fw.op("dve", lambda E, am=am, d=d, g=g: E.tensor_reduce(CM[:, d, g, :], am[:], AX.X, ALU.max), [amn], ["CM"])
                        fw.op("dve", lambda E, p3=p3, d=d, g=g: E.tensor_reduce(TM[:, d, g, :], p3, AX.X, ALU.max), [pn], ["TM"])
                        k += 1
                fw.op("pool", lambda E: E.memset(PM[:], 0.0), [], ["PM"])
                for d in range(2):
                    for b in range(NB):
                        rng = range(1, 16) if d == 0 else range(14, -1, -1)
                        for kt in rng:
                            g = b * 16 + kt; gp = g - 1 if d == 0 else g + 1
                            fw.op("dve", lambda E, d=d, g=g, gp=gp: E.tensor_tensor(PM[:, d, g, :], PM[:, d, gp, :], TM[:, d, gp, :], ALU.max), ["PM", "TM"], ["PM"])
                fw.op("dve", lambda E: E.tensor_tensor(MM[:], CM[:], PM[:], ALU.max), ["CM", "PM"], ["MM"])
                fw.op("dve", lambda E: E.tensor_tensor(EM[:, 0], MM[:, 0], BWF[:, :, 4:8], ALU.add), ["MM", "BWF"], ["EM"])
                fw.op("dve", lambda E: E.tensor_tensor(EM[:, 1], MM[:, 1], BWB[:, :, 12:16], ALU.add), ["MM", "BWB"], ["EM"])
                fw.op("act", lambda E: E.activation(EM[:], EM[:], AF.Exp, scale=-1.0), ["EM"], ["EM"])

                QH = SB(es, "QH", [128, 4, L], BF16); KH = SB(es, "KH", [128, 4, L], BF16); VH = SB(es, "VH", [128, 16, 512], BF16)
                HACC = SB(es, "HACC", [128, 16, 512]); MBt = SB(es, "MBt", [128, L]); HNF = SB(es, "HNF", [128, 4, L], BF16)
                WT = [SB(es, f"WT{i}", [128, TB]) for i in range(2)]
                PT = [SB(es, f"PT{i}", [128, TB], BF16) for i in range(2)]
                sm4 = SB(es, "sm4", [128, 8, 4]); st6 = SB(es, "st6", [128, 6]); mv = SB(es, "mv", [128, 2]); hn = SB(es, "hnt", [128, 512])
                for b in range(NB):
                    bs = slice(b * L, (b + 1) * L)
                    for hd in range(4):
                        fw.dma("sp", QH[:], QT[hd * 4:(hd + 1) * 4, :, bs].rearrange("k p t -> p k t"), reads=["QT"], writes=["QH"])
                        fw.dma("sp", KH[:], KT[hd * 4:(hd + 1) * 4, :, bs].rearrange("k p t -> p k t"), reads=["KT"], writes=["KH"])
                        fw.dma("sp", VH[:], VTOK[bs, hd * 512:(hd + 1) * 512].rearrange("(t p) e -> p t e", p=128), reads=["VTOK"], writes=["VH"])
                        for d in range(2):
                            tmask = triF if d == 0 else triB
                            tmn = "triF" if d == 0 else "triB"
                            for q4 in range(4):
                                dg = DG[q4 % 2]; dgn = f"DG{q4 % 2}"
                                g0 = b * 16 + q4 * 4
                                fw.op("pool", lambda E, dg=dg, d=d, g0=g0, hd=hd: E.tensor_tensor(dg[:], identb, MM[:, d, g0:g0 + 4, hd:hd + 1].to_broadcast([128, 4, 128]), ALU.mult),
                                      ["ident", "MM"], [dgn])
                                fw.op("pe", lambda E, dg=dg: E.matmul(PS[7][:], ones[:], dg[:].rearrange("p a b -> p (a b)"), start=True, stop=True), ["ones", dgn], ["ps7"])
                                fw.op("act", lambda E, q4=q4: E.copy(MBt[:, q4 * TB:(q4 + 1) * TB], PS[7][:]), ["ps7"], ["MBt"])
                            kk = 0
                            for Q in range(4):
                                keys = range(0, 4 * Q + 4) if d == 0 else range(4 * Q, 16)
                                qsl = slice(Q * TB, (Q + 1) * TB)
                                rs_started = [False]
                                for tk in keys:
                                    w = kk % 2; ps_s = PS[5 + w]; psn = PSN[5 + w]; wt = WT[w]; wtn = f"WT{w}"; pt = PT[w]; ptn = f"PT{w}"
                                    ksl = slice(tk * 128, (tk + 1) * 128)
                                    for kc in range(4):
                                        fw.op("pe", lambda E, ps_s=ps_s, kc=kc, ksl=ksl, qsl=qsl: E.matmul(ps_s[:], KH[:, kc, ksl], QH[:, kc, qsl], start=(kc == 0), stop=(kc == 3)),
                                              ["KH", "QH"], [psn])
                                    acol = AALL[:, d, b * 16 + tk, hd:hd + 1]
                                    fw.op("act", lambda E, wt=wt, qsl=qsl, acol=acol: E.activation(wt[:], MBt[:, qsl], AF.Exp, bias=acol, scale=-1.0), ["MBt", "AALL"], [wtn])
                                    fw.op("dve", lambda E, pt=pt, ps_s=ps_s, wt=wt: E.scalar_tensor_tensor(pt[:], ps_s[:], SC, wt[:], ALU.mult, ALU.mult), [psn, wtn], [ptn])
                                    for qs in range(4):
                                        tq = 4 * Q + qs
                                        valid = (tk <= tq) if d == 0 else (tk >= tq)
                                        if not valid:
                                            continue
                                        sub = slice(qs * 128, (qs + 1) * 128)
                                        if tk == tq:
                                            fw.op("pool", lambda E, pt=pt, sub=sub, tmask=tmask: E.tensor_tensor(pt[:, sub], pt[:, sub], tmask[:], ALU.mult), [ptn, tmn], [ptn])
                                        first = (tk == 0) if d == 0 else (tk == tq)
                                        last = (tk == tq) if d == 0 else (tk == 15)
                                        fw.op("pe", lambda E, qs=qs, pt=pt, sub=sub, tk=tk, first=first, last=last: E.matmul(PS[qs][:], pt[:, sub], VH[:, tk, :], start=first, stop=last),
                                              [ptn, "VH"], [PSN[qs]])
                                        rfirst = not rs_started[0]
                                        rs_started[0] = True
                                        fw.op("pe", lambda E, qs=qs, pt=pt, sub=sub, rfirst=rfirst, last=last: E.matmul(PS[4][:, qs:qs + 1], pt[:, sub], onesb[:, 0:1], start=rfirst, stop=last),
                                              [ptn, "onesb"], ["ps4"])
                                    kk += 1
                                for qs in range(4):
                                    tq = 4 * Q + qs; g = b * 16 + tq
                                    c0 = sm4[:, qs, 0:1]; c1 = sm4[:, qs, 1:2]
                                    fw.op("dve", lambda E, qs=qs, c1=c1: E.tensor_scalar(c1, PS[4][:, qs:qs + 1], -1.0, None, ALU.mult), ["ps4"], ["sm4"])
                                    fw.op("dve", lambda E, qs=qs, c0=c0, c1=c1: E.tensor_tensor(c0, PS[4][:, qs:qs + 1], c1, ALU.max), ["ps4", "sm4"], ["sm4"])
                                    fw.op("dve", lambda E, c0=c0, d=d, g=g, hd=hd: E.tensor_tensor(c0, c0, EM[:, d, g, hd:hd + 1], ALU.max), ["sm4", "EM"], ["sm4"])
                                    fw.op("dve", lambda E, c0=c0, c1=c1: E.reciprocal(c1, c0), ["sm4"], ["sm4"])
                                    if d == 0:
                                        fw.op("act", lambda E, qs=qs, tq=tq, c1=c1: E.activation(HACC[:, tq, :], PS[qs][:], AF.Copy, scale=c1), [PSN[qs], "sm4"], ["HACC"])
                                    else:
                                        fw.op("dve", lambda E, qs=qs, tq=tq, c1=c1: E.scalar_tensor_tensor(HACC[:, tq, :], PS[qs][:], c1, HACC[:, tq, :], ALU.mult, ALU.add),
                                              [PSN[qs], "sm4", "HACC"], ["HACC"])
                        for tt in range(16):
                            fw.op("dve", lambda E, tt=tt: E.bn_stats(st6[:], HACC[:, tt, :]), ["HACC"], ["st6"])
                            fw.op("dve", lambda E: E.bn_aggr(mv[:], st6[:]), ["st6"], ["mv"])
                            fw.op("act", lambda E: E.activation(mv[:, 1:2], mv[:, 1:2], AF.Sqrt, bias=1e-5, scale=1.0), ["mv"], ["mv"])
                            fw.op("dve", lambda E: E.reciprocal(mv[:, 1:2], mv[:, 1:2]), ["mv"], ["mv"])
                            fw.op("dve", lambda E, tt=tt: E.tensor_scalar(hn[:], HACC[:, tt, :], mv[:, 0:1], mv[:, 1:2], ALU.subtract, ALU.mult), ["HACC", "mv"], ["hnt"])
                            p = PS[5 + tt % 2]; pn = PSN[5 + tt % 2]
                            for es_ in range(4):
                                fw.op("pe", lambda E, p=p, es_=es_: E.transpose(p[:, es_ * 128:(es_ + 1) * 128], hn[:, es_ * 128:(es_ + 1) * 128], ident[:]), ["hnt", "ident"], [pn])
                            fw.op("act", lambda E, p=p, tt=tt: E.copy(HNF[:, :, tt * 128:(tt + 1) * 128], p[:].rearrange("p (a b) -> p a b", b=128)), [pn], ["HNF"])
                        fw.dma("act", HNT[hd * 4:(hd + 1) * 4, :, bs].rearrange("k p t -> p k t"), HNF[:], reads=["HNF"], writes=["HNT"])

        def phase_ml_out(j, li):
            with ExitStack() as es:
                Wo = SB(es, "Wo2", [128, 16, D], BF16)
                load_w_bf16(es, Wo, "Wo2", I["ml_w_out"][j], 16, D, "wo2")
                gn = SB(es, "gn", [128, 16]); sk = SB(es, "sk", [128, 16])
                fw.dma("sp", gn[:], I["ml_gnT"][:, j], writes=["gn"])
                fw.dma("sp", sk[:], I["ml_skipT"][:, j], writes=["sk"])
                HN = SB(es, "HNb", [128, 16, TB], BF16); XCb = SB(es, "XCb", [128, 16, TB], BF16); SZ = SB(es, "SZb2", [128, 16, TB], BF16)
                Y = SB(es, "Yb", [128, 16, TB], BF16); XB = SB(es, "XBo2", [128, 8, TB])
                tm = [SB(es, f"tm{i}", [128, TB]) for i in range(2)]
                for tb in range(NTB):
                    b = tb // (NTB // NB); ts_ = slice(tb * TB, (tb + 1) * TB)
                    fw.dma("sp", HN[:], HNT[:, :, ts_].rearrange("k p t -> p k t"), reads=["HNT"], writes=["HNb"])
                    fw.dma("sp", XCb[:], XCT[:, :, ts_].rearrange("k p t -> p k t"), reads=["XCT"], writes=["XCb"])
                    fw.dma("sp", SZ[:], SZT[:, :, ts_].rearrange("k p t -> p k t"), reads=["dst_b"], writes=["SZb2"])
                    fw.dma("sp", XB[:], XT[:, :, ts_].rearrange("k p t -> p k t"), reads=["XT"], writes=["XBo2"])
                    for cc in range(16):
                        t_ = tm[cc % 2]; tn_ = f"tm{cc % 2}"
                        fw.op("pool", lambda E, t_=t_, cc=cc: E.tensor_scalar(t_[:], XCb[:, cc, :], sk[:, cc:cc + 1], None, ALU.mult), ["XCb", "sk"], [tn_])
                        fw.op("dve", lambda E, t_=t_, cc=cc: E.scalar_tensor_tensor(t_[:], HN[:, cc, :], gn[:, cc:cc + 1], t_[:], ALU.mult, ALU.add), ["HNb", "gn", tn_], [tn_])
                        fw.op("dve" if cc % 2 else "pool", lambda E, t_=t_, cc=cc: E.tensor_tensor(Y[:, cc, :], t_[:], SZ[:, cc, :], ALU.mult), [tn_, "SZb2"], ["Yb"])
                    for mt in range(8):
                        p = PS[mt % 4]; pn = PSN[mt % 4]
                        for kc in range(16):
                            fw.op("pe", lambda E, p=p, kc=kc, mt=mt: E.matmul(p[:], Wo[:, kc, mt * 128:(mt + 1) * 128], Y[:, kc, :], start=(kc == 0), stop=(kc == 15)),
                                  ["Wo2", "Yb"], [pn])
                        fw.op("dve", lambda E, p=p, mt=mt, b=b: E.scalar_tensor_tensor(XB[:, mt, :], p[:], MOD[:, li, 16 + mt, b:b + 1], XB[:, mt, :], ALU.mult, ALU.add),
                              [pn, "MOD", "XBo2"], ["XBo2"])
                    fw.dma("act", XT[:, :, ts_].rearrange("k p t -> p k t"), XB[:], reads=["XBo2"], writes=["XT"])

        def phase_final():
            with ExitStack() as es:
                XB = SB(es, "XBf", [128, 8, TB]); FO = SB(es, "FO", [128, 8, TB])
                sq = SB(es, "sq", [128, 4, TB]); rs = SB(es, "rs", [128, TB])
                ot = [SB(es, f"ot{i}", [128, D]) for i in range(2)]
                k = 0
                for tb in range(NTB):
                    ts_ = slice(tb * TB, (tb + 1) * TB)
                    fw.dma("sp", XB[:], XT[:, :, ts_].rearrange("k p t -> p k t"), reads=["XT"], writes=["XBf"])
                    norm_block(XB, "XBf", FO, "FO", (sq, rs), 0, 0, final=True)
                    for t4 in range(4):
                        o = ot[k % 2]; on = f"ot{k % 2}"
                        for h in range(2):
                            p = PS[h]; pn = PSN[h]
                            for q in range(4):
                                kc = h * 4 + q
                                fw.op("pe", lambda E, p=p, q=q, kc=kc, t4=t4: E.transpose(p[:, q * 128:(q + 1) * 128], FO[:, kc, t4 * 128:(t4 + 1) * 128], ident[:]),
                                      ["FO", "ident"], [pn])
                            copy_op("dve" if h == 0 else "act", o[:, h * 512:(h + 1) * 512], p[:], [pn], [on])
                        r0 = tb * TB + t4 * 128
                        fw.dma("sp", OUT[r0:r0 + 128, :], o[:], reads=[on], writes=["OUT"])
                        k += 1

        stages = []
        stages.append(("mod", phase_mod))
        stages.append(("in", phase_in))
        for li in range(4):
            j = li // 2
            if li % 2 == 0:
                stages.append((f"inproj{li}", lambda li=li, j=j: phase_inproj(li, I["s5_w_in"][j], 2 * D, UT, SZT)))
                stages.append((f"ssm{li}", lambda j=j: phase_s5_ssm(j)))
                stages.append((f"s5out{li}", lambda li=li, j=j: phase_s5_out(j, li)))
            else:
                stages.append((f"inproj{li}", lambda li=li, j=j: phase_inproj(li, I["ml_w_in"][j], 4 * D, XMT, SZT)))
                stages.append((f"qkv{li}", lambda j=j: phase_ml_qkv(j, GATES)))
                stages.append((f"attn{li}", lambda j=j: phase_ml_attn(j, GATES)))
                stages.append((f"mlout{li}", lambda li=li, j=j: phase_ml_out(j, li)))
        stages.append(("final", phase_final))
        GATES = SB(top, "GATES", [128, 32, 16])
        for name, fn in stages:
            if dbg_stop and name in dbg_stop:
                break
            fn()
            fw.barrier()
        if "MODD" in dbg:
            MODD = nc.dram_tensor("MODD", [128, 4 * 24 * NB], F32, kind="ExternalOutput").ap()
            fw.dma("sp", MODD, MOD[:].rearrange("p a b c -> p (a b c)"), reads=["MOD"], writes=["MODD"])
        if "GATESD" in dbg:
            GD = nc.dram_tensor("GATESD", [128, 512], F32, kind="ExternalOutput").ap()
            fw.dma("sp", GD, GATES[:].rearrange("p a b -> p (a b)"), reads=["GATES"], writes=["GATESD"])
        fw.finish(["OUT", "MODD", "GATESD", "XT", "dst_a", "dst_b", "GT", "YT", "XCT", "QT", "KT", "KTOK", "VTOK", "HNT"], "sp")
        fw.run_block()
    print("n_inst", fw.n_inst, {e: len(fw.prog[e]) for e in fw.prog})
    return nc


def _prep_shared(inp):
    f = np.float32
    S = {}
    S["ada_w"] = np.ascontiguousarray(inp["ada_w"], dtype=f)
    S["ada_bT"] = np.ascontiguousarray(inp["ada_b"].reshape(4, 24, 128).transpose(2, 0, 1), dtype=f)
    S["norm_gT"] = np.ascontiguousarray(inp["norm_g"].reshape(4, 8, 128).transpose(2, 0, 1), dtype=f)
    S["final_gT"] = np.ascontiguousarray(inp["final_g"].reshape(8, 128).T, dtype=f)
    S["ident"] = np.eye(128, dtype=f)
    S["ones"] = np.ones((128, 128), f)
    S["j1"] = np.ascontiguousarray(np.broadcast_to(np.arange(1, TB + 1, dtype=f), (128, TB)))
    t = np.arange(128)[:, None]; s = np.arange(128)[None, :]
    S["maskF"] = np.where(s <= t, 0.0, -30000.0).astype(f)
    S["maskB"] = np.where(s >= t, 0.0, -30000.0).astype(f)
    S["triF"] = (t <= s).astype(f)
    S["triB"] = (t >= s).astype(f)
    for k in ("s5_w_in", "s5_w_glu", "s5_w_out", "ml_w_in", "ml_w_out"):
        S[k] = np.ascontiguousarray(inp[k], dtype=f)
    S["s5_b_gluT"] = np.ascontiguousarray(inp["s5_b_glu"].reshape(2, 8, 128).transpose(2, 0, 1), dtype=f)
    S["s5_dT"] = np.ascontiguousarray(inp["s5_d"].reshape(2, 8, 128).transpose(2, 0, 1), dtype=f)

    def st_layout(a):
        a = a.reshape(2, 2, 32, 2, 64)
        return np.ascontiguousarray(a.transpose(3, 4, 0, 1, 2).reshape(128, 2, 2, 32), dtype=f)
    S["s5_lre"] = st_layout(inp["s5_lam_re"])
    S["s5_lim"] = st_layout(inp["s5_lam_im"])
    S["s5_ldt"] = st_layout(np.broadcast_to(inp["s5_log_dt"][..., None], (2, 2, 64, 64)))

    def b_layout(bm):
        out = np.zeros((2, 2, 128, 32, 128), f)
        for st in range(32):
            pi = st % 4
            for gl in range(2):
                g = 2 * st + gl
                k0 = (2 * pi + gl) * 16
                out[:, :, k0:k0 + 16, st, gl * 64:(gl + 1) * 64] = bm[:, :, g].transpose(0, 1, 3, 2)
        return out.reshape(2, 2, 128, 32 * 128)

    def c_layout(cm):
        out = np.zeros((2, 2, 128, 32, 128), f)
        for st in range(32):
            pi = st % 4
            for gl in range(2):
                g = 2 * st + gl
                m0 = (2 * pi + gl) * 16
                out[:, :, gl * 64:(gl + 1) * 64, st, m0:m0 + 16] = cm[:, :, g].transpose(0, 1, 3, 2)
        return out.reshape(2, 2, 128, 32 * 128)
    S["s5_bre"] = b_layout(np.asarray(inp["s5_b_re"], f)); S["s5_bim"] = b_layout(np.asarray(inp["s5_b_im"], f))
    S["s5_cre"] = c_layout(np.asarray(inp["s5_c_re"], f)); S["s5_cim"] = c_layout(np.asarray(inp["s5_c_im"], f))
    S["ml_convT"] = np.ascontiguousarray(inp["ml_conv_w"].reshape(2, 5, 16, 128).transpose(3, 0, 2, 1), dtype=f)
    S["ml_convbT"] = np.ascontiguousarray(inp["ml_conv_b"].reshape(2, 16, 128).transpose(2, 0, 1), dtype=f)

    def bd_layout(w):
        out = np.zeros((2, 128, 16, 128), f)
        wr = np.asarray(w, f).reshape(2, 16, 32, 4, 4)
        for n in range(32):
            out[:, 4 * n:4 * n + 4, :, 4 * n:4 * n + 4] = wr[:, :, n].transpose(0, 2, 1, 3)
        return out.reshape(2, 128, 16 * 128)
    S["ml_wq_bd"] = bd_layout(inp["ml_w_q"]); S["ml_wk_bd"] = bd_layout(inp["ml_w_k"]); S["ml_wv_bd"] = bd_layout(inp["ml_w_v"])
    wg = np.asarray(inp["ml_w_gates"], f).reshape(2, 3, 16, 128, 16)
    S["ml_wg"] = np.ascontiguousarray(wg.transpose(0, 3, 1, 2, 4).reshape(2, 128, 768))
    S["ml_bg_rep"] = np.ascontiguousarray(np.broadcast_to(np.asarray(inp["ml_b_gates"], f)[None], (128, 2, 16)))
    S["ml_gnT"] = np.ascontiguousarray(inp["ml_gn_w"].reshape(2, 16, 128).transpose(2, 0, 1), dtype=f)
    S["ml_skipT"] = np.ascontiguousarray(inp["ml_skip"].reshape(2, 16, 128).transpose(2, 0, 1), dtype=f)
    return S


_NC_CACHE = {}


def kernel(**inputs):
    inp = {k: np.asarray(v) for k, v in inputs.items()}
    S = _prep_shared(inp)
    x = np.asarray(inp["x"], np.float32); c = np.asarray(inp["c"], np.float32)
    in_maps = []
    for core in range(NCORES):
        m = dict(S)
        m["x"] = np.ascontiguousarray(x[core * NB:(core + 1) * NB].reshape(NT, D))
        cc = c[core * NB:(core + 1) * NB]
        m["cT"] = np.ascontiguousarray(cc.reshape(NB, 8, 128).transpose(2, 1, 0))
        in_maps.append(m)
    if "nc" not in _NC_CACHE:
        _NC_CACHE["nc"] = build_program()
    res = run_bass_kernel_spmd(_NC_CACHE["nc"], in_maps, core_ids=list(range(NCORES)))
    out = np.concatenate([r["out"].reshape(NB, L, D) for r in res.results], axis=0)
    return out.astype(np.float32)
```

```python
import numpy as np
from contextlib import ExitStack
import concourse.bass as bass
import concourse.mybir as mybir
from concourse.ap import AP
from concourse.bass_utils import run_bass_kernel_spmd

F32 = mybir.dt.float32
BF16 = mybir.dt.bfloat16
I32 = mybir.dt.int32
ALU = mybir.AluOpType
AF = mybir.ActivationFunctionType
AX = mybir.AxisListType

NCORES = 8
D = 1024
L = 2048
NB = 2
NT = NB * L
TB = 512
NTB = NT // TB
SEM_LIMIT = 30000
N_DMA_SEMS = 12
TWO_PI = float(2 * np.pi)
SERIAL_DMA = False


class FW:
    ENGS = ("pe", "act", "dve", "pool", "sp")
    same_engine_sync = True

    def __init__(self, nc):
        self.nc = nc
        self.prog = {e: [] for e in self.ENGS}
        self.cur_sem = {}
        self.cnt = {}
        self.nsem = 0
        for e in self.ENGS:
            self._new_sem(e)
        self.waited = {}
        self.res = {}
        self.dma_sems = {}
        self.dma_rr = {}
        for e in ("sp", "act", "pool"):
            self.dma_sems[e] = [[self._alloc_sem(f"dma_{e}_{i}"), 0] for i in range(N_DMA_SEMS)]
            self.dma_rr[e] = 0
        self.n_inst = 0
        self.last_dma = {}

    def _alloc_sem(self, name):
        self.nsem += 1
        return self.nc.alloc_semaphore(name=f"{name}_{self.nsem}")

    def _new_sem(self, e):
        self.cur_sem[e] = self._alloc_sem(f"cnt_{e}")
        self.cnt[e] = 0

    def _need(self, eng, tok, waits):
        if tok is None:
            return
        sem, val, owner = tok
        if self.waited.get((eng, id(sem)), 0) >= val:
            return
        if owner == eng and (eng == "pe" or not self.same_engine_sync):
            return
        old = waits.get(id(sem))
        if old is None or old[1] < val:
            waits[id(sem)] = (sem, val)

    def _deps(self, eng, reads, writes):
        waits = {}
        for r in reads:
            ent = self.res.get(r)
            if ent is not None:
                self._need(eng, ent[0], waits)
        for w in writes:
            ent = self.res.get(w)
            if ent is not None:
                self._need(eng, ent[0], waits)
                for t in ent[1]:
                    self._need(eng, t, waits)
        return waits

    def _emit_waits(self, eng, waits):
        for sid, (sem, val) in waits.items():
            self.waited[(eng, sid)] = val
            self.prog[eng].append(lambda E, sem=sem, val=val: E.wait_ge(sem, val))
            self.n_inst += 1

    def _update(self, tok, reads, writes):
        for r in reads:
            ent = self.res.setdefault(r, [None, []])
            ent[1].append(tok)
            if len(ent[1]) > 48:
                best = {}
                for t in ent[1]:
                    k = id(t[0])
                    if k not in best or best[k][1] < t[1]:
                        best[k] = t
                ent[1] = list(best.values())
        for w in writes:
            self.res[w] = [tok, []]

    def op(self, eng, fn, reads=(), writes=()):
        reads = [r for r in reads if r is not None]
        writes = [w for w in writes if w is not None]
        waits = self._deps(eng, reads, writes)
        self._emit_waits(eng, waits)
        if self.cnt[eng] >= SEM_LIMIT:
            self._new_sem(eng)
        sem = self.cur_sem[eng]
        self.cnt[eng] += 1
        val = self.cnt[eng]
        self.prog[eng].append(lambda E, fn=fn, sem=sem: fn(E).then_inc(sem, 1))
        self.n_inst += 1
        tok = (sem, val, eng)
        self._update(tok, reads, writes)
        return tok

    def dma(self, q, out, in_, reads=(), writes=()):
        reads = [r for r in reads if r is not None]
        writes = [w for w in writes if w is not None]
        waits = self._deps(q, reads, writes)
        slot = self.dma_sems[q][self.dma_rr[q] % N_DMA_SEMS]
        self.dma_rr[q] += 1
        sem, used = slot
        if used > 0 and self.waited.get((q, id(sem)), 0) < used:
            old = waits.get(id(sem))
            if old is None or old[1] < used:
                waits[id(sem)] = (sem, used)
        if SERIAL_DMA and self.last_dma.get(q) is not None:
            ps_, pv_ = self.last_dma[q]
            if self.waited.get((q, id(ps_)), 0) < pv_:
                old = waits.get(id(ps_))
                if old is None or old[1] < pv_:
                    waits[id(ps_)] = (ps_, pv_)
        self._emit_waits(q, waits)
        slot[1] = used + 16
        self.last_dma[q] = (sem, slot[1])
        val = slot[1]
        self.prog[q].append(lambda E, out=out, in_=in_, sem=sem: E.dma_start(out=out, in_=in_).then_inc(sem, 16))
        self.n_inst += 1
        tok = (sem, val, "dma")
        self._update(tok, reads, writes)
        return tok

    def barrier(self):
        for eng in self.ENGS:
            waits = {}
            for other in self.ENGS:
                if other == eng or self.cnt[other] == 0:
                    continue
                sem = self.cur_sem[other]
                if self.waited.get((eng, id(sem)), 0) < self.cnt[other]:
                    waits[id(sem)] = (sem, self.cnt[other])
            for q in self.dma_sems:
                for sem, used in self.dma_sems[q]:
                    if used > 0 and self.waited.get((eng, id(sem)), 0) < used:
                        waits[id(sem)] = (sem, used)
            self._emit_waits(eng, waits)

    def finish(self, names, eng="sp"):
        waits = {}
        for n in names:
            ent = self.res.get(n)
            if ent is not None:
                self._need(eng, ent[0], waits)
        self._emit_waits(eng, waits)

    def run_block(self):
        nc = self.nc
        with nc.Block() as block:
            @block.tensor
            def _(E):
                for f in self.prog["pe"]:
                    f(E)

            @block.scalar
            def _(E):
                for f in self.prog["act"]:
                    f(E)

            @block.vector
            def _(E):
                for f in self.prog["dve"]:
                    f(E)

            @block.gpsimd
            def _(E):
                for f in self.prog["pool"]:
                    f(E)

            @block.sync
            def _(E):
                for f in self.prog["sp"]:
                    f(E)


def rev_ap(ap2d, start, n):
    a = list(ap2d.ap)
    return AP(ap2d.tensor, ap2d.offset + start * a[-1][0], [list(a[0]), [-a[-1][0], n]])


INPUT_SPECS = [
    ("x", [NT, D]), ("cT", [128, 8, NB]), ("ada_w", [4, D, 3 * D]), ("ada_bT", [128, 4, 24]),
    ("norm_gT", [128, 4, 8]), ("final_gT", [128, 8]), ("ident", [128, 128]), ("ones", [128, 128]),
    ("j1", [128, TB]), ("maskF", [128, 128]), ("maskB", [128, 128]), ("triF", [128, 128]), ("triB", [128, 128]),
    ("s5_w_in", [2, D, 2 * D]), ("s5_w_glu", [2, D, D]), ("s5_w_out", [2, D, D]),
    ("s5_b_gluT", [128, 2, 8]), ("s5_dT", [128, 2, 8]),
    ("s5_lre", [128, 2, 2, 32]), ("s5_lim", [128, 2, 2, 32]), ("s5_ldt", [128, 2, 2, 32]),
    ("s5_bre", [2, 2, 128, 32 * 128]), ("s5_bim", [2, 2, 128, 32 * 128]),
    ("s5_cre", [2, 2, 128, 32 * 128]), ("s5_cim", [2, 2, 128, 32 * 128]),
    ("ml_w_in", [2, D, 4 * D]), ("ml_w_out", [2, 2 * D, D]),
    ("ml_convT", [128, 2, 16, 5]), ("ml_convbT", [128, 2, 16]),
    ("ml_wq_bd", [2, 128, 16 * 128]), ("ml_wk_bd", [2, 128, 16 * 128]), ("ml_wv_bd", [2, 128, 16 * 128]),
    ("ml_wg", [2, 128, 3 * 16 * 16]), ("ml_bg_rep", [128, 2, 16]),
    ("ml_gnT", [128, 2, 16]), ("ml_skipT", [128, 2, 16]),
]


def build_program(dbg=(), dbg_stop=()):
    nc = bass.Bass("TRN2", target_bir_lowering=False)
    I = {}
    for name, shape in INPUT_SPECS:
        I[name] = nc.dram_tensor(name, shape, F32, kind="ExternalInput").ap()
    OUT = nc.dram_tensor("out", [NT, D], F32, kind="ExternalOutput").ap()

    def scratch(name, shape, dt):
        kind = "ExternalOutput" if name in dbg else "Internal"
        return nc.dram_tensor(name, shape, dt, kind=kind).ap()

    XT = scratch("XT", [8, 128, NT], F32)
    UT = scratch("UT", [8, 128, NT], BF16)
    SZT = scratch("SZT", [16, 128, NT], BF16)
    GT = scratch("GT", [8, 128, NT], BF16)
    XMT = scratch("XMT", [16, 128, NT], BF16)
    XCT = scratch("XCT", [16, 128, NT], BF16)
    QT = scratch("QT", [16, 128, NT], BF16)
    KT = scratch("KT", [16, 128, NT], BF16)
    KTOK = scratch("KTOK", [NT, 2 * D], BF16)
    VTOK = scratch("VTOK", [NT, 2 * D], BF16)
    HNT = scratch("HNT", [16, 128, NT], BF16)

    fw = FW(nc)
    rr = {"cast": 0, "ev": 0}

    with ExitStack() as top:
        def SB(es, name, shape, dt=F32):
            rr["sb"] = rr.get("sb", 0) + 1
            return es.enter_context(nc.sbuf_tensor(f"sb{rr['sb']}_{name}", shape, dt))

        PS = [top.enter_context(nc.psum_tensor(f"ps{i}", [128, 512], F32)) for i in range(8)]
        PSN = [f"ps{i}" for i in range(8)]

        ident = SB(top, "ident", [128, 128]); ones = SB(top, "ones", [128, 128])
        onesb = SB(top, "onesb", [128, 128], BF16)
        maskF = SB(top, "maskF", [128, 128]); maskB = SB(top, "maskB", [128, 128])
        triF = SB(top, "triF", [128, 128]); triB = SB(top, "triB", [128, 128])
        j1 = SB(top, "j1", [128, TB])
        MOD = SB(top, "MOD", [128, 4, 24, NB])
        S1 = SB(top, "S1", [128, 4, 8, NB])
        ngT = SB(top, "ngT", [128, 4, 8]); fgT = SB(top, "fgT", [128, 8])
        for t, n, rn in ((ident, "ident", "ident"), (ones, "ones", "ones"), (maskF, "maskF", "maskF"), (maskB, "maskB", "maskB"),
                         (triF, "triF", "triF"), (triB, "triB", "triB"), (j1, "j1", "j1"), (ngT, "norm_gT", "ngT"), (fgT, "final_gT", "fgT")):
            fw.dma("sp", t[:], I[n], writes=[rn])
        fw.op("dve", lambda E: E.tensor_copy(onesb[:], ones[:]), ["ones"], ["onesb"])

        def cast_eng():
            rr["cast"] += 1
            return ("dve", "pool", "act")[rr["cast"] % 3]

        def copy_op(eng, out, in_, r, w):
            if eng == "act":
                fw.op("act", lambda E: E.copy(out, in_), r, w)
            else:
                fw.op(eng, lambda E: E.tensor_copy(out, in_), r, w)

        def load_w_bf16(es, dst, dname, src, KC, N, tag):
            CH = min(N, 2048)
            stg = [SB(es, f"stg_{tag}_{i}", [128, CH]) for i in range(2)]
            k = 0
            for kc in range(KC):
                for n0 in range(0, N, CH):
                    s = stg[k % 2]; sn = f"stg_{tag}_{k % 2}"
                    fw.dma("sp", s[:], src[kc * 128:(kc + 1) * 128, n0:n0 + CH], writes=[sn])
                    copy_op(cast_eng(), dst[:, kc, n0:n0 + CH], s[:], [sn], [dname])
                    k += 1

        def phase_mod():
            with ExitStack() as es:
                cT = SB(es, "cT", [128, 8, NB]); sc = SB(es, "sc", [128, 8, NB]); abT = SB(es, "abT", [128, 4, 24])
                wt = [SB(es, f"adaw{i}", [128, 8, 128]) for i in range(2)]
                fw.dma("sp", cT[:], I["cT"], writes=["cT"])
                fw.dma("sp", abT[:], I["ada_bT"], writes=["abT"])
                fw.op("act", lambda E: E.activation(sc[:], cT[:], AF.Silu), ["cT"], ["sc"])
                k = 0
                for i in range(4):
                    for m in range(24):
                        w = wt[k % 2]; wn = f"adaw{k % 2}"
                        src = I["ada_w"][i].rearrange("(kc p) n -> p kc n", p=128)[:, :, m * 128:(m + 1) * 128]
                        fw.dma("sp" if k % 2 == 0 else "act", w[:], src, writes=[wn])
                        for kc in range(8):
                            fw.op("pe", lambda E, w=w, kc=kc: E.matmul(PS[0][:, 0:NB], w[:, kc, :], sc[:, kc, :],
                                                                       start=(kc == 0), stop=(kc == 7)), [wn, "sc"], ["ps0"])
                        fw.op("dve", lambda E, i=i, m=m: E.tensor_scalar(MOD[:, i, m, :], PS[0][:, 0:NB], abT[:, i, m:m + 1], None, ALU.add),
                              ["ps0", "abT"], ["MOD"])
                        k += 1
                for i in range(4):
                    for b in range(NB):
                        fw.op("dve", lambda E, i=i, b=b: E.scalar_tensor_tensor(S1[:, i, :, b], MOD[:, i, 8:16, b], 1.0, ngT[:, i, :], ALU.add, ALU.mult),
                              ["MOD", "ngT"], ["S1"])

        def phase_in():
            with ExitStack() as es:
                xi = [SB(es, f"xi{i}", [128, D]) for i in range(2)]
                xo = [SB(es, f"xo{i}", [128, 8, 128]) for i in range(2)]
                for tt in range(NT // 128):
                    a = xi[tt % 2]; an = f"xi{tt % 2}"; o = xo[tt % 2]; on = f"xo{tt % 2}"
                    fw.dma("sp", a[:], I["x"][tt * 128:(tt + 1) * 128, :], writes=[an])
                    for h in range(2):
                        p = PS[h]; pn = PSN[h]
                        for q in range(4):
                            kc = h * 4 + q
                            fw.op("pe", lambda E, p=p, q=q, kc=kc, a=a: E.transpose(p[:, q * 128:(q + 1) * 128], a[:, kc * 128:(kc + 1) * 128], ident[:]),
                                  [an, "ident"], [pn])
                        copy_op("dve" if h == 0 else "act", o[:, h * 4:(h + 1) * 4, :].rearrange("p a b -> p (a b)"), p[:], [pn], [on])
                    fw.dma("act", XT[:, :, tt * 128:(tt + 1) * 128].rearrange("k p t -> p k t"), o[:], reads=[on], writes=["XT"])

        def norm_block(XB, xbn, HB, hbn, tmps, li, b, final=False):
            sq, rs = tmps
            for kc in range(8):
                fw.op("act", lambda E, kc=kc: E.activation(sq[:, kc % 2, :], XB[:, kc, :], AF.Square), [xbn], [f"sq{kc % 2}"])
                fw.op("pe", lambda E, kc=kc: E.matmul(PS[7][:], ones[:], sq[:, kc % 2, :], start=(kc == 0), stop=(kc == 7)),
                      [f"sq{kc % 2}", "ones"], ["ps7"])
            fw.op("act", lambda E: E.activation(rs[:], PS[7][:], AF.Sqrt, bias=1e-6, scale=1.0 / D), ["ps7"], ["rs"])
            fw.op("dve", lambda E: E.reciprocal(rs[:], rs[:]), ["rs"], ["rs"])
            for kc in range(8):
                eng = "dve"
                if final:
                    fw.op("dve", lambda E, kc=kc: E.scalar_tensor_tensor(HB[:, kc, :], XB[:, kc, :], fgT[:, kc:kc + 1], rs[:], ALU.mult, ALU.mult),
                          [xbn, "rs", "fgT"], [hbn])
                else:
                    fw.op("dve", lambda E, kc=kc: E.scalar_tensor_tensor(sq[:, 2 + kc % 2, :], XB[:, kc, :], S1[:, li, kc, b:b + 1], rs[:], ALU.mult, ALU.mult),
                          [xbn, "rs", "S1"], [f"sq{2 + kc % 2}"])
                    fw.op(eng, lambda E, kc=kc: E.tensor_scalar(HB[:, kc, :], sq[:, 2 + kc % 2, :], MOD[:, li, kc, b:b + 1], None, ALU.add),
                          [f"sq{2 + kc % 2}", "MOD"], [hbn])

        def phase_inproj(li, w_src, NOUT, dst_a, dst_b):
            NM = NOUT // 128
            with ExitStack() as es:
                W = SB(es, "Win", [128, 8, NOUT], BF16)
                load_w_bf16(es, W, "Win", w_src, 8, NOUT, "win")
                XBs = [SB(es, f"XB{i}", [128, 8, TB]) for i in range(2)]
                HB = SB(es, "HB", [128, 8, TB], BF16)
                sq = SB(es, "sq", [128, 4, TB]); rs = SB(es, "rs", [128, TB])
                ob = [SB(es, f"ob{i}", [128, TB], BF16) for i in range(4)]
                fw.dma("sp", XBs[0][:], XT[:, :, 0:TB].rearrange("k p t -> p k t"), reads=["XT"], writes=["XB0"])
                for tb in range(NTB):
                    XB = XBs[tb % 2]; xbn = f"XB{tb % 2}"; b = tb // (NTB // NB)
                    if tb + 1 < NTB:
                        fw.dma("sp", XBs[(tb + 1) % 2][:], XT[:, :, (tb + 1) * TB:(tb + 2) * TB].rearrange("k p t -> p k t"),
                               reads=["XT"], writes=[f"XB{(tb + 1) % 2}"])
                    norm_block(XB, xbn, HB, "HB", (sq, rs), li, b)
                    for mt in range(NM):
                        p = PS[mt % 4]; pn = PSN[mt % 4]
                        for kc in range(8):
                            fw.op("pe", lambda E, p=p, kc=kc, mt=mt: E.matmul(p[:], W[:, kc, mt * 128:(mt + 1) * 128], HB[:, kc, :],
                                                                              start=(kc == 0), stop=(kc == 7)), ["Win", "HB"], [pn])
                        o = ob[mt % 4]; on = f"ob{mt % 4}"
                        if mt < NM // 2:
                            copy_op("dve" if mt % 2 == 0 else "act", o[:], p[:], [pn], [on])
                            fw.dma("act", dst_a[mt][:, tb * TB:(tb + 1) * TB], o[:], reads=[on], writes=["dst_a"])
                        else:
                            fw.op("act", lambda E, o=o, p=p: E.activation(o[:], p[:], AF.Silu), [pn], [on])
                            fw.dma("act", dst_b[mt - NM // 2][:, tb * TB:(tb + 1) * TB], o[:], reads=[on], writes=["dst_b"])

        YT = scratch("YT", [8, 128, NT], F32)

        def phase_s5_ssm(j):
            with ExitStack() as es:
                lre = SB(es, "lre", [128, 2, 32]); lim = SB(es, "lim", [128, 2, 32]); ldt = SB(es, "ldt", [128, 2, 32])
                fw.dma("sp", lre[:], I["s5_lre"][:, j], writes=["lre"])
                fw.dma("sp", lim[:], I["s5_lim"][:, j], writes=["lim"])
                fw.dma("sp", ldt[:], I["s5_ldt"][:, j], writes=["ldt"])
                dT = SB(es, "dT", [128, 8])
                fw.dma("sp", dT[:], I["s5_dT"][:, j], writes=["dT"])
                tn = ["dt", "th", "rmag", "ar", "ai", "k1", "k2", "k3", "den", "zre", "zim", "nzim", "t1s", "t2s"]
                T = {n: SB(es, "s5t_" + n, [128, 2, 32]) for n in tn}
                ki = SB(es, "s5t_ki", [128, 2, 32], I32)

                def sm(eng, fn, r, w):
                    fw.op(eng, fn, ["s5t_" + x if x in T else x for x in r], ["s5t_" + x if x in T else x for x in w])

                sm("act", lambda E: E.activation(T["dt"][:], ldt[:], AF.Exp), ["ldt"], ["dt"])
                sm("dve", lambda E: E.tensor_tensor(T["th"][:], lim[:], T["dt"][:], ALU.mult), ["lim", "dt"], ["th"])
                sm("dve", lambda E: E.tensor_tensor(T["k1"][:], lre[:], T["dt"][:], ALU.mult), ["lre", "dt"], ["k1"])
                sm("act", lambda E: E.activation(T["rmag"][:], T["k1"][:], AF.Exp), ["k1"], ["rmag"])
                for dst, sh in (("ai", 0.0), ("ar", float(np.pi / 2))):
                    sm("dve", lambda E, sh=sh: E.tensor_scalar(T["k2"][:], T["th"][:], sh, None, ALU.add), ["th"], ["k2"])
                    sm("dve", lambda E: E.tensor_scalar(ki[:], T["k2"][:], 1.0 / TWO_PI, None, ALU.mult), ["k2"], ["s5t_ki"])
                    sm("dve", lambda E: E.tensor_copy(T["k3"][:], ki[:]), ["s5t_ki"], ["k3"])
                    sm("dve", lambda E: E.scalar_tensor_tensor(T["k2"][:], T["k3"][:], -TWO_PI, T["k2"][:], ALU.mult, ALU.add), ["k3", "k2"], ["k2"])
                    sm("act", lambda E, dst=dst: E.activation(T[dst][:], T["k2"][:], AF.Sin), ["k2"], [dst])
                sm("dve", lambda E: E.tensor_tensor(T["ar"][:], T["ar"][:], T["rmag"][:], ALU.mult), ["ar", "rmag"], ["ar"])
                sm("dve", lambda E: E.tensor_tensor(T["ai"][:], T["ai"][:], T["rmag"][:], ALU.mult), ["ai", "rmag"], ["ai"])
                sm("dve", lambda E: E.tensor_scalar(T["k1"][:], T["ar"][:], -1.0, None, ALU.add), ["ar"], ["k1"])
                sm("dve", lambda E: E.tensor_tensor(T["den"][:], lre[:], lre[:], ALU.mult), ["lre"], ["den"])
                sm("dve", lambda E: E.tensor_tensor(T["k2"][:], lim[:], lim[:], ALU.mult), ["lim"], ["k2"])
                sm("dve", lambda E: E.tensor_tensor(T["den"][:], T["den"][:], T["k2"][:], ALU.add), ["den", "k2"], ["den"])
                sm("dve", lambda E: E.reciprocal(T["den"][:], T["den"][:]), ["den"], ["den"])
                sm("dve", lambda E: E.tensor_tensor(T["t1s"][:], T["k1"][:], lre[:], ALU.mult), ["k1", "lre"], ["t1s"])
                sm("dve", lambda E: E.tensor_tensor(T["t2s"][:], T["ai"][:], lim[:], ALU.mult), ["ai", "lim"], ["t2s"])
                sm("dve", lambda E: E.tensor_tensor(T["zre"][:], T["t1s"][:], T["t2s"][:], ALU.add), ["t1s", "t2s"], ["zre"])
                sm("dve", lambda E: E.tensor_tensor(T["zre"][:], T["zre"][:], T["den"][:], ALU.mult), ["zre", "den"], ["zre"])
                sm("dve", lambda E: E.tensor_tensor(T["t1s"][:], T["ai"][:], lre[:], ALU.mult), ["ai", "lre"], ["t1s"])
                sm("dve", lambda E: E.tensor_tensor(T["t2s"][:], T["k1"][:], lim[:], ALU.mult), ["k1", "lim"], ["t2s"])
                sm("dve", lambda E: E.tensor_tensor(T["zim"][:], T["t1s"][:], T["t2s"][:], ALU.subtract), ["t1s", "t2s"], ["zim"])
                sm("dve", lambda E: E.tensor_tensor(T["zim"][:], T["zim"][:], T["den"][:], ALU.mult), ["zim", "den"], ["zim"])
                sm("dve", lambda E: E.tensor_scalar(T["nzim"][:], T["zim"][:], -1.0, None, ALU.mult), ["zim"], ["nzim"])

                B4 = SB(es, "B4", [128, 2, 4 * 128], BF16)
                C4 = SB(es, "C4", [128, 2, 4 * 128], BF16)
                stg = [SB(es, f"s5stg{i}", [128, 4 * 128]) for i in range(4)]
                tC = SB(es, "tC", [128, 128])
                cosT = SB(es, "cosT", [128, 4, TB]); sinT = SB(es, "sinT", [128, 4, TB]); RM = SB(es, "RM", [128, 4, TB])
                ph = SB(es, "ph", [128, TB]); phk = SB(es, "phk", [128, TB]); phi = SB(es, "phi", [128, TB], I32)
                Us = [SB(es, f"Uc{i}", [128, NT], BF16) for i in range(2)]
                YACC = SB(es, "YACC", [128, NT])
                Gc = SB(es, "Gc", [128, NT], BF16)
                tmps = [{n: SB(es, f"w{q}_{n}", [128, TB]) for n in ("t1", "t2", "t3", "t4", "pre", "pim", "sre", "sim")} for q in range(3)]
                u2 = [SB(es, f"u2_{q}", [128, 2, TB]) for q in range(3)]
                sbf = [SB(es, f"sbf{q}", [128, 2, TB], BF16) for q in range(3)]
                carry = SB(es, "carry", [128, 4, NB, 4])
                it = 0
                for d in range(2):
                    for cc in range(8):
                        U = Us[it % 2]; un = f"Uc{it % 2}"
                        fw.dma("sp", U[:], UT[cc], reads=["dst_a"], writes=[un])
                        for q, key in enumerate(("s5_bre", "s5_bim", "s5_cre", "s5_cim")):
                            fw.dma("sp", stg[q][:], I[key][j, d][:, cc * 512:(cc + 1) * 512], writes=[f"s5stg{q}"])
                        fw.op("act", lambda E: E.copy(B4[:, 0, :], stg[0][:]), ["s5stg0"], ["B4"])
                        fw.op("act", lambda E: E.copy(B4[:, 1, :], stg[1][:]), ["s5stg1"], ["B4"])
                        for pi in range(4):
                            st = 4 * cc + pi
                            sl = slice(pi * 128, (pi + 1) * 128)
                            zr = T["zre"][:, d, st:st + 1]; zi = T["zim"][:, d, st:st + 1]; nzi = T["nzim"][:, d, st:st + 1]
                            fw.op("dve", lambda E, sl=sl, zi=zi: E.tensor_scalar(tC[:], stg[3][:, sl], zi, None, ALU.mult), ["s5stg3", "s5t_zim"], ["tC"])
                            fw.op("dve", lambda E, sl=sl, zr=zr: E.scalar_tensor_tensor(C4[:, 0, sl], stg[2][:, sl], zr, tC[:], ALU.mult, ALU.subtract),
                                  ["s5stg2", "s5t_zre", "tC"], ["C4"])
                            fw.op("dve", lambda E, sl=sl, zr=zr: E.tensor_scalar(tC[:], stg[3][:, sl], zr, None, ALU.mult), ["s5stg3", "s5t_zre"], ["tC"])
                            fw.op("dve", lambda E, sl=sl, nzi=nzi: E.scalar_tensor_tensor(C4[:, 1, sl], stg[2][:, sl], nzi, tC[:], ALU.mult, ALU.subtract),
                                  ["s5stg2", "s5t_nzim", "tC"], ["C4"])
                            th = T["th"][:, d, st:st + 1]
                            fw.op("dve", lambda E, th=th: E.tensor_scalar(ph[:], j1[:], th, None, ALU.mult), ["j1", "s5t_th"], ["ph"])
                            for dstT, dn, sh in ((sinT, "sinT", 0.0), (cosT, "cosT", float(np.pi / 2))):
                                fw.op("dve", lambda E, sh=sh: E.tensor_scalar(phk[:], ph[:], sh, None, ALU.add), ["ph"], ["phk"])
                                fw.op("dve", lambda E: E.tensor_scalar(phi[:], phk[:], 1.0 / TWO_PI, None, ALU.mult), ["phk"], ["phi"])
                                fw.op("dve", lambda E: E.tensor_copy(ph[:] if False else tmps[0]["t1"][:], phi[:]), ["phi"], ["w0_t1"])
                                fw.op("dve", lambda E: E.scalar_tensor_tensor(phk[:], tmps[0]["t1"][:], -TWO_PI, phk[:], ALU.mult, ALU.add), ["w0_t1", "phk"], ["phk"])
                                fw.op("act", lambda E, dstT=dstT, pi=pi: E.activation(dstT[:, pi, :], phk[:], AF.Sin), ["phk"], [dn])
                            rm = T["rmag"][:, d, st:st + 1]
                            fw.op("dve", lambda E, pi=pi, rm=rm: E.tensor_scalar(RM[:, pi, :], j1[:], 0.0, rm, ALU.mult, ALU.add), ["j1", "s5t_rmag"], ["RM"])
                        fw.op("pool", lambda E: E.memset(carry[:], 0.0), [], [f"carry{p_}_{b_}" for p_ in range(4) for b_ in range(NB)])
                        if d == 0:
                            fw.op("dve", lambda E, U=U, cc=cc: E.tensor_scalar(YACC[:], U[:], dT[:, cc:cc + 1], None, ALU.mult), [un, "dT"], ["YACC"])
                        else:
                            fw.dma("sp", YACC[:], YT[cc], reads=["YT"], writes=["YACC"])
                        k = 0
                        for b in range(NB):
                            for chi in range(4):
                                ch = chi if d == 0 else 3 - chi
                                T0 = b * L + ch * TB
                                if d == 0:
                                    rhs = U[:, T0:T0 + TB]; yv = YACC[:, T0:T0 + TB]
                                else:
                                    rhs = rev_ap(U[:], T0 + TB - 1, TB); yv = rev_ap(YACC[:], T0 + TB - 1, TB)
                                py = PS[6 + (k // 4) % 2]; pyn = PSN[6 + (k // 4) % 2]
                                for pi in range(4):
                                    q = k % 3
                                    W = tmps[q]; wn = lambda n, q=q: f"w{q}_{n}"
                                    pa = PS[2 * q]; pan = PSN[2 * q]; pb = PS[2 * q + 1]; pbn = PSN[2 * q + 1]
                                    sl = slice(pi * 128, (pi + 1) * 128)
                                    fw.op("pe", lambda E, pa=pa, sl=sl, rhs=rhs: E.matmul(pa[:], B4[:, 0, sl], rhs, start=True, stop=True), ["B4", un], [pan])
                                    fw.op("pe", lambda E, pb=pb, sl=sl, rhs=rhs: E.matmul(pb[:], B4[:, 1, sl], rhs, start=True, stop=True), ["B4", un], [pbn])
                                    cs = cosT[:, pi, :]; sn = sinT[:, pi, :]
                                    fw.op("dve", lambda E, W=W, pa=pa, cs=cs: E.tensor_tensor(W["t1"][:], pa[:], cs, ALU.mult), [pan, "cosT"], [wn("t1")])
                                    fw.op("dve", lambda E, W=W, pb=pb, sn=sn: E.tensor_tensor(W["t2"][:], pb[:], sn, ALU.mult), [pbn, "sinT"], [wn("t2")])
                                    fw.op("dve", lambda E, W=W, pb=pb, cs=cs: E.tensor_tensor(W["t3"][:], pb[:], cs, ALU.mult), [pbn, "cosT"], [wn("t3")])
                                    fw.op("dve", lambda E, W=W, pa=pa, sn=sn: E.tensor_tensor(W["t4"][:], pa[:], sn, ALU.mult), [pan, "sinT"], [wn("t4")])
                                    fw.op("dve", lambda E, W=W: E.tensor_tensor(W["pre"][:], W["t1"][:], W["t2"][:], ALU.add), [wn("t1"), wn("t2")], [wn("pre")])
                                    fw.op("dve", lambda E, W=W: E.tensor_tensor(W["pim"][:], W["t3"][:], W["t4"][:], ALU.subtract), [wn("t3"), wn("t4")], [wn("pim")])
                                    fw.op("dve", lambda E, W=W, pi=pi, b=b: E.tensor_tensor_scan(W["sre"][:], RM[:, pi, :], W["pre"][:], carry[:, pi, b, 0:1], ALU.mult, ALU.add),
                                          ["RM", wn("pre"), f"carry{pi}_{b}"], [wn("sre")])
                                    fw.op("dve", lambda E, W=W, pi=pi, b=b: E.tensor_tensor_scan(W["sim"][:], RM[:, pi, :], W["pim"][:], carry[:, pi, b, 1:2], ALU.mult, ALU.add),
                                          ["RM", wn("pim"), f"carry{pi}_{b}"], [wn("sim")])
                                    fw.op("dve", lambda E, W=W, cs=cs: E.tensor_tensor(W["t1"][:], W["sre"][:], cs, ALU.mult), [wn("sre"), "cosT"], [wn("t1")])
                                    fw.op("dve", lambda E, W=W, sn=sn: E.tensor_tensor(W["t2"][:], W["sim"][:], sn, ALU.mult), [wn("sim"), "sinT"], [wn("t2")])
                                    fw.op("dve", lambda E, W=W, sn=sn: E.tensor_tensor(W["t3"][:], W["sre"][:], sn, ALU.mult), [wn("sre"), "sinT"], [wn("t3")])
                                    fw.op("dve", lambda E, W=W, cs=cs: E.tensor_tensor(W["t4"][:], W["sim"][:], cs, ALU.mult), [wn("sim"), "cosT"], [wn("t4")])
                                    uu = u2[q]; uun = f"u2_{q}"
                                    fw.op("dve", lambda E, W=W, uu=uu: E.tensor_tensor(uu[:, 0, :], W["t1"][:], W["t2"][:], ALU.subtract), [wn("t1"), wn("t2")], [uun])
                                    fw.op("dve", lambda E, W=W, uu=uu: E.tensor_tensor(uu[:, 1, :], W["t3"][:], W["t4"][:], ALU.add), [wn("t3"), wn("t4")], [uun])
                                    sb_ = sbf[q]; sbn = f"sbf{q}"
                                    fw.op("act", lambda E, sb_=sb_, uu=uu: E.copy(sb_[:], uu[:]), [uun], [sbn])
                                    fw.op("pool", lambda E, uu=uu, pi=pi, b=b: E.tensor_copy(carry[:, pi, b, 0:2], uu[:, :, TB - 1]), [uun], [f"carry{pi}_{b}"])
                                    fw.op("pe", lambda E, py=py, sl=sl, sb_=sb_, pi=pi: E.matmul(py[:], C4[:, 0, sl], sb_[:, 0, :], start=(pi == 0), stop=False), ["C4", sbn], [pyn])
                                    fw.op("pe", lambda E, py=py, sl=sl, sb_=sb_, pi=pi: E.matmul(py[:], C4[:, 1, sl], sb_[:, 1, :], start=False, stop=(pi == 3)), ["C4", sbn], [pyn])
                                    k += 1
                                fw.op("dve", lambda E, py=py, yv=yv: E.tensor_tensor(yv, py[:], yv, ALU.add), [pyn, "YACC"], ["YACC"])
                                if "CARRYD" in dbg and d == 0 and cc == 0:
                                    CD2 = nc.dram_tensor(f"CARRYD_b{b}c{chi}", [128, 32], F32, kind="ExternalOutput").ap()
                                    fw.dma("sp", CD2, carry[:].rearrange("p a b c -> p (a b c)"), reads=["carry"], writes=["CARRYD"])
                                    if b == 1 and chi == 1:
                                        UD3 = nc.dram_tensor("CARRYD_U", [128, NT], BF16, kind="ExternalOutput").ap()
                                        fw.dma("sp", UD3, U[:], reads=[un], writes=["CARRYD"])
                                        for nm in ("t1", "pre", "sre"):
                                            UD4 = nc.dram_tensor("CARRYD_" + nm, [128, TB], F32, kind="ExternalOutput").ap()
                                            fw.dma("sp", UD4, tmps[(k - 1) % 3][nm][:], reads=[f"w{(k - 1) % 3}_{nm}"], writes=["CARRYD"])
                                        UD5 = nc.dram_tensor("CARRYD_cos", [128, 4 * TB], F32, kind="ExternalOutput").ap()
                                        fw.dma("sp", UD5, cosT[:].rearrange("p a b -> p (a b)"), reads=["cosT"], writes=["CARRYD"])
                                        UD6 = nc.dram_tensor("CARRYD_RM", [128, 4 * TB], F32, kind="ExternalOutput").ap()
                                        fw.dma("sp", UD6, RM[:].rearrange("p a b -> p (a b)"), reads=["RM"], writes=["CARRYD"])
                                    UD2 = nc.dram_tensor(f"CARRYDU_b{b}c{chi}", [128, 2 * TB], F32, kind="ExternalOutput").ap()
                                    fw.dma("sp", UD2, u2[(k - 1) % 3][:].rearrange("p a b -> p (a b)"), reads=[f"u2_{(k - 1) % 3}"], writes=["CARRYD"])
                        if "CARRYD" in dbg and d == 0 and cc < 2:
                            CD = nc.dram_tensor(f"CARRYD{cc}", [128, 32], F32, kind="ExternalOutput").ap()
                            fw.dma("sp", CD, carry[:].rearrange("p a b c -> p (a b c)"), reads=["carry"], writes=["CARRYD"])
                        if d == 0:
                            fw.dma("sp", YT[cc], YACC[:], reads=["YACC"], writes=["YT"])
                        else:
                            fw.op("act", lambda E: E.activation(Gc[:], YACC[:], AF.Gelu), ["YACC"], ["Gc"])
                            fw.dma("act", GT[cc], Gc[:], reads=["Gc"], writes=["GT"])
                        it += 1

        def phase_s5_out(j, li):
            with ExitStack() as es:
                Wg = SB(es, "Wg", [128, 8, D], BF16); Wo = SB(es, "Wo", [128, 8, D], BF16)
                load_w_bf16(es, Wg, "Wg", I["s5_w_glu"][j], 8, D, "wg")
                load_w_bf16(es, Wo, "Wo", I["s5_w_out"][j], 8, D, "wo")
                bg = SB(es, "bg", [128, 8])
                fw.dma("sp", bg[:], I["s5_b_gluT"][:, j], writes=["bg"])
                G = SB(es, "Gb", [128, 8, TB], BF16); SZ = SB(es, "SZb", [128, 8, TB], BF16); XB = SB(es, "XBo", [128, 8, TB])
                Y2 = SB(es, "Y2", [128, 8, TB], BF16)
                sg = [SB(es, f"sg{i}", [128, TB]) for i in range(2)]
                for tb in range(NTB):
                    b = tb // (NTB // NB); ts_ = slice(tb * TB, (tb + 1) * TB)
                    fw.dma("sp", G[:], GT[:, :, ts_].rearrange("k p t -> p k t"), reads=["GT"], writes=["Gb"])
                    fw.dma("sp", SZ[:], SZT[0:8, :, ts_].rearrange("k p t -> p k t"), reads=["dst_b"], writes=["SZb"])
                    fw.dma("sp", XB[:], XT[:, :, ts_].rearrange("k p t -> p k t"), reads=["XT"], writes=["XBo"])
                    for mt in range(8):
                        p = PS[mt % 4]; pn = PSN[mt % 4]; s_ = sg[mt % 2]; sn = f"sg{mt % 2}"
                        for kc in range(8):
                            fw.op("pe", lambda E, p=p, kc=kc, mt=mt: E.matmul(p[:], Wg[:, kc, mt * 128:(mt + 1) * 128], G[:, kc, :], start=(kc == 0), stop=(kc == 7)),
                                  ["Wg", "Gb"], [pn])
                        fw.op("act", lambda E, p=p, s_=s_, mt=mt: E.activation(s_[:], p[:], AF.Sigmoid, bias=bg[:, mt:mt + 1]), [pn, "bg"], [sn])
                        fw.op("dve", lambda E, s_=s_, mt=mt: E.tensor_tensor(s_[:], s_[:], G[:, mt, :], ALU.mult), [sn, "Gb"], [sn])
                        fw.op("dve", lambda E, s_=s_, mt=mt: E.tensor_tensor(Y2[:, mt, :], s_[:], SZ[:, mt, :], ALU.mult), [sn, "SZb"], ["Y2"])
                    for mt in range(8):
                        p = PS[4 + mt % 4]; pn = PSN[4 + mt % 4]
                        for kc in range(8):
                            fw.op("pe", lambda E, p=p, kc=kc, mt=mt: E.matmul(p[:], Wo[:, kc, mt * 128:(mt + 1) * 128], Y2[:, kc, :], start=(kc == 0), stop=(kc == 7)),
                                  ["Wo", "Y2"], [pn])
                        fw.op("dve", lambda E, p=p, mt=mt, b=b: E.scalar_tensor_tensor(XB[:, mt, :], p[:], MOD[:, li, 16 + mt, b:b + 1], XB[:, mt, :], ALU.mult, ALU.add),
                              [pn, "MOD", "XBo"], ["XBo"])
                    fw.dma("act", XT[:, :, ts_].rearrange("k p t -> p k t"), XB[:], reads=["XBo"], writes=["XT"])

        def phase_ml_qkv(j, GATES):
            with ExitStack() as es:
                cw = SB(es, "cw", [128, 16, 5]); cb = SB(es, "cb", [128, 16]); bgr = SB(es, "bgr", [128, 16])
                fw.dma("sp", cw[:], I["ml_convT"][:, j], writes=["cw"])
                fw.dma("sp", cb[:], I["ml_convbT"][:, j], writes=["cb"])
                fw.dma("sp", bgr[:], I["ml_bg_rep"][:, j], writes=["bgr"])
                Wq = SB(es, "Wq", [128, 1, 2048], BF16); Wk = SB(es, "Wk", [128, 1, 2048], BF16); Wv = SB(es, "Wv", [128, 1, 2048], BF16)
                load_w_bf16(es, Wq, "Wq", I["ml_wq_bd"][j], 1, 2048, "wq")
                load_w_bf16(es, Wk, "Wk", I["ml_wk_bd"][j], 1, 2048, "wk")
                load_w_bf16(es, Wv, "Wv", I["ml_wv_bd"][j], 1, 2048, "wv")
                wgs = SB(es, "wgs", [128, 768]); wg = SB(es, "wgb", [128, 768], BF16)
                fw.dma("sp", wgs[:], I["ml_wg"][j], writes=["wgs"])
                fw.op("dve", lambda E: E.tensor_copy(wg[:], wgs[:]), ["wgs"], ["wgb"])
                XMH = [SB(es, f"XMH{i}", [128, L + 4], BF16) for i in range(2)]
                for i in range(2):
                    fw.op("pool", lambda E, i=i: E.memset(XMH[i][:], 0.0), [], [f"XMH{i}"])
                acc = SB(es, "cacc", [128, L]); XC = SB(es, "XCc", [128, L], BF16)
                QTB = SB(es, "QTB", [128, L], BF16); KTB = SB(es, "KTB", [128, L], BF16); VTB = SB(es, "VTB", [128, L], BF16)
                KTK = SB(es, "KTK", [128, 16, 128], BF16); VTK = SB(es, "VTK", [128, 16, 128], BF16)
                it = 0
                for cc in range(16):
                    csl = slice(cc * 128, (cc + 1) * 128)
                    for b in range(NB):
                        xm = XMH[it % 2]; xn = f"XMH{it % 2}"; bs = slice(b * L, (b + 1) * L)
                        fw.dma("sp", xm[:, 2:2 + L], XMT[cc][:, bs], reads=["dst_a"], writes=[xn])
                        ce = "dve"
                        fw.op(ce, lambda E, xm=xm, cc=cc: E.tensor_scalar(acc[:], xm[:, 0:L], cw[:, cc, 0:1], cb[:, cc:cc + 1], ALU.mult, ALU.add), [xn, "cw", "cb"], ["cacc"])
                        for kk in range(1, 5):
                            fw.op(ce, lambda E, xm=xm, cc=cc, kk=kk: E.scalar_tensor_tensor(acc[:], xm[:, kk:kk + L], cw[:, cc, kk:kk + 1], acc[:], ALU.mult, ALU.add),
                                  [xn, "cw", "cacc"], ["cacc"])
                        fw.op("act", lambda E: E.activation(XC[:], acc[:], AF.Silu), ["cacc"], ["XCc"])
                        fw.dma("act", XCT[cc][:, bs], XC[:], reads=["XCc"], writes=["XCT"])
                        for q4 in range(4):
                            qs = slice(q4 * TB, (q4 + 1) * TB)
                            for wi, (Wm, wn, src, srn, dst, dn) in enumerate(((Wq, "Wq", XC[:, qs], "XCc", QTB, "QTB"), (Wk, "Wk", XC[:, qs], "XCc", KTB, "KTB"),
                                                                           (Wv, "Wv", xm[:, 2 + q4 * TB:2 + (q4 + 1) * TB], xn, VTB, "VTB"))):
                                p = PS[wi]; pn = PSN[wi]
                                fw.op("pe", lambda E, p=p, Wm=Wm, src=src, csl=csl: E.matmul(p[:], Wm[:, 0, csl], src, start=True, stop=True), [wn, srn], [pn])
                                copy_op("dve" if wi != 1 else "act", dst[:, qs], p[:], [pn], [dn])
                        fw.dma("act", QT[cc][:, bs], QTB[:], reads=["QTB"], writes=["QT"])
                        fw.dma("act", KT[cc][:, bs], KTB[:], reads=["KTB"], writes=["KT"])
                        for t4 in range(4):
                            pk = PS[3]; pv = PS[4]
                            for tq in range(4):
                                tt = t4 * 4 + tq; tsl = slice(tt * 128, (tt + 1) * 128); osl = slice(tq * 128, (tq + 1) * 128)
                                fw.op("pe", lambda E, tsl=tsl, osl=osl, csl=csl: E.matmul(pk[:, osl], XC[:, tsl], Wk[:, 0, csl], start=True, stop=True), ["XCc", "Wk"], ["ps3"])
                                fw.op("pe", lambda E, tsl=tsl, osl=osl, xm=xm, tt=tt, csl=csl: E.matmul(pv[:, osl], xm[:, 2 + tt * 128:2 + (tt + 1) * 128], Wv[:, 0, csl], start=True, stop=True),
                                      [xn, "Wv"], ["ps4"])
                                gsl = slice((b * 16 + tt) * 16, (b * 16 + tt + 1) * 16)
                                for wi, (src, srn) in enumerate(((QTB, "QTB"), (KTB, "KTB"), (VTB, "VTB"))):
                                    fw.op("pe", lambda E, src=src, tsl=tsl, gsl=gsl, wi=wi, cc=cc, b=b, tt=tt: E.matmul(
                                        PS[6][:, gsl], src[:, tsl], wg[:, (wi * 16 + cc) * 16:(wi * 16 + cc + 1) * 16],
                                        start=(cc == 0 and wi == 0 and b == 0 and tt == 0), stop=(cc == 15 and wi == 2)), [srn, "wgb"], ["ps6"])
                            copy_op("dve", KTK[:, t4 * 4:(t4 + 1) * 4, :].rearrange("p a b -> p (a b)"), pk[:], ["ps3"], ["KTK"])
                            copy_op("act", VTK[:, t4 * 4:(t4 + 1) * 4, :].rearrange("p a b -> p (a b)"), pv[:], ["ps4"], ["VTK"])
                        fw.dma("act", KTOK[bs, csl].rearrange("(t p) c -> p t c", p=128), KTK[:], reads=["KTK"], writes=["KTOK"])
                        fw.dma("act", VTOK[bs, csl].rearrange("(t p) c -> p t c", p=128), VTK[:], reads=["VTK"], writes=["VTOK"])
                        it += 1
                fw.op("dve", lambda E: E.tensor_tensor(GATES[:], PS[6][:].rearrange("p (a b) -> p a b", b=16),
                                                       bgr[:].unsqueeze(1).to_broadcast([128, 32, 16]), ALU.add), ["ps6", "bgr"], ["GATES"])

        def phase_ml_attn(j, GATES):
            SC = float(512 ** -0.5)
            with ExitStack() as es:
                def G3(name, last=16, dt=F32):
                    return SB(es, name, [128, 32, last], dt)
                E1 = G3("E1"); LF = G3("LF"); BWF = G3("BWF"); BWB = G3("BWB"); TOT = G3("TOT"); OFF = G3("OFF")
                fw.op("act", lambda E: E.activation(E1[:], GATES[:], AF.Exp, scale=-1.0), ["GATES"], ["E1"])
                fw.op("act", lambda E: E.activation(E1[:], E1[:], AF.Ln, bias=1.0), ["E1"], ["E1"])
                fw.op("dve", lambda E: E.tensor_scalar(LF[:], E1[:], -1.0, None, ALU.mult), ["E1"], ["LF"])
                LFf = LF[:].rearrange("p a b -> p (a b)")
                fw.op("pe", lambda E: E.matmul(PS[0][:], triF[:], LFf, start=True, stop=True), ["triF", "LF"], ["ps0"])
                fw.op("pe", lambda E: E.matmul(PS[1][:], triB[:], LFf, start=True, stop=True), ["triB", "LF"], ["ps1"])
                fw.op("pe", lambda E: E.matmul(PS[2][:], ones[:], LFf, start=True, stop=True), ["ones", "LF"], ["ps2"])
                fw.op("dve", lambda E: E.tensor_copy(BWF[:].rearrange("p a b -> p (a b)"), PS[0][:]), ["ps0"], ["BWF"])
                fw.op("act", lambda E: E.copy(BWB[:].rearrange("p a b -> p (a b)"), PS[1][:]), ["ps1"], ["BWB"])
                fw.op("dve", lambda E: E.tensor_copy(TOT[:].rearrange("p a b -> p (a b)"), PS[2][:]), ["ps2"], ["TOT"])
                for (BW, bwn, fwd) in ((BWF, "BWF", True), (BWB, "BWB", False)):
                    fw.op("pool", lambda E: E.memset(OFF[:], 0.0), [], ["OFF"])
                    for b in range(NB):
                        rng = range(1, 16) if fwd else range(14, -1, -1)
                        for kt in rng:
                            g = b * 16 + kt; gp = g - 1 if fwd else g + 1
                            fw.op("pool", lambda E, g=g, gp=gp: E.tensor_tensor(OFF[:, g, :], OFF[:, gp, :], TOT[:, gp, :], ALU.add), ["OFF", "TOT"], ["OFF"])
                    fw.op("pool", lambda E, BW=BW: E.tensor_tensor(BW[:], BW[:], OFF[:], ALU.add), [bwn, "OFF"], [bwn])
                AALL = SB(es, "AALL", [128, 2, 32, 4]); CM = SB(es, "CM", [128, 2, 32, 4]); TM = SB(es, "TM", [128, 2, 32, 4])
                PM = SB(es, "PM", [128, 2, 32, 4]); MM = SB(es, "MM", [128, 2, 32, 4]); EM = SB(es, "EM", [128, 2, 32, 4])
                fw.op("dve", lambda E: E.tensor_tensor(AALL[:, 0], GATES[:, :, 0:4], BWF[:, :, 4:8], ALU.subtract), ["GATES", "BWF"], ["AALL"])
                fw.op("dve", lambda E: E.tensor_tensor(AALL[:, 1], GATES[:, :, 8:12], BWB[:, :, 12:16], ALU.subtract), ["GATES", "BWB"], ["AALL"])
                DG = [SB(es, f"DG{i}", [128, 4, 128]) for i in range(2)]
                AM = [SB(es, f"AM{i}", [128, 4, 128]) for i in range(2)]
                identb = ident[:].unsqueeze(1).to_broadcast([128, 4, 128])
                k = 0
                for d in range(2):
                    mk = maskF if d == 0 else maskB
                    mkn = "maskF" if d == 0 else "maskB"
                    for g in range(32):
                        q = k % 2; dg = DG[q]; dgn = f"DG{q}"; am = AM[q]; amn = f"AM{q}"; p = PS[q]; pn = PSN[q]
                        fw.op("pool", lambda E, dg=dg, d=d, g=g: E.tensor_tensor(dg[:], identb, AALL[:, d, g, :].unsqueeze(2).to_broadcast([128, 4, 128]), ALU.mult),
                              ["ident", "AALL"], [dgn])
                        fw.op("pe", lambda E, p=p, dg=dg: E.matmul(p[:], ones[:], dg[:].rearrange("p a b -> p (a b)"), start=True, stop=True), ["ones", dgn], [pn])
                        p3 = p[:].rearrange("p (a b) -> p a b", b=128)
                        fw.op("dve", lambda E, am=am, p3=p3, mk=mk: E.tensor_tensor(am[:], p3, mk[:].unsqueeze(1).to_broadcast([128, 4, 128]), ALU.add), [pn, mkn], [amn])
                        fw.op("dve", lambda E, am=am, d=d, g=g: E.tensor_reduce(CM[:, d, g, :], am[:], AX.X, ALU.max), [amn], ["CM"])
                        fw.op("dve", lambda E, p3=p3, d=d, g=g: E.tensor_reduce(TM[:, d, g, :], p3, AX.X, ALU.max), [pn], ["TM"])
                        k += 1
                fw.op("pool", lambda E: E.memset(PM[:], 0.0), [], ["PM"])
                for d in range(2):
                    for b in range(NB):
                        rng = range(1, 16) if d == 0 else range(14, -1, -1)
                        for kt in rng:
                            g = b * 16 + kt; gp = g - 1 if d == 0 else g + 1
                            fw.op("dve", lambda E, d=d, g=g, gp=gp: E.tensor_tensor(PM[:, d, g, :], PM[:, d, gp, :], TM[:, d, gp, :], ALU.max), ["PM", "TM"], ["PM"])
                fw.op("dve", lambda E: E.tensor_tensor(MM[:], CM[:], PM[:], ALU.max), ["CM", "PM"], ["MM"])
                fw.op("dve", lambda E: E.tensor_tensor(EM[:, 0], MM[:, 0], BWF[:, :, 4:8], ALU.add), ["MM", "BWF"], ["EM"])
                fw.op("dve", lambda E: E.tensor_tensor(EM[:, 1], MM[:, 1], BWB[:, :, 12:16], ALU.add), ["MM", "BWB"], ["EM"])
                fw.op("act", lambda E: E.activation(EM[:], EM[:], AF.Exp, scale=-1.0), ["EM"], ["EM"])

                QH = SB(es, "QH", [128, 4, L], BF16); KH = SB(es, "KH", [128, 4, L], BF16); VH = SB(es, "VH", [128, 16, 512], BF16)
                HACC = SB(es, "HACC", [128, 16, 512]); MBt = SB(es, "MBt", [128, L]); HNF = SB(es, "HNF", [128, 4, L], BF16)
                WT = [SB(es, f"WT{i}", [128, TB]) for i in range(2)]
                PT = [SB(es, f"PT{i}", [128, TB], BF16) for i in range(2)]
                sm4 = SB(es, "sm4", [128, 8, 4]); st6 = SB(es, "st6", [128, 6]); mv = SB(es, "mv", [128, 2]); hn = SB(es, "hnt", [128, 512])
                for b in range(NB):
                    bs = slice(b * L, (b + 1) * L)
                    for hd in range(4):
                        fw.dma("sp", QH[:], QT[hd * 4:(hd + 1) * 4, :, bs].rearrange("k p t -> p k t"), reads=["QT"], writes=["QH"])
                        fw.dma("sp", KH[:], KT[hd * 4:(hd + 1) * 4, :, bs].rearrange("k p t -> p k t"), reads=["KT"], writes=["KH"])
                        fw.dma("sp", VH[:], VTOK[bs, hd * 512:(hd + 1) * 512].rearrange("(t p) e -> p t e", p=128), reads=["VTOK"], writes=["VH"])
                        for d in range(2):
                            tmask = triF if d == 0 else triB
                            tmn = "triF" if d == 0 else "triB"
                            for q4 in range(4):
                                dg = DG[q4 % 2]; dgn = f"DG{q4 % 2}"
                                g0 = b * 16 + q4 * 4
                                fw.op("pool", lambda E, dg=dg, d=d, g0=g0, hd=hd: E.tensor_tensor(dg[:], identb, MM[:, d, g0:g0 + 4, hd:hd + 1].to_broadcast([128, 4, 128]), ALU.mult),
                                      ["ident", "MM"], [dgn])
                                fw.op("pe", lambda E, dg=dg: E.matmul(PS[7][:], ones[:], dg[:].rearrange("p a b -> p (a b)"), start=True, stop=True), ["ones", dgn], ["ps7"])
                                fw.op("act", lambda E, q4=q4: E.copy(MBt[:, q4 * TB:(q4 + 1) * TB], PS[7][:]), ["ps7"], ["MBt"])
                            kk = 0
                            for Q in range(4):
                                keys = range(0, 4 * Q + 4) if d == 0 else range(4 * Q, 16)
                                qsl = slice(Q * TB, (Q + 1) * TB)
                                rs_started = [False]
                                for tk in keys:
                                    w = kk % 2; ps_s = PS[5 + w]; psn = PSN[5 + w]; wt = WT[w]; wtn = f"WT{w}"; pt = PT[w]; ptn = f"PT{w}"
                                    ksl = slice(tk * 128, (tk + 1) * 128)
                                    for kc in range(4):
                                        fw.op("pe", lambda E, ps_s=ps_s, kc=kc, ksl=ksl, qsl=qsl: E.matmul(ps_s[:], KH[:, kc, ksl], QH[:, kc, qsl], start=(kc == 0), stop=(kc == 3)),
                                              ["KH", "QH"], [psn])
                                    acol = AALL[:, d, b * 16 + tk, hd:hd + 1]
                                    fw.op("act", lambda E, wt=wt, qsl=qsl, acol=acol: E.activation(wt[:], MBt[:, qsl], AF.Exp, bias=acol, scale=-1.0), ["MBt", "AALL"], [wtn])
                                    fw.op("dve", lambda E, pt=pt, ps_s=ps_s, wt=wt: E.scalar_tensor_tensor(pt[:], ps_s[:], SC, wt[:], ALU.mult, ALU.mult), [psn, wtn], [ptn])
                                    for qs in range(4):
                                        tq = 4 * Q + qs
                                        valid = (tk <= tq) if d == 0 else (tk >= tq)
                                        if not valid:
                                            continue
                                        sub = slice(qs * 128, (qs + 1) * 128)
                                        if tk == tq:
                                            fw.op("pool", lambda E, pt=pt, sub=sub, tmask=tmask: E.tensor_tensor(pt[:, sub], pt[:, sub], tmask[:], ALU.mult), [ptn, tmn], [ptn])
                                        first = (tk == 0) if d == 0 else (tk == tq)
                                        last = (tk == tq) if d == 0 else (tk == 15)
                                        fw.op("pe", lambda E, qs=qs, pt=pt, sub=sub, tk=tk, first=first, last=last: E.matmul(PS[qs][:], pt[:, sub], VH[:, tk, :], start=first, stop=last),
                                              [ptn, "VH"], [PSN[qs]])
                                        rfirst = not rs_started[0]
                                        rs_started[0] = True
                                        fw.op("pe", lambda E, qs=qs, pt=pt, sub=sub, rfirst=rfirst, last=last: E.matmul(PS[4][:, qs:qs + 1], pt[:, sub], onesb[:, 0:1], start=rfirst, stop=last),
                                              [ptn, "onesb"], ["ps4"])
                                    kk += 1
                                for qs in range(4):
                                    tq = 4 * Q + qs; g = b * 16 + tq
                                    c0 = sm4[:, qs, 0:1]; c1 = sm4[:, qs, 1:2]
                                    fw.op("dve", lambda E, qs=qs, c1=c1: E.tensor_scalar(c1, PS[4][:, qs:qs + 1], -1.0, None, ALU.mult), ["ps4"], ["sm4"])
                                    fw.op("dve", lambda E, qs=qs, c0=c0, c1=c1: E.tensor_tensor(c0, PS[4][:, qs:qs + 1], c1, ALU.max), ["ps4", "sm4"], ["sm4"])
                                    fw.op("dve", lambda E, c0=c0, d=d, g=g, hd=hd: E.tensor_tensor(c0, c0, EM[:, d, g, hd:hd + 1], ALU.max), ["sm4", "EM"], ["sm4"])
                                    fw.op("dve", lambda E, c0=c0, c1=c1: E.reciprocal(c1, c0), ["sm4"], ["sm4"])
                                    if d == 0:
                                        fw.op("act", lambda E, qs=qs, tq=tq, c1=c1: E.activation(HACC[:, tq, :], PS[qs][:], AF.Copy, scale=c1), [PSN[qs], "sm4"], ["HACC"])
                                    else:
                                        fw.op("dve", lambda E, qs=qs, tq=tq, c1=c1: E.scalar_tensor_tensor(HACC[:, tq, :], PS[qs][:], c1, HACC[:, tq, :], ALU.mult, ALU.add),
                                              [PSN[qs], "sm4", "HACC"], ["HACC"])
                        for tt in range(16):
                            fw.op("dve", lambda E, tt=tt: E.bn_stats(st6[:], HACC[:, tt, :]), ["HACC"], ["st6"])
                            fw.op("dve", lambda E: E.bn_aggr(mv[:], st6[:]), ["st6"], ["mv"])
                            fw.op("act", lambda E: E.activation(mv[:, 1:2], mv[:, 1:2], AF.Sqrt, bias=1e-5, scale=1.0), ["mv"], ["mv"])
                            fw.op("dve", lambda E: E.reciprocal(mv[:, 1:2], mv[:, 1:2]), ["mv"], ["mv"])
                            fw.op("dve", lambda E, tt=tt: E.tensor_scalar(hn[:], HACC[:, tt, :], mv[:, 0:1], mv[:, 1:2], ALU.subtract, ALU.mult), ["HACC", "mv"], ["hnt"])
                            p = PS[5 + tt % 2]; pn = PSN[5 + tt % 2]
                            for es_ in range(4):
                                fw.op("pe", lambda E, p=p, es_=es_: E.transpose(p[:, es_ * 128:(es_ + 1) * 128], hn[:, es_ * 128:(es_ + 1) * 128], ident[:]), ["hnt", "ident"], [pn])
                            fw.op("act", lambda E, p=p, tt=tt: E.copy(HNF[:, :, tt * 128:(tt + 1) * 128], p[:].rearrange("p (a b) -> p a b", b=128)), [pn], ["HNF"])
                        fw.dma("act", HNT[hd * 4:(hd + 1) * 4, :, bs].rearrange("k p t -> p k t"), HNF[:], reads=["HNF"], writes=["HNT"])

        def phase_ml_out(j, li):
            with ExitStack() as es:
                Wo = SB(es, "Wo2", [128, 16, D], BF16)
                load_w_bf16(es, Wo, "Wo2", I["ml_w_out"][j], 16, D, "wo2")
                gn = SB(es, "gn", [128, 16]); sk = SB(es, "sk", [128, 16])
                fw.dma("sp", gn[:], I["ml_gnT"][:, j], writes=["gn"])
                fw.dma("sp", sk[:], I["ml_skipT"][:, j], writes=["sk"])
                HN = SB(es, "HNb", [128, 16, TB], BF16); XCb = SB(es, "XCb", [128, 16, TB], BF16); SZ = SB(es, "SZb2", [128, 16, TB], BF16)
                Y = SB(es, "Yb", [128, 16, TB], BF16); XB = SB(es, "XBo2", [128, 8, TB])
                tm = [SB(es, f"tm{i}", [128, TB]) for i in range(2)]
                for tb in range(NTB):
                    b = tb // (NTB // NB); ts_ = slice(tb * TB, (tb + 1) * TB)
                    fw.dma("sp", HN[:], HNT[:, :, ts_].rearrange("k p t -> p k t"), reads=["HNT"], writes=["HNb"])
                    fw.dma("sp", XCb[:], XCT[:, :, ts_].rearrange("k p t -> p k t"), reads=["XCT"], writes=["XCb"])
                    fw.dma("sp", SZ[:], SZT[:, :, ts_].rearrange("k p t -> p k t"), reads=["dst_b"], writes=["SZb2"])
                    fw.dma("sp", XB[:], XT[:, :, ts_].rearrange("k p t -> p k t"), reads=["XT"], writes=["XBo2"])
                    for cc in range(16):
                        t_ = tm[cc % 2]; tn_ = f"tm{cc % 2}"
                        fw.op("dve", lambda E, t_=t_, cc=cc: E.tensor_scalar(t_[:], XCb[:, cc, :], sk[:, cc:cc + 1], None, ALU.mult), ["XCb", "sk"], [tn_])
                        fw.op("dve", lambda E, t_=t_, cc=cc: E.scalar_tensor_tensor(t_[:], HN[:, cc, :], gn[:, cc:cc + 1], t_[:], ALU.mult, ALU.add), ["HNb", "gn", tn_], [tn_])
                        fw.op("dve", lambda E, t_=t_, cc=cc: E.tensor_tensor(Y[:, cc, :], t_[:], SZ[:, cc, :], ALU.mult), [tn_, "SZb2"], ["Yb"])
                    for mt in range(8):
                        p = PS[mt % 4]; pn = PSN[mt % 4]
                        for kc in range(16):
                            fw.op("pe", lambda E, p=p, kc=kc, mt=mt: E.matmul(p[:], Wo[:, kc, mt * 128:(mt + 1) * 128], Y[:, kc, :], start=(kc == 0), stop=(kc == 15)),
                                  ["Wo2", "Yb"], [pn])
                        fw.op("dve", lambda E, p=p, mt=mt, b=b: E.scalar_tensor_tensor(XB[:, mt, :], p[:], MOD[:, li, 16 + mt, b:b + 1], XB[:, mt, :], ALU.mult, ALU.add),
                              [pn, "MOD", "XBo2"], ["XBo2"])
                    fw.dma("act", XT[:, :, ts_].rearrange("k p t -> p k t"), XB[:], reads=["XBo2"], writes=["XT"])

        def phase_final():
            with ExitStack() as es:
                XB = SB(es, "XBf", [128, 8, TB]); FO = SB(es, "FO", [128, 8, TB])
                sq = SB(es, "sq", [128, 4, TB]); rs = SB(es, "rs", [128, TB])
                ot = [SB(es, f"ot{i}", [128, D]) for i in range(2)]
                k = 0
                for tb in range(NTB):
                    ts_ = slice(tb * TB, (tb + 1) * TB)
                    fw.dma("sp", XB[:], XT[:, :, ts_].rearrange("k p t -> p k t"), reads=["XT"], writes=["XBf"])
                    norm_block(XB, "XBf", FO, "FO", (sq, rs), 0, 0, final=True)
                    for t4 in range(4):
                        o = ot[k % 2]; on = f"ot{k % 2}"
                        for h in range(2):
                            p = PS[h]; pn = PSN[h]
                            for q in range(4):
                                kc = h * 4 + q
                                fw.op("pe", lambda E, p=p, q=q, kc=kc, t4=t4: E.transpose(p[:, q * 128:(q + 1) * 128], FO[:, kc, t4 * 128:(t4 + 1) * 128], ident[:]),
                                      ["FO", "ident"], [pn])
                            copy_op("dve" if h == 0 else "act", o[:, h * 512:(h + 1) * 512], p[:], [pn], [on])
                        r0 = tb * TB + t4 * 128
                        fw.dma("sp", OUT[r0:r0 + 128, :], o[:], reads=[on], writes=["OUT"])
                        k += 1

        stages = []
        stages.append(("mod", phase_mod))
        stages.append(("in", phase_in))
        for li in range(4):
            j = li // 2
            if li % 2 == 0:
                stages.append((f"inproj{li}", lambda li=li, j=j: phase_inproj(li, I["s5_w_in"][j], 2 * D, UT, SZT)))
                stages.append((f"ssm{li}", lambda j=j: phase_s5_ssm(j)))
                stages.append((f"s5out{li}", lambda li=li, j=j: phase_s5_out(j, li)))
            else:
                stages.append((f"inproj{li}", lambda li=li, j=j: phase_inproj(li, I["ml_w_in"][j], 4 * D, XMT, SZT)))
                stages.append((f"qkv{li}", lambda j=j: phase_ml_qkv(j, GATES)))
                stages.append((f"attn{li}", lambda j=j: phase_ml_attn(j, GATES)))
                stages.append((f"mlout{li}", lambda li=li, j=j: phase_ml_out(j, li)))
        stages.append(("final", phase_final))
        GATES = SB(top, "GATES", [128, 32, 16])
        for name, fn in stages:
            if dbg_stop and name in dbg_stop:
                break
            fn()
            fw.barrier()
        if "MODD" in dbg:
            MODD = nc.dram_tensor("MODD", [128, 4 * 24 * NB], F32, kind="ExternalOutput").ap()
            fw.dma("sp", MODD, MOD[:].rearrange("p a b c -> p (a b c)"), reads=["MOD"], writes=["MODD"])
        if "GATESD" in dbg:
            GD = nc.dram_tensor("GATESD", [128, 512], F32, kind="ExternalOutput").ap()
            fw.dma("sp", GD, GATES[:].rearrange("p a b -> p (a b)"), reads=["GATES"], writes=["GATESD"])
        fw.finish(["OUT", "MODD", "GATESD", "XT", "dst_a", "dst_b", "GT", "YT", "XCT", "QT", "KT", "KTOK", "VTOK", "HNT"], "sp")
        fw.run_block()
    print("n_inst", fw.n_inst, {e: len(fw.prog[e]) for e in fw.prog})
    return nc


def _prep_shared(inp):
    f = np.float32
    S = {}
    S["ada_w"] = np.ascontiguousarray(inp["ada_w"], dtype=f)
    S["ada_bT"] = np.ascontiguousarray(inp["ada_b"].reshape(4, 24, 128).transpose(2, 0, 1), dtype=f)
    S["norm_gT"] = np.ascontiguousarray(inp["norm_g"].reshape(4, 8, 128).transpose(2, 0, 1), dtype=f)
    S["final_gT"] = np.ascontiguousarray(inp["final_g"].reshape(8, 128).T, dtype=f)
    S["ident"] = np.eye(128, dtype=f)
    S["ones"] = np.ones((128, 128), f)
    S["j1"] = np.ascontiguousarray(np.broadcast_to(np.arange(1, TB + 1, dtype=f), (128, TB)))
    t = np.arange(128)[:, None]; s = np.arange(128)[None, :]
    S["maskF"] = np.where(s <= t, 0.0, -30000.0).astype(f)
    S["maskB"] = np.where(s >= t, 0.0, -30000.0).astype(f)
    S["triF"] = (t <= s).astype(f)
    S["triB"] = (t >= s).astype(f)
    for k in ("s5_w_in", "s5_w_glu", "s5_w_out", "ml_w_in", "ml_w_out"):
        S[k] = np.ascontiguousarray(inp[k], dtype=f)
    S["s5_b_gluT"] = np.ascontiguousarray(inp["s5_b_glu"].reshape(2, 8, 128).transpose(2, 0, 1), dtype=f)
    S["s5_dT"] = np.ascontiguousarray(inp["s5_d"].reshape(2, 8, 128).transpose(2, 0, 1), dtype=f)

    def st_layout(a):
        a = a.reshape(2, 2, 32, 2, 64)
        return np.ascontiguousarray(a.transpose(3, 4, 0, 1, 2).reshape(128, 2, 2, 32), dtype=f)
    S["s5_lre"] = st_layout(inp["s5_lam_re"])
    S["s5_lim"] = st_layout(inp["s5_lam_im"])
    S["s5_ldt"] = st_layout(np.broadcast_to(inp["s5_log_dt"][..., None], (2, 2, 64, 64)))

    def b_layout(bm):
        out = np.zeros((2, 2, 128, 32, 128), f)
        for st in range(32):
            pi = st % 4
            for gl in range(2):
                g = 2 * st + gl
                k0 = (2 * pi + gl) * 16
                out[:, :, k0:k0 + 16, st, gl * 64:(gl + 1) * 64] = bm[:, :, g].transpose(0, 1, 3, 2)
        return out.reshape(2, 2, 128, 32 * 128)

    def c_layout(cm):
        out = np.zeros((2, 2, 128, 32, 128), f)
        for st in range(32):
            pi = st % 4
            for gl in range(2):
                g = 2 * st + gl
                m0 = (2 * pi + gl) * 16
                out[:, :, gl * 64:(gl + 1) * 64, st, m0:m0 + 16] = cm[:, :, g].transpose(0, 1, 3, 2)
        return out.reshape(2, 2, 128, 32 * 128)
    S["s5_bre"] = b_layout(np.asarray(inp["s5_b_re"], f)); S["s5_bim"] = b_layout(np.asarray(inp["s5_b_im"], f))
    S["s5_cre"] = c_layout(np.asarray(inp["s5_c_re"], f)); S["s5_cim"] = c_layout(np.asarray(inp["s5_c_im"], f))
    S["ml_convT"] = np.ascontiguousarray(inp["ml_conv_w"].reshape(2, 5, 16, 128).transpose(3, 0, 2, 1), dtype=f)
    S["ml_convbT"] = np.ascontiguousarray(inp["ml_conv_b"].reshape(2, 16, 128).transpose(2, 0, 1), dtype=f)

    def bd_layout(w):
        out = np.zeros((2, 128, 16, 128), f)
        wr = np.asarray(w, f).reshape(2, 16, 32, 4, 4)
        for n in range(32):
            out[:, 4 * n:4 * n + 4, :, 4 * n:4 * n + 4] = wr[:, :, n].transpose(0, 2, 1, 3)
        return out.reshape(2, 128, 16 * 128)
    S["ml_wq_bd"] = bd_layout(inp["ml_w_q"]); S["ml_wk_bd"] = bd_layout(inp["ml_w_k"]); S["ml_wv_bd"] = bd_layout(inp["ml_w_v"])
    wg = np.asarray(inp["ml_w_gates"], f).reshape(2, 3, 16, 128, 16)
    S["ml_wg"] = np.ascontiguousarray(wg.transpose(0, 3, 1, 2, 4).reshape(2, 128, 768))
    S["ml_bg_rep"] = np.ascontiguousarray(np.broadcast_to(np.asarray(inp["ml_b_gates"], f)[None], (128, 2, 16)))
    S["ml_gnT"] = np.ascontiguousarray(inp["ml_gn_w"].reshape(2, 16, 128).transpose(2, 0, 1), dtype=f)
    S["ml_skipT"] = np.ascontiguousarray(inp["ml_skip"].reshape(2, 16, 128).transpose(2, 0, 1), dtype=f)
    return S


_NC_CACHE = {}


def kernel(**inputs):
    inp = {k: np.asarray(v) for k, v in inputs.items()}
    S = _prep_shared(inp)
    x = np.asarray(inp["x"], np.float32); c = np.asarray(inp["c"], np.float32)
    in_maps = []
    for core in range(NCORES):
        m = dict(S)
        m["x"] = np.ascontiguousarray(x[core * NB:(core + 1) * NB].reshape(NT, D))
        cc = c[core * NB:(core + 1) * NB]
        m["cT"] = np.ascontiguousarray(cc.reshape(NB, 8, 128).transpose(2, 1, 0))
        in_maps.append(m)
    if "nc" not in _NC_CACHE:
        _NC_CACHE["nc"] = build_program()
    res = run_bass_kernel_spmd(_NC_CACHE["nc"], in_maps, core_ids=list(range(NCORES)))
    out = np.concatenate([r["out"].reshape(NB, L, D) for r in res.results], axis=0)
    return out.astype(np.float32)
```
